# Optimizing a Trainium2 kernel written in Bass

```python
import math
import jax, jax.numpy as jnp
from jax import lax
import numpy as np

D_MODEL = 2048
BATCH = 1
SEQ = 16384
DEPTH = 1

GRID_W = 64
CTX_LEN = 256
MIX_W = D_MODEL
ATTN_W = MIX_W // 2
HEAD_DIM = 128
N_HEADS = ATTN_W // HEAD_DIM
N_KV_HEADS = 2
GQA_GROUP = N_HEADS // N_KV_HEADS
KV_W = N_KV_HEADS * HEAD_DIM
ATTN_SCALE = HEAD_DIM ** -0.5
ROPE_THETA = 10000.0
ROPE_AXIS_DIM = HEAD_DIM // 2
Q_BLOCK = 128
RG_W = MIX_W - ATTN_W
RG_HEADS = 8
RG_HD = RG_W // RG_HEADS
CONV_W = 4
RG_C = 8.0
PROJ_W = ATTN_W + 2 * KV_W + 2 * RG_W
N_EXPERTS = 64
N_GROUPS = 8
TOPK_GROUPS = 4
TOP_K = 8
EXPERT_FF = 512
SHARED_FF = 512
ROUTED_SCALE = 2.5
MOE_BLOCK = 128
NORM_EPS = 1e-6
DEEPNORM_ALPHA = (2.0 * DEPTH) ** 0.25
DEEPNORM_BETA = (8.0 * DEPTH) ** -0.25

kernel_name = "hybrid_attn_rglru_moe_dit_layer"


def _layer_norm(x, g, b):
    xf = x.astype(jnp.float32)
    mu = jnp.mean(xf, axis=-1, keepdims=True)
    var = jnp.mean(jnp.square(xf - mu), axis=-1, keepdims=True)
    y = (xf - mu) * lax.rsqrt(var + NORM_EPS) * g.astype(jnp.float32) + b.astype(jnp.float32)
    return y.astype(x.dtype)


def _rms_norm(x, g):
    xf = x.astype(jnp.float32)
    y = xf * lax.rsqrt(jnp.mean(jnp.square(xf), axis=-1, keepdims=True) + NORM_EPS)
    return (y * g.astype(jnp.float32)).astype(x.dtype)


def _rope_axis(x, pos):
    half = ROPE_AXIS_DIM // 2
    inv_freq = ROPE_THETA ** (-jnp.arange(half, dtype=jnp.float32) / half)
    ang = pos.astype(jnp.float32)[:, None] * inv_freq[None, :]
    cos = jnp.concatenate([jnp.cos(ang), jnp.cos(ang)], axis=-1)[None, :, None, :]
    sin = jnp.concatenate([jnp.sin(ang), jnp.sin(ang)], axis=-1)[None, :, None, :]
    xf = x.astype(jnp.float32)
    rot = jnp.concatenate([-xf[..., half:], xf[..., :half]], axis=-1)
    return (xf * cos + rot * sin).astype(x.dtype)


def _rope_2d(x, row, col):
    return jnp.concatenate([_rope_axis(x[..., :ROPE_AXIS_DIM], row),
                            _rope_axis(x[..., ROPE_AXIS_DIM:], col)], axis=-1)


def _block_attention(q, k, v):
    B, Sq = q.shape[0], q.shape[1]
    nblk = Sq // Q_BLOCK
    qb = q.reshape(B, nblk, Q_BLOCK, N_KV_HEADS, GQA_GROUP, HEAD_DIM).transpose(1, 0, 2, 3, 4, 5)

    def one_block(qblk):
        s = jnp.einsum('bqkgd,bskd->bkgqs', qblk, k).astype(jnp.float32) * ATTN_SCALE
        p = jax.nn.softmax(s, axis=-1)
        return jnp.einsum('bkgqs,bskd->bqkgd', p.astype(v.dtype), v)

    o = lax.map(one_block, qb)
    return o.transpose(1, 0, 2, 3, 4, 5).reshape(B, Sq, ATTN_W)


def _centred_dwconv(x, w, b):
    S = x.shape[1]
    left = CONV_W // 2
    right = CONV_W - 1 - left
    xp = jnp.pad(x, ((0, 0), (left, right), (0, 0)))
    out = b
    for j in range(CONV_W):
        out = out + w[j] * xp[:, j:j + S]
    return out


def _rglru_coeffs(xc, w_a, b_a, w_x, b_x, lam):
    B, S, _ = xc.shape
    xh = xc.reshape(B, S, RG_HEADS, RG_HD)
    r = jax.nn.sigmoid((jnp.einsum('bshi,hij->bshj', xh, w_a).reshape(B, S, RG_W) + b_a).astype(jnp.float32))
    i = jax.nn.sigmoid((jnp.einsum('bshi,hij->bshj', xh, w_x).reshape(B, S, RG_W) + b_x).astype(jnp.float32))
    log_a = RG_C * r * jax.nn.log_sigmoid(lam.astype(jnp.float32))
    a = jnp.exp(log_a)
    b = jnp.sqrt(-jnp.expm1(2.0 * log_a)) * (i * xc.astype(jnp.float32))
    return a, b


def _combine(left, right):
    a1, b1 = left
    a2, b2 = right
    return a1 * a2, a2 * b1 + b2


def _linear_scan(a, b, h0, reverse):
    a_cum, h = lax.associative_scan(_combine, (a, b), axis=1, reverse=reverse)
    return h + a_cum * h0[:, None, :]


def _split_proj(p):
    B, S = p.shape[0], p.shape[1]
    q, k, v, xr, yr = jnp.split(p, [ATTN_W, ATTN_W + KV_W, ATTN_W + 2 * KV_W,
                                   ATTN_W + 2 * KV_W + RG_W], axis=-1)
    return (q.reshape(B, S, N_HEADS, HEAD_DIM), k.reshape(B, S, N_KV_HEADS, HEAD_DIM),
            v.reshape(B, S, N_KV_HEADS, HEAD_DIM), xr, yr)


def _token_mixer(u_lat, u_ctx, row, col, w_in, q_norm, k_norm, conv_w, conv_b,
                 rg_wa, rg_ba, rg_wx, rg_bx, rg_lam, w_out, with_ctx_out):
    q_l, k_l, v_l, xr_l, yr_l = _split_proj(u_lat @ w_in)
    q_c, k_c, v_c, xr_c, yr_c = _split_proj(u_ctx @ w_in)
    q_l = _rope_2d(_rms_norm(q_l, q_norm), row, col)
    k_l = _rope_2d(_rms_norm(k_l, k_norm), row, col)
    k_c = _rms_norm(k_c, k_norm)
    k_all = jnp.concatenate([k_c, k_l], axis=1)
    v_all = jnp.concatenate([v_c, v_l], axis=1)
    attn_l = _block_attention(q_l, k_all, v_all)
    xc_c = _centred_dwconv(xr_c, conv_w, conv_b)
    xc_l = _centred_dwconv(xr_l, conv_w, conv_b)
    B = u_lat.shape[0]
    zero_state = jnp.zeros((B, RG_W), jnp.float32)
    rg_l = jnp.zeros(xc_l.shape, jnp.float32)
    ctx_scans = []
    for d, reverse in enumerate((False, True)):
        a_c, b_c = _rglru_coeffs(xc_c, rg_wa[d], rg_ba[d], rg_wx[d], rg_bx[d], rg_lam[d])
        h_c = _linear_scan(a_c, b_c, zero_state, reverse)
        ctx_scans.append(h_c)
        h0 = h_c[:, 0] if reverse else h_c[:, -1]
        a_l, b_l = _rglru_coeffs(xc_l, rg_wa[d], rg_ba[d], rg_wx[d], rg_bx[d], rg_lam[d])
        rg_l = rg_l + _linear_scan(a_l, b_l, h0, reverse)
    rg_l = (rg_l * jax.nn.gelu(yr_l).astype(jnp.float32)).astype(u_lat.dtype)
    out_l = jnp.concatenate([attn_l, rg_l], axis=-1) @ w_out
    if not with_ctx_out:
        return out_l, None
    attn_c = _block_attention(_rms_norm(q_c, q_norm), k_c, v_c)
    rg_c = ((ctx_scans[0] + ctx_scans[1]) * jax.nn.gelu(yr_c).astype(jnp.float32)).astype(u_ctx.dtype)
    out_c = jnp.concatenate([attn_c, rg_c], axis=-1) @ w_out
    return out_l, out_c


def _swiglu(x, w1, w3, w2):
    return (jax.nn.silu(x @ w1) * (x @ w3)) @ w2


def _moe(h, w_router, e_bias, w_e1, w_e3, w_e2, w_s1, w_s3, w_s2):
    n, d = h.shape
    scores = jax.nn.sigmoid((h @ w_router).astype(jnp.float32))
    biased = scores + e_bias.astype(jnp.float32)
    per_group = N_EXPERTS // N_GROUPS
    group_score = lax.top_k(biased.reshape(n, N_GROUPS, per_group), 2)[0].sum(-1)
    _, top_groups = lax.top_k(group_score, TOPK_GROUPS)
    group_mask = jax.nn.one_hot(top_groups, N_GROUPS, dtype=jnp.float32).sum(1) > 0
    expert_mask = jnp.repeat(group_mask, per_group, axis=1)
    _, idx = lax.top_k(jnp.where(expert_mask, biased, -jnp.inf), TOP_K)
    wts = jnp.take_along_axis(scores, idx, axis=-1)
    wts = wts / jnp.sum(wts, axis=-1, keepdims=True) * ROUTED_SCALE
    nk = n * TOP_K
    flat_e = idx.reshape(nk)
    flat_t = jnp.repeat(jnp.arange(n, dtype=jnp.int32), TOP_K)
    flat_w = wts.reshape(nk)
    order = jnp.argsort(flat_e)
    e_s, t_s, w_s = flat_e[order], flat_t[order], flat_w[order]
    counts = jnp.bincount(flat_e, length=N_EXPERTS)
    start = jnp.cumsum(counts) - counts
    padded = (counts + MOE_BLOCK - 1) // MOE_BLOCK * MOE_BLOCK
    pad_end = jnp.cumsum(padded)
    pad_start = pad_end - padded
    dest = pad_start[e_s] + jnp.arange(nk, dtype=jnp.int32) - start[e_s]
    n_blocks = -(-nk // MOE_BLOCK) + N_EXPERTS
    slot_tok = jnp.full((n_blocks * MOE_BLOCK,), n, jnp.int32).at[dest].set(t_s)
    slot_w = jnp.zeros((n_blocks * MOE_BLOCK,), jnp.float32).at[dest].set(w_s)
    blk_e = jnp.minimum(jnp.searchsorted(pad_end, jnp.arange(n_blocks, dtype=jnp.int32) * MOE_BLOCK,
                                         side='right'), N_EXPERTS - 1)
    h_pad = jnp.concatenate([h, jnp.zeros((1, d), h.dtype)], axis=0)

    def body(acc, blk):
        tok, wt, e = blk
        xb = h_pad[tok]
        yb = _swiglu(xb, w_e1[e], w_e3[e], w_e2[e]) * wt[:, None].astype(h.dtype)
        return acc.at[tok].add(yb), None

    acc, _ = lax.scan(body, jnp.zeros((n + 1, d), h.dtype),
                      (slot_tok.reshape(n_blocks, MOE_BLOCK), slot_w.reshape(n_blocks, MOE_BLOCK), blk_e))
    return acc[:n] + _swiglu(h, w_s1, w_s3, w_s2)


def setup_inputs(seed: int = 0) -> dict:
    key = jax.random.key(seed)
    ks = jax.random.split(key, 32)
    f32 = jnp.float32
    L, D = DEPTH, D_MODEL

    def nrm(k, shape, scale):
        return jax.random.normal(k, shape, f32) * scale

    u = jax.random.uniform(ks[13], (L, 2, RG_W), f32, 0.9, 0.999)
    a0 = u ** (1.0 / RG_C)
    return {
        "x": nrm(ks[0], (BATCH, SEQ, D), 1.0),
        "c": nrm(ks[1], (BATCH, D), 1.0),
        "ctx": nrm(ks[2], (BATCH, CTX_LEN, D), 1.0),
        "c_ctx": nrm(ks[3], (D,), 1.0),
        "w_mod": nrm(ks[4], (L, D, 6 * D), 0.5 * D ** -0.5),
        "b_mod": nrm(ks[5], (L, 6 * D), 0.02),
        "w_in": nrm(ks[6], (L, D, PROJ_W), D ** -0.5),
        "q_norm": 1.0 + nrm(ks[7], (L, HEAD_DIM), 0.02),
        "k_norm": 1.0 + nrm(ks[8], (L, HEAD_DIM), 0.02),
        "conv_w": nrm(ks[9], (L, CONV_W, RG_W), CONV_W ** -0.5),
        "conv_b": nrm(ks[10], (L, RG_W), 0.02),
        "rg_wa": nrm(ks[11], (L, 2, RG_HEADS, RG_HD, RG_HD), RG_HD ** -0.5),
        "rg_ba": nrm(ks[12], (L, 2, RG_W), 0.02),
        "rg_wx": nrm(ks[14], (L, 2, RG_HEADS, RG_HD, RG_HD), RG_HD ** -0.5),
        "rg_bx": nrm(ks[15], (L, 2, RG_W), 0.02),
        "rg_lam": jnp.log(a0) - jnp.log1p(-a0),
        "w_out": nrm(ks[16], (L, MIX_W, D), MIX_W ** -0.5 * DEEPNORM_BETA),
        "ln1_g": 1.0 + nrm(ks[17], (L, D), 0.02),
        "ln1_b": nrm(ks[18], (L, D), 0.02),
        "w_router": nrm(ks[19], (L, D, N_EXPERTS), D ** -0.5),
        "e_bias": nrm(ks[20], (L, N_EXPERTS), 0.01),
        "w_e1": nrm(ks[21], (L, N_EXPERTS, D, EXPERT_FF), D ** -0.5),
        "w_e3": nrm(ks[22], (L, N_EXPERTS, D, EXPERT_FF), D ** -0.5),
        "w_e2": nrm(ks[23], (L, N_EXPERTS, EXPERT_FF, D), EXPERT_FF ** -0.5 * DEEPNORM_BETA),
        "w_s1": nrm(ks[24], (L, D, SHARED_FF), D ** -0.5),
        "w_s3": nrm(ks[25], (L, D, SHARED_FF), D ** -0.5),
        "w_s2": nrm(ks[26], (L, SHARED_FF, D), SHARED_FF ** -0.5 * DEEPNORM_BETA),
        "ln2_g": 1.0 + nrm(ks[27], (L, D), 0.02),
        "ln2_b": nrm(ks[28], (L, D), 0.02),
    }


def reference(x, c, ctx, c_ctx, w_mod, b_mod, w_in, q_norm, k_norm, conv_w, conv_b,
              rg_wa, rg_ba, rg_wx, rg_bx, rg_lam, w_out, ln1_g, ln1_b, w_router, e_bias,
              w_e1, w_e3, w_e2, w_s1, w_s3, w_s2, ln2_g, ln2_b):
    B, S, D = x.shape
    n_ctx = ctx.shape[1]
    rows = S // GRID_W
    row = jnp.repeat(jnp.arange(rows, dtype=jnp.int32), GRID_W)
    col = jnp.tile(jnp.arange(GRID_W, dtype=jnp.int32), rows)
    h_lat, h_ctx = x, ctx
    for l in range(DEPTH):
        update_ctx = l + 1 < DEPTH
        mod_lat = jax.nn.silu(c) @ w_mod[l] + b_mod[l]
        mod_ctx = jax.nn.silu(c_ctx) @ w_mod[l] + b_mod[l]
        sh1, sc1, g1, sh2, sc2, g2 = jnp.split(mod_lat[:, None, :], 6, axis=-1)
        csh1, csc1, cg1, csh2, csc2, cg2 = jnp.split(mod_ctx, 6, axis=-1)
        u_lat = h_lat * (1.0 + sc1) + sh1
        u_ctx = h_ctx * (1.0 + csc1) + csh1
        mix_lat, mix_ctx = _token_mixer(u_lat, u_ctx, row, col, w_in[l], q_norm[l], k_norm[l],
                                        conv_w[l], conv_b[l], rg_wa[l], rg_ba[l], rg_wx[l],
                                        rg_bx[l], rg_lam[l], w_out[l], update_ctx)
        h_lat = _layer_norm(DEEPNORM_ALPHA * h_lat + g1 * mix_lat, ln1_g[l], ln1_b[l])
        v_lat = (h_lat * (1.0 + sc2) + sh2).reshape(B * S, D)
        if update_ctx:
            h_ctx = _layer_norm(DEEPNORM_ALPHA * h_ctx + cg1 * mix_ctx, ln1_g[l], ln1_b[l])
            v_ctx = (h_ctx * (1.0 + csc2) + csh2).reshape(B * n_ctx, D)
            ff = _moe(jnp.concatenate([v_ctx, v_lat], axis=0), w_router[l], e_bias[l],
                      w_e1[l], w_e3[l], w_e2[l], w_s1[l], w_s3[l], w_s2[l])
            ff_ctx = ff[:B * n_ctx].reshape(B, n_ctx, D)
            ff_lat = ff[B * n_ctx:].reshape(B, S, D)
            h_ctx = _layer_norm(DEEPNORM_ALPHA * h_ctx + cg2 * ff_ctx, ln2_g[l], ln2_b[l])
        else:
            ff_lat = _moe(v_lat, w_router[l], e_bias[l], w_e1[l], w_e3[l], w_e2[l],
                          w_s1[l], w_s3[l], w_s2[l]).reshape(B, S, D)
        h_lat = _layer_norm(DEEPNORM_ALPHA * h_lat + g2 * ff_lat, ln2_g[l], ln2_b[l])
    return h_lat
```

```python
import numpy as np
import concourse.bass as bass
import concourse.mybir as mybir
from concourse.bass_utils import run_bass_kernel_spmd

F32 = mybir.dt.float32
BF16 = mybir.dt.bfloat16
ALU = mybir.AluOpType
AF = mybir.ActivationFunctionType
AX = mybir.AxisListType

D = 2048
KC = 16
NCTX = 256
NE = 64
FF = 512
ALPHA = 2.0 ** 0.25
EPS = 1e-6
ATT_SCALE = 128.0 ** -0.5
ARENA_F = 50176

TRACE_LOG = None
ENGS = ("pe", "act", "dve", "pool", "sp")
EPOCH = 12000
DPOOL = 8
QDEPTH = {"pool": 1}
QSLOTS = {"pool": 4}


class Prog:
    def __init__(self, nc):
        self.nc = nc
        self.ops = []
        self.lastw = {}
        self.rd_c = {}
        self.rd_d = {}
        self.barrier = set()
        self.eng_ops = {e: [] for e in ENGS}
        self.ncomp = {e: 0 for e in ENGS}
        self.ndma = {e: 0 for e in ENGS}
        self.dma_ids = {e: [] for e in ENGS}

    def add(self, eng, fn, r=(), w=(), dma=False):
        oid = len(self.ops)
        deps = set(self.barrier)
        for k in r:
            if k in self.lastw:
                deps.add(self.lastw[k])
        for k in w:
            if k in self.lastw:
                deps.add(self.lastw[k])
            deps.update(self.rd_c.get(k, {}).values())
            deps.update(self.rd_d.get(k, ()))
        for k in r:
            if dma:
                self.rd_d.setdefault(k, []).append(oid)
            else:
                self.rd_c.setdefault(k, {})[eng] = oid
        for k in w:
            self.lastw[k] = oid
            self.rd_c[k] = {}
            self.rd_d[k] = []
        op = dict(eng=eng, fn=fn, deps=deps, dma=dma, tag=(tuple(r), tuple(w)))
        if dma:
            i = self.ndma[eng]
            self.ndma[eng] += 1
            dp = QDEPTH.get(eng, DPOOL)
            ns = QSLOTS.get(eng, DPOOL)
            op["slot"] = i % ns
            op["target"] = 16 * (i // ns + 1)
            if i >= dp:
                deps.add(self.dma_ids[eng][i - dp])
            self.dma_ids[eng].append(oid)
        else:
            n = self.ncomp[eng]
            self.ncomp[eng] += 1
            op["epoch"] = n // EPOCH
            op["idx"] = n % EPOCH + 1
        self.ops.append(op)
        self.eng_ops[eng].append(oid)
        return oid

    def fence(self):
        b = set()
        for e in ENGS:
            if self.eng_ops[e]:
                comp = [o for o in self.eng_ops[e] if not self.ops[o]["dma"]]
                if comp:
                    b.add(comp[-1])
            b.update(self.dma_ids[e][-DPOOL:])
        self.barrier = b

    def emit(self, stack):
        nc = self.nc
        csem = {}
        dsem = {}
        for e in ENGS:
            for ep in range(self.ncomp[e] // EPOCH + 1):
                csem[(e, ep)] = stack.enter_context(nc.semaphore(f"c_{e}_{ep}"))
            if self.ndma[e]:
                for s in range(DPOOL):
                    dsem[(e, s)] = stack.enter_context(nc.semaphore(f"d_{e}_{s}"))
        block = stack.enter_context(nc.Block())
        ops = self.ops

        def run(engname, eng):
            waited = {}
            for oid in self.eng_ops[engname]:
                op = ops[oid]
                need = {}
                for d in op["deps"]:
                    p = ops[d]
                    if p["dma"]:
                        key = ("d", p["eng"], p["slot"])
                        val = p["target"]
                    else:
                        if p["eng"] == engname and engname == "pe":
                            continue
                        key = ("c", p["eng"], p["epoch"])
                        val = p["idx"]
                    if need.get(key, 0) < val:
                        need[key] = val
                for key, val in need.items():
                    if waited.get(key, 0) >= val:
                        continue
                    waited[key] = val
                    sem = dsem[(key[1], key[2])] if key[0] == "d" else csem[(key[1], key[2])]
                    eng.wait_ge(sem, val)
                    if TRACE_LOG is not None:
                        TRACE_LOG.append((engname, oid, "wait", key, val))
                if TRACE_LOG is not None:
                    TRACE_LOG.append((engname, oid, "op", op.get("tag"), (op.get("slot"), op.get("target")) if op["dma"] else (op["epoch"], op["idx"])))
                inst = op["fn"](eng)
                if op["dma"]:
                    inst.then_inc(dsem[(engname, op["slot"])], 16)
                else:
                    inst.then_inc(csem[(engname, op["epoch"])], 1)
            for oid in self.dma_ids[engname][-DPOOL:]:
                op = ops[oid]
                key = ("d", engname, op["slot"])
                if waited.get(key, 0) < op["target"]:
                    waited[key] = op["target"]
                    eng.wait_ge(dsem[(engname, op["slot"])], op["target"])

        @block.tensor
        def _(e):
            run("pe", e)

        @block.scalar
        def _(e):
            run("act", e)

        @block.vector
        def _(e):
            run("dve", e)

        @block.gpsimd
        def _(e):
            run("pool", e)

        @block.sync
        def _(e):
            run("sp", e)


class Arena:
    def __init__(self, ap):
        self.ap = ap
        self.lo = 0
        self.hi = ARENA_F

    def reset(self, lo=0, hi=ARENA_F):
        self.lo = lo
        self.hi = hi

    def f32(self, n0, top=False):
        n = (n0 + 7) // 8 * 8
        if top:
            self.hi -= n
            off = self.hi
        else:
            off = self.lo
            self.lo += n
        assert self.lo <= self.hi, ("arena overflow", self.lo, self.hi)
        return self.ap[:, off:off + n0]

    def bf16(self, n, top=False):
        nf = (n + 1) // 2
        a = self.f32(nf, top=top)
        return a.bitcast(BF16)[:, 0:n]


def r3(ap, a):
    return ap.rearrange("p (a b) -> p a b", a=a)


def build(S, stop=99, nexp=NE, debug=False, osub=9):
    TOK = S // 8
    NB = S // 512
    NBO = TOK // 512
    NTO = TOK // 128
    SA = NCTX + S
    NKC = SA // 128
    TH = min(TOK, 1024)
    NH = TOK // TH
    nc = bass.Bass("TRN2", target_bir_lowering=False)

    def din(name, shape, dt=F32):
        return nc.dram_tensor(name, list(shape), dt, kind="ExternalInput").ap()

    x_all = din("x_all", [S, D]); x_own = din("x_own", [TOK, D]); ctx = din("ctx", [NCTX, D])
    cT = din("cT", [128, 32])
    w_mod = din("w_mod", [D, 6 * D]); b_mod_bc = din("b_mod_bc", [128, 6 * D])
    w_in = din("w_in", [D, 3584])
    qk_g = din("qk_g", [128, 2])
    conv_wT = din("conv_wT", [128, 32]); conv_bT = din("conv_bT", [128, 8])
    rg_w = din("rg_w", [128, 32 * 128])
    rg_vec = din("rg_vec", [128, 48])
    w_out = din("w_out", [D, D])
    ln_bc = din("ln_bc", [4, 128, D])
    w_router = din("w_router", [D, NE]); eb_bc = din("eb_bc", [128, NE])
    w_e1 = din("w_e1", [nexp + 1, D, FF]); w_e3 = din("w_e3", [nexp + 1, D, FF]); w_e2 = din("w_e2", [nexp + 1, FF, D])
    cs_all = din("cs_all", [2, 128, S]); cs_own = din("cs_own", [2, 128, TOK])
    consts = din("consts", [4, 128, 128])
    sel_in = din("sel", [128, 8])
    out = nc.dram_tensor("out", [TOK, D], F32, kind="ExternalOutput").ap()

    skind = "ExternalOutput" if debug else "Internal"

    def dscr(name, shape, dt):
        return nc.dram_tensor(name, list(shape), dt, kind=skind).ap()

    kT_s = dscr("kT_s", [2, 128, SA], BF16)
    V_s = dscr("V_s", [SA, 256], BF16)
    XP = S + 8
    xr_s = dscr("xr_s", [1024, XP], F32)
    xrc_s = dscr("xrc_s", [1024, NCTX + 8], F32)
    gy_s = dscr("gy_s", [1024, TOK], F32)
    h1_s = dscr("h1_s", [TOK, D], F32)
    vT_s = dscr("vT_s", [128, KC * TOK], BF16)
    mod_s = dscr("mod_s", [8, 128, D], F32)
    mix_s = dscr("mix_s", [128, KC * TOK], BF16)

    import contextlib
    stack = contextlib.ExitStack()
    with stack:
        def sb(name, shape, dt=F32):
            return stack.enter_context(nc.sbuf_tensor("sb_" + name, list(shape), dt))

        FA = sb("arena", [128, ARENA_F])
        identf = sb("identf", [128, 128]); onesf = sb("onesf", [128, 128]); RTf = sb("RTf", [128, 128])
        zerof = sb("zerof", [128, 128])
        identb = sb("identb", [128, 128], BF16); onesb = sb("onesb", [128, 128], BF16)
        sT = sb("sT", [128, 32]); csT = sb("csT", [128, 32])
        qkg = sb("qkg", [128, 2]); qkg2 = sb("qkg2", [128, 2])
        cwT = sb("cwT", [128, 32]); cbT = sb("cbT", [128, 8])
        rgv = sb("rgv", [128, 48]); c8 = sb("c8", [128, 16]); c8t = sb("c8t", [128, 16])
        sel = sb("sel", [128, 8]); ebb = sb("ebb", [128, NE])
        Wr = sb("Wr", [128, NTO * (NE + 1)])
        ps = [stack.enter_context(nc.psum_tensor(f"ps{i}", [128, 512], F32)) for i in range(6)]
        ptb = [stack.enter_context(nc.psum_tensor(f"ptb{i}", [128, 1024], BF16)) for i in range(2)]

        P = Prog(nc)
        A = Arena(FA)
        uid = [0]

        def K(name):
            uid[0] += 1
            return (name, uid[0])

        def dma(q, out_ap, in_ap, r=(), w=(), **kw):
            P.add(q, lambda e: e.dma_start(out=out_ap, in_=in_ap, **kw), r=r, w=w, dma=True)

        def mm(o, lhsT, rhs, start, stop, r=(), w=()):
            P.add("pe", lambda e: e.matmul(o, lhsT, rhs, start=start, stop=stop), r=r, w=w)

        def tr(o, in_, ident, r=(), w=()):
            P.add("pe", lambda e: e.transpose(o, in_, ident), r=r, w=w)

        def act(o, in_, func, r=(), w=(), **kw):
            P.add("act", lambda e: e.activation(out=o, in_=in_, func=func, **kw), r=r, w=w)

        def tt(eng, o, a, b, op, r=(), w=()):
            P.add(eng, lambda e: e.tensor_tensor(out=o, in0=a, in1=b, op=op), r=r, w=w)

        def ts(eng, o, a, s1, s2, op0, op1=None, r=(), w=()):
            if op1 is None:
                P.add(eng, lambda e: e.tensor_scalar(out=o, in0=a, scalar1=s1, scalar2=None, op0=op0), r=r, w=w)
            else:
                P.add(eng, lambda e: e.tensor_scalar(out=o, in0=a, scalar1=s1, scalar2=s2, op0=op0, op1=op1), r=r, w=w)

        def stt(eng, o, a, s, b, op0, op1, r=(), w=()):
            P.add(eng, lambda e: e.scalar_tensor_tensor(out=o, in0=a, scalar=s, in1=b, op0=op0, op1=op1), r=r, w=w)

        def cp(eng, o, a, r=(), w=()):
            P.add(eng, lambda e: e.tensor_copy(out=o, in_=a), r=r, w=w)

        dly_t = sb("dly_t", [128, 512])

        def settle():
            P.fence()
            for _i in range(32):
                dma("sp", dly_t[:, :], x_own[0:128, 0:512], w=["dly_t"])
            P.fence()

        kc_ = "const"
        for i, t in enumerate((identf, onesf, RTf, zerof)):
            dma("sp", t[:, :], consts[i], w=[kc_])
        for src, dst in ((cT, csT), (qk_g, qkg), (conv_wT, cwT), (conv_bT, cbT), (rg_vec, rgv),
                         (sel_in, sel), (eb_bc, ebb)):
            dma("sp", dst[:, :], src, w=[kc_])
        cp("dve", identb[:, :], identf[:, :], r=[kc_], w=["identb"])
        cp("dve", onesb[:, :], onesf[:, :], r=[kc_], w=["onesb"])
        ts("dve", qkg2[:, :], qkg[:, :], float(128.0 ** 0.5), None, ALU.mult, r=[kc_], w=["qkg2"])
        act(sT[:, :], csT[:, :], AF.Silu, r=[kc_], w=["sT"])
        act(c8t[:, :], rgv[:, 32:48], AF.Exp, scale=-1.0, r=[kc_], w=["c8t"])
        act(c8[:, :], c8t[:, :], AF.Ln, bias=1.0, r=["c8t"], w=["c8"])
        ts("dve", c8[:, :], c8[:, :], -8.0, None, ALU.mult, r=["c8"], w=["c8"])
        for rr in range(8):
            rows = slice(rr * 128, (rr + 1) * 128)
            dma("sp", xr_s[rows, 0:2], zerof[:, 0:2], r=[kc_], w=["xr_s"])
            dma("sp", xr_s[rows, S + 2:S + 8], zerof[:, 0:6], r=[kc_], w=["xr_s"])
            dma("sp", xrc_s[rows, 0:2], zerof[:, 0:2], r=[kc_], w=["xrc_s"])
            dma("sp", xrc_s[rows, NCTX + 2:NCTX + 8], zerof[:, 0:6], r=[kc_], w=["xrc_s"])

        A.reset()
        sbc = A.f32(KC * 2 * 128)
        sbc4 = sbc.rearrange("p (a v m) -> p a v m", a=KC, v=2)
        for kc in range(KC):
            for v in range(2):
                ts("dve", sbc4[:, kc, v, :], onesf[:, :], sT[:, 2 * kc + v:2 * kc + v + 1], None, ALU.mult,
                   r=["sT", kc_], w=["sbc"])
        wm = [r3(A.f32(KC * 512), KC) for _ in range(2)]
        bmb = [A.f32(512) for _ in range(2)]
        mo = [A.f32(512) for _ in range(2)]
        MODV = {(0, 0): 0, (1, 0): 1, (0, 1): 2, (1, 1): 3, (2, 0): 4, (3, 0): 5, (4, 0): 6, (5, 0): 7}
        cnt = 0
        for nb in range(24):
            sec = nb // 4
            buf = nb % 2
            col = slice(nb * 512, (nb + 1) * 512)
            wsrc = w_mod[:, col].rearrange("(a p) n -> p a n", p=128)
            for c0 in range(0, KC, 4):
                dma("sp", wm[buf][:, c0:c0 + 4, :], wsrc[:, c0:c0 + 4, :], w=[("wm", buf)])
            dma("sp", bmb[buf], b_mod_bc[:, col], w=[("bmb", buf)])
            for v in ((0, 1) if sec < 2 else (0,)):
                pt = ps[cnt % 2]; pk = ("ps", cnt % 2)
                for kc in range(KC):
                    mm(pt[:, :], sbc4[:, kc, v, :], wm[buf][:, kc, :], kc == 0, kc == KC - 1,
                       r=[("wm", buf), "sbc"], w=[pk])
                mb = mo[cnt % 2]; mk = ("mo", cnt % 2)
                tt("dve", mb, pt[:, :], bmb[buf], ALU.add, r=[pk, ("bmb", buf)], w=[mk])
                if sec in (1, 4):
                    ts("dve", mb, mb, 1.0, None, ALU.add, r=[mk], w=[mk])
                idx = MODV[(sec, v)]
                dma("sp", mod_s[idx][:, (nb % 4) * 512:(nb % 4 + 1) * 512], mb, r=[mk], w=[("mod_s", idx)])
                cnt += 1
        settle()

        def norm_rope(praw, n, gcol, lat, cosb, sinb, kout, tmp, rkeys, wkeys):
            sq, t0, kn, t1 = tmp
            act(sq[:, :n], praw[:, :n], AF.Square, r=rkeys, w=["nr_sq"])
            mm(ps[1][:, :n], onesf[:, :], sq[:, :n], True, True, r=["nr_sq", kc_], w=[("ps", 1)])
            act(t0[:, :n], ps[1][:, :n], AF.Sqrt, bias=float(128 * EPS), r=[("ps", 1)], w=["nr_t0"])
            P.add("dve", lambda e: e.reciprocal(out=t0[:, :n], in_=t0[:, :n]), r=["nr_t0"], w=["nr_t0"])
            stt("dve", kn[:, :n], praw[:, :n], qkg2[:, gcol:gcol + 1], t0[:, :n], ALU.mult, ALU.mult,
                r=rkeys + ["nr_t0", "qkg2"], w=["nr_kn"])
            if lat:
                mm(ps[1][:, :n], RTf[:, :], kn[:, :n], True, True, r=["nr_kn", kc_], w=[("ps", 1)])
                tt("dve", t1[:, :n], kn[:, :n], cosb[:, :n], ALU.mult, r=["nr_kn", "cs"], w=["nr_t1"])
                tt("dve", sq[:, :n], ps[1][:, :n], sinb[:, :n], ALU.mult, r=[("ps", 1), "cs"], w=["nr_sq"])
                tt("dve", kout, t1[:, :n], sq[:, :n], ALU.add, r=["nr_t1", "nr_sq"], w=wkeys)
            else:
                cp("dve", kout, kn[:, :n], r=["nr_kn"], w=wkeys)

        def proj_pass(blocks, scb, shb, consumer):
            xt = [A.f32(D) for _ in range(2)]
            ub = [A.bf16(D) for _ in range(2)]
            uT = [r3(A.bf16(KC * 512), KC) for _ in range(2)]
            tcount = 0
            for bi, (src, ntl, isc, info) in enumerate(blocks):
                ubuf = bi % 2
                for t in range(ntl):
                    b = tcount % 2
                    tcount += 1
                    dma("sp", xt[b], src[t * 128:(t + 1) * 128, :], w=[("xt", b)])
                    tt("dve", xt[b], xt[b], scb[isc], ALU.mult, r=[("xt", b), "bcmod"], w=[("xt", b)])
                    tt("dve", ub[b], xt[b], shb[isc], ALU.add, r=[("xt", b), "bcmod"], w=[("ub", b)])
                    for hh in range(2):
                        for c in range(8):
                            kc = hh * 8 + c
                            tr(ptb[hh][:, c * 128:(c + 1) * 128], ub[b][:, kc * 128:(kc + 1) * 128], identb[:, :],
                               r=[("ub", b), "identb"], w=[("ptb", hh)])
                        act(uT[ubuf][:, hh * 8:(hh + 1) * 8, t * 128:(t + 1) * 128],
                            r3(ptb[hh][:, :], 8), AF.Copy, r=[("ptb", hh)], w=[("uT", ubuf)])
                consumer(uT[ubuf], ("uT", ubuf), ntl, isc, info)

        if stop <= 0:
            P.emit(stack)
            return nc
        A.reset()
        scb = [A.f32(D, top=True), A.f32(D, top=True)]
        shb = [A.f32(D, top=True), A.f32(D, top=True)]
        dma("sp", scb[0], mod_s[1], r=[("mod_s", 1)], w=["bcmod"])
        dma("sp", shb[0], mod_s[0], r=[("mod_s", 0)], w=["bcmod"])
        dma("sp", scb[1], mod_s[3], r=[("mod_s", 3)], w=["bcmod"])
        dma("sp", shb[1], mod_s[2], r=[("mod_s", 2)], w=["bcmod"])
        wA = r3(A.bf16(KC * 1536), KC)
        for kc in range(KC):
            dma("pool", wA[:, kc, :], w_in[kc * 128:(kc + 1) * 128, 1024:2560], w=["wA"])
        cosb = A.f32(512); sinb = A.f32(512)
        nrt = [A.f32(512) for _ in range(4)]
        kob = [A.bf16(512) for _ in range(2)]
        vb = [A.bf16(256) for _ in range(2)]
        xst = [A.f32(512) for _ in range(2)]
        ctrA = [0, 0, 0]

        def consA(uTb, uk, ntl, isc, info):
            n = ntl * 128
            t0 = info
            if not isc:
                dma("sp", cosb[:, :n], cs_all[0][:, t0 - NCTX:t0 - NCTX + n], w=["cs"])
                dma("sp", sinb[:, :n], cs_all[1][:, t0 - NCTX:t0 - NCTX + n], w=["cs"])
            for h in range(2):
                for kc in range(KC):
                    mm(ps[0][:, :n], wA[:, kc, h * 128:(h + 1) * 128], uTb[:, kc, :n], kc == 0, kc == KC - 1,
                       r=[uk, "wA"], w=[("ps", 0)])
                ko = kob[ctrA[0] % 2]; kk = ("kob", ctrA[0] % 2); ctrA[0] += 1
                norm_rope(ps[0], n, 1, not isc, cosb, sinb, ko[:, :n], nrt, [("ps", 0)], [kk])
                dma("sp", kT_s[h][:, t0:t0 + n], ko[:, :n], r=[kk], w=["kT_s"])
            for t in range(ntl):
                pi = 2 + ctrA[1] % 2
                for kc in range(KC):
                    mm(ps[pi][:, 0:256], uTb[:, kc, t * 128:(t + 1) * 128], wA[:, kc, 256:512], kc == 0, kc == KC - 1,
                       r=[uk, "wA"], w=[("ps", pi)])
                v_ = vb[ctrA[1] % 2]; vk = ("vb", ctrA[1] % 2); ctrA[1] += 1
                act(v_, ps[pi][:, 0:256], AF.Copy, r=[("ps", pi)], w=[vk])
                dma("sp", V_s[t0 + t * 128:t0 + (t + 1) * 128, :], v_, r=[vk], w=["V_s"])
            for ct in range(8):
                pi = 4 + ctrA[2] % 2
                for kc in range(KC):
                    mm(ps[pi][:, :n], wA[:, kc, 512 + ct * 128:512 + (ct + 1) * 128], uTb[:, kc, :n], kc == 0, kc == KC - 1,
                       r=[uk, "wA"], w=[("ps", pi)])
                xs = xst[ctrA[2] % 2]; xk = ("xst", ctrA[2] % 2); ctrA[2] += 1
                act(xs[:, :n], ps[pi][:, :n], AF.Copy, r=[("ps", pi)], w=[xk])
                rows = slice(ct * 128, (ct + 1) * 128)
                if isc:
                    dma("sp", xrc_s[rows, 2:2 + n], xs[:, :n], r=[xk], w=["xrc_s"])
                else:
                    tl = t0 - NCTX
                    dma("sp", xr_s[rows, 2 + tl:2 + tl + n], xs[:, :n], r=[xk], w=["xr_s"])

        blocksA = [(ctx, 2, 1, 0)] + [(x_all[b * 512:(b + 1) * 512, :], 4, 0, NCTX + b * 512) for b in range(NB)]
        proj_pass(blocksA, scb, shb, consA)
        settle()

        if stop <= 1:
            P.emit(stack)
            return nc
        A.reset()
        qT = r3(A.bf16(8 * TOK, top=True), 8)
        q_hi = A.hi
        scb = [A.f32(D)]; shb = [A.f32(D)]
        dma("sp", scb[0], mod_s[1], r=[("mod_s", 1)], w=["bcmod"])
        dma("sp", shb[0], mod_s[0], r=[("mod_s", 0)], w=["bcmod"])
        wQ = r3(A.bf16(KC * 2048), KC)
        for kc in range(KC):
            dma("pool", wQ[:, kc, 0:1024], w_in[kc * 128:(kc + 1) * 128, 0:1024], w=["wQ"])
            dma("pool", wQ[:, kc, 1024:2048], w_in[kc * 128:(kc + 1) * 128, 2560:3584], w=["wQ"])
        cosb = A.f32(512); sinb = A.f32(512)
        nrt = [A.f32(512) for _ in range(4)]
        gyb = [A.f32(512) for _ in range(2)]
        ctrQ = [0]

        def consQ(uTb, uk, ntl, isc, info):
            n = ntl * 128
            t0 = info
            dma("sp", cosb[:, :n], cs_own[0][:, t0:t0 + n], w=["cs"])
            dma("sp", sinb[:, :n], cs_own[1][:, t0:t0 + n], w=["cs"])
            for h in range(8):
                for kc in range(KC):
                    mm(ps[0][:, :n], wQ[:, kc, h * 128:(h + 1) * 128], uTb[:, kc, :n], kc == 0, kc == KC - 1,
                       r=[uk, "wQ"], w=[("ps", 0)])
                norm_rope(ps[0], n, 0, True, cosb, sinb, qT[:, h, t0:t0 + n], nrt, [("ps", 0)], ["qT"])
            for ct in range(8):
                pi = 4 + ctrQ[0] % 2
                for kc in range(KC):
                    mm(ps[pi][:, :n], wQ[:, kc, 1024 + ct * 128:1024 + (ct + 1) * 128], uTb[:, kc, :n], kc == 0, kc == KC - 1,
                       r=[uk, "wQ"], w=[("ps", pi)])
                g_ = gyb[ctrQ[0] % 2]; gk = ("gyb", ctrQ[0] % 2); ctrQ[0] += 1
                act(g_[:, :n], ps[pi][:, :n], AF.Gelu, r=[("ps", pi)], w=[gk])
                dma("sp", gy_s[ct * 128:(ct + 1) * 128, t0:t0 + n], g_[:, :n], r=[gk], w=["gy_s"])

        blocksQ = [(x_own[b * 512:(b + 1) * 512, :], 4, 0, b * 512) for b in range(NBO)]
        proj_pass(blocksQ, scb, shb, consQ)
        settle()

        if stop <= 2:
            P.emit(stack)
            return nc
        A.reset(0, q_hi)
        mixT = r3(A.bf16(KC * TOK), KC)
        mix_lo = A.lo
        kTg = A.bf16(SA)
        Vg = r3(A.bf16(NKC * 128), NKC)
        pT = [A.bf16(512) for _ in range(3)]
        rec = A.f32(512)
        it = 0
        for g in range(2):
            dma("sp", kTg, kT_s[g], r=["kT_s"], w=["kTg"])
            Vsrc = V_s[:, g * 128:(g + 1) * 128].rearrange("(n p) d -> p n d", p=128)
            for c0 in range(0, NKC, 4):
                c1 = min(c0 + 4, NKC)
                dma("sp", Vg[:, c0:c1, :], Vsrc[:, c0:c1, :], r=["V_s"], w=["Vg"])
            for h in range(4 * g, 4 * g + 4):
                for qb in range(NBO):
                    qs = slice(qb * 512, (qb + 1) * 512)
                    for kc in range(NKC):
                        si = it % 2; pi = it % 3; it += 1
                        mm(ps[si][:, :], kTg[:, kc * 128:(kc + 1) * 128], qT[:, h, qs], True, True,
                           r=["kTg", "qT"], w=[("ps", si)])
                        act(pT[pi], ps[si][:, :], AF.Exp, scale=float(ATT_SCALE), r=[("ps", si)], w=[("pT", pi)])
                        mm(ps[2][:, :], Vg[:, kc, :], pT[pi], kc == 0, kc == NKC - 1, r=["Vg", ("pT", pi)], w=[("ps", 2)])
                        mm(ps[3][:, :], onesb[:, :], pT[pi], kc == 0, kc == NKC - 1, r=["onesb", ("pT", pi)], w=[("ps", 3)])
                    P.add("dve", lambda e: e.reciprocal(out=rec, in_=ps[3][:, :]), r=[("ps", 3)], w=["rec"])
                    tt("dve", mixT[:, h, qs], ps[2][:, :], rec, ALU.mult, r=[("ps", 2), "rec"], w=["mixT"])
        settle()

        if stop <= 3:
            P.emit(stack)
            return nc
        A.reset(mix_lo, ARENA_F)
        rgw = r3(A.f32(4 * 128), 4)
        rg_w3 = rg_w.rearrange("p (a j) -> p a j", a=32)
        xc = A.f32(NCTX + S)
        xin = [A.f32(520) for _ in range(2)]
        acc = A.f32(TOK)
        gy = A.f32(TOK)
        rt = [A.f32(512) for _ in range(2)]
        itl = [A.f32(512) for _ in range(2)]
        at = [A.f32(512) for _ in range(2)]
        a2 = [A.f32(512) for _ in range(2)]
        bt = [A.f32(512) for _ in range(2)]
        hb = [A.f32(512) for _ in range(3)]
        ci = 0
        gi = 0
        hi_ = 0
        for ct in range(8):
            rows = slice(ct * 128, (ct + 1) * 128)
            dma("sp", gy, gy_s[rows, :], r=["gy_s"], w=["gy"])
            for q4 in range(4):
                dma("sp", rgw[:, q4, :], rg_w3[:, q4 * 8 + ct, :], w=["rgw"])
            P.add("dve", lambda e: e.memset(acc, 0.0), w=["acc"])
            segs = [(xrc_s, 0, NCTX, 0)] + [(xr_s, b * 512, 512, NCTX + b * 512) for b in range(NB)]
            for (srct, c0, n, xo) in segs:
                xi = xin[ci % 2]; xk = ("xin", ci % 2); ci += 1
                dma("sp", xi[:, :n + 3], srct[rows, c0:c0 + n + 3], r=["xr_s", "xrc_s"], w=[xk])
                o = xc[:, xo:xo + n]
                ts("dve", o, xi[:, 0:n], cwT[:, ct * 4:ct * 4 + 1], cbT[:, ct:ct + 1], ALU.mult, ALU.add,
                   r=[xk, kc_], w=["xc"])
                for j in range(1, 4):
                    stt("dve", o, xi[:, j:j + n], cwT[:, ct * 4 + j:ct * 4 + j + 1], o, ALU.mult, ALU.add,
                        r=[xk, "xc", kc_], w=["xc"])
            for d in range(2):
                wi = d * 8 + ct
                order = [(0, NCTX, None)] + [(NCTX + b * 512, 512, b) for b in range(NB)]
                if d == 1:
                    order = [(0, NCTX, None)] + [(NCTX + b * 512, 512, b) for b in reversed(range(NB))]
                prev = None
                for (xo, n, b) in order:
                    gb = gi % 2; gi += 1
                    xs = xc[:, xo:xo + n]
                    mm(ps[gb][:, :n], rgw[:, d, :], xs, True, True, r=["rgw", "xc"], w=[("ps", gb)])
                    mm(ps[2 + gb][:, :n], rgw[:, 2 + d, :], xs, True, True, r=["rgw", "xc"], w=[("ps", 2 + gb)])
                    r_ = rt[gb]; i_ = itl[gb]; a_ = at[gb]; a2_ = a2[gb]; b_ = bt[gb]
                    act(r_[:, :n], ps[gb][:, :n], AF.Sigmoid, bias=rgv[:, wi:wi + 1], r=[("ps", gb), kc_], w=[("rt", gb)])
                    act(i_[:, :n], ps[2 + gb][:, :n], AF.Sigmoid, bias=rgv[:, 16 + wi:16 + wi + 1], r=[("ps", 2 + gb), kc_], w=[("it", gb)])
                    act(a_[:, :n], r_[:, :n], AF.Exp, scale=c8[:, wi:wi + 1], r=[("rt", gb), "c8"], w=[("at", gb)])
                    tt("dve", a2_[:, :n], a_[:, :n], a_[:, :n], ALU.mult, r=[("at", gb)], w=[("a2", gb)])
                    act(a2_[:, :n], a2_[:, :n], AF.Sqrt, scale=-1.0, bias=1.0, r=[("a2", gb)], w=[("a2", gb)])
                    tt("dve", b_[:, :n], a2_[:, :n], i_[:, :n], ALU.mult, r=[("a2", gb), ("it", gb)], w=[("bt", gb)])
                    tt("dve", b_[:, :n], b_[:, :n], xs, ALU.mult, r=[("bt", gb), "xc"], w=[("bt", gb)])
                    h_ = hb[hi_ % 3]; hk = ("hb", hi_ % 3); hi_ += 1
                    init = 0.0 if prev is None else prev[0]
                    rk = [("at", gb), ("bt", gb)] + ([] if prev is None else [prev[1]])
                    if d == 0:
                        P.add("dve", lambda e, h_=h_, a_=a_, b_=b_, n=n, init=init: e.tensor_tensor_scan(
                            out=h_[:, :n], data0=a_[:, :n], data1=b_[:, :n], initial=init, op0=ALU.mult, op1=ALU.add),
                            r=rk, w=[hk])
                        prev = (h_[:, n - 1:n], hk)
                    else:
                        P.add("dve", lambda e, h_=h_, a_=a_, b_=b_, n=n, init=init: e.tensor_tensor_scan(
                            out=h_[:, n - 1::-1] if False else h_[:, 0:n][:, ::-1], data0=a_[:, 0:n][:, ::-1], data1=b_[:, 0:n][:, ::-1],
                            initial=init, op0=ALU.mult, op1=ALU.add), r=rk, w=[hk])
                        prev = (h_[:, 0:1], hk)
                    if b is not None:
                        c = b // NBO
                        po = (b % NBO) * 512
                        stt("dve", acc[:, po:po + 512], h_[:, :n], sel[:, c:c + 1], acc[:, po:po + 512], ALU.mult, ALU.add,
                            r=[hk, "acc", kc_], w=["acc"])
            tt("dve", mixT[:, 8 + ct, :], acc, gy, ALU.mult, r=["acc", "gy"], w=["mixT"])
        for c0 in range(0, KC, 4):
            dma("sp", r3(mix_s, KC)[:, c0:c0 + 4, :], mixT[:, c0:c0 + 4, :], r=["mixT"], w=["mix_s"])
        settle()

        if stop <= 4:
            P.emit(stack)
            return nc
        A.reset()
        mtb = [r3(A.bf16(KC * 128), KC) for _ in range(2)]
        wo = r3(A.bf16(KC * D), KC)
        for kc in range(KC):
            dma("pool", wo[:, kc, :], w_out[kc * 128:(kc + 1) * 128, :], w=["wo"])
        g1b = A.f32(D); l1g = A.f32(D); l1b = A.f32(D); s2b = A.f32(D); h2b = A.f32(D)
        dma("sp", g1b, mod_s[4], r=[("mod_s", 4)], w=["bco"])
        dma("sp", l1g, ln_bc[0], w=["bco"])
        dma("sp", l1b, ln_bc[1], w=["bco"])
        dma("sp", s2b, mod_s[6], r=[("mod_s", 6)], w=["bco"])
        dma("sp", h2b, mod_s[5], r=[("mod_s", 5)], w=["bco"])
        wr = r3(A.f32(KC * NE), KC)
        wrs = w_router.rearrange("(a p) n -> p a n", p=128)
        for c0 in range(0, KC, 4):
            dma("sp", wr[:, c0:c0 + 4, :], wrs[:, c0:c0 + 4, :], w=["wr"])
        xo_ = A.f32(D); zt = A.f32(D); h1t = A.f32(D)
        vTb = r3(A.bf16(KC * 128), KC); vT32 = r3(A.f32(KC * 128), KC)
        stats = A.f32(24); mv = A.f32(2); rs = A.f32(1)
        sc_ = A.f32(NE); bi_ = A.f32(NE); m8 = A.f32(64); gs = A.f32(8); g8 = A.f32(8); pen = A.f32(8)
        mk_ = A.f32(NE); t8 = A.f32(8); wsel = A.f32(NE); wsum = A.f32(1)
        Wr3 = r3(Wr[:, :], NTO)

        def layer_norm(z, key, stats, mv, rs):
            for c4 in range(4):
                P.add("dve", lambda e, c4=c4: e.bn_stats(out=stats[:, c4 * 6:(c4 + 1) * 6], in_=z[:, c4 * 512:(c4 + 1) * 512]),
                      r=[key], w=["stats"])
            P.add("dve", lambda e: e.bn_aggr(out=mv, in_=stats), r=["stats"], w=["mv"])
            act(rs, mv[:, 1:2], AF.Sqrt, bias=float(EPS), r=["mv"], w=["rs"])
            P.add("dve", lambda e: e.reciprocal(out=rs, in_=rs), r=["rs"], w=["rs"])
            ts("dve", z, z, mv[:, 0:1], rs[:, 0:1], ALU.subtract, ALU.mult, r=[key, "mv", "rs"], w=[key])

        for t in range(NTO):
            tsl = slice(t * 128, (t + 1) * 128)
            dma("sp", xo_, x_own[tsl, :], w=["xo"])
            mt = mtb[t % 2]; mtk = ("mt", t % 2)
            for c0 in range(0, KC, 8):
                dma("sp", mt[:, c0:c0 + 8, :], r3(mix_s, KC)[:, c0:c0 + 8, tsl], r=["mix_s"], w=[mtk])
            for fb in range(4):
                pi = fb
                for kc in range(KC):
                    mm(ps[pi][:, :], mt[:, kc, :], wo[:, kc, fb * 512:(fb + 1) * 512], kc == 0, kc == KC - 1,
                       r=[mtk, "wo"], w=[("ps", pi)])
                tt("dve", zt[:, fb * 512:(fb + 1) * 512], ps[pi][:, :], g1b[:, fb * 512:(fb + 1) * 512], ALU.mult,
                   r=[("ps", pi), "bco"], w=["zt"])
            stt("dve", zt, xo_, float(ALPHA), zt, ALU.mult, ALU.add, r=["xo", "zt"], w=["zt"])
            layer_norm(zt, "zt", stats, mv, rs)
            tt("dve", h1t, zt, l1g, ALU.mult, r=["zt", "bco"], w=["h1t"])
            tt("dve", h1t, h1t, l1b, ALU.add, r=["h1t", "bco"], w=["h1t"])
            dma("sp", h1_s[tsl, :], h1t, r=["h1t"], w=["h1_s"])
            if osub >= 2:
                tt("dve", zt, h1t, s2b, ALU.mult, r=["h1t", "bco"], w=["zt"])
                tt("dve", zt, zt, h2b, ALU.add, r=["zt", "bco"], w=["zt"])
                for q4 in range(4):
                    pi = q4
                    for c in range(4):
                        kc = q4 * 4 + c
                        mm(ps[pi][:, c * 128:(c + 1) * 128], zt[:, kc * 128:(kc + 1) * 128], identf[:, :], True, True, r=["zt", kc_], w=[("ps", pi)])
                    cp("dve", vT32[:, q4 * 4:(q4 + 1) * 4, :], r3(ps[pi][:, :], 4), r=[("ps", pi)], w=["vT32"])
                    act(vTb[:, q4 * 4:(q4 + 1) * 4, :], vT32[:, q4 * 4:(q4 + 1) * 4, :], AF.Copy, r=["vT32"], w=["vTb"])
                for c0 in range(0, KC, 8):
                    dma("sp", r3(vT_s, KC)[:, c0:c0 + 8, tsl], vTb[:, c0:c0 + 8, :], r=["vTb"], w=["vT_s"])
            if osub >= 3:
                for kc in range(KC):
                    mm(ps[4][:, 0:NE], vT32[:, kc, :], wr[:, kc, :], kc == 0, kc == KC - 1, r=["vT32", "wr"], w=[("ps", 4)])
                act(sc_, ps[4][:, 0:NE], AF.Sigmoid, r=[("ps", 4)], w=["sc"])
            if osub >= 4:
                tt("dve", bi_, sc_, ebb[:, :], ALU.add, r=["sc", kc_], w=["bi"])
                for g in range(8):
                    P.add("dve", lambda e, g=g: e.max(out=m8[:, g * 8:(g + 1) * 8], in_=bi_[:, g * 8:(g + 1) * 8]), r=["bi"], w=["m8"])
                m83 = r3(m8, 8)
                tt("dve", gs.rearrange("p (a b) -> p a b", b=1), m83[:, :, 0:1], m83[:, :, 1:2], ALU.add, r=["m8"], w=["gs"])
                P.add("dve", lambda e: e.max(out=g8, in_=gs), r=["gs"], w=["g8"])
                ts("dve", pen, gs, g8[:, 3:4], None, ALU.is_ge, r=["gs", "g8"], w=["pen"])
                ts("dve", pen, pen, -1.0, 1.0e9, ALU.add, ALU.mult, r=["pen"], w=["pen"])
                for g in range(8):
                    ts("dve", mk_[:, g * 8:(g + 1) * 8], bi_[:, g * 8:(g + 1) * 8], pen[:, g:g + 1], None, ALU.add,
                       r=["bi", "pen"], w=["mk"])
                P.add("dve", lambda e: e.max(out=t8, in_=mk_), r=["mk"], w=["t8"])
                ts("dve", wsel, mk_, t8[:, 7:8], None, ALU.is_ge, r=["mk", "t8"], w=["wsel"])
                tt("dve", wsel, wsel, sc_, ALU.mult, r=["wsel", "sc"], w=["wsel"])
                P.add("dve", lambda e: e.reduce_sum(out=wsum, in_=wsel, axis=AX.X), r=["wsel"], w=["wsum"])
                P.add("dve", lambda e: e.reciprocal(out=wsum, in_=wsum), r=["wsum"], w=["wsum"])
                ts("dve", Wr3[:, t, 0:NE], wsel, wsum[:, 0:1], 2.5, ALU.mult, ALU.mult, r=["wsel", "wsum"], w=["Wr"])
                ts("dve", Wr3[:, t, NE:NE + 1], onesf[:, 0:1], 1.0, None, ALU.mult, r=[kc_], w=["Wr"])
        if debug:
            wr_dbg = nc.dram_tensor("wr_dbg", [128, NTO * (NE + 1)], F32, kind="ExternalOutput").ap()
            dma("sp", wr_dbg, Wr[:, :], r=["Wr"], w=["wr_dbg"])
        settle()

        if stop <= 5:
            P.emit(stack)
            return nc
        NTH = TH // 128
        for hf in range(NH):
            A.reset()
            vTh = r3(A.bf16(KC * TH), KC)
            for c0 in range(0, KC, 4):
                dma("sp", vTh[:, c0:c0 + 4, :], r3(vT_s, KC)[:, c0:c0 + 4, hf * TH:(hf + 1) * TH], r=["vT_s"], w=["vTh"])
            if debug and hf == 0:
                vth0_dbg = nc.dram_tensor("vth0_dbg", [128, KC * TH], BF16, kind="ExternalOutput").ap()
                dma("sp", vth0_dbg, vTh.rearrange("p a b -> p (a b)"), r=["vTh"], w=["vth0_dbg"])
            accm = r3(A.f32(NTH * D), NTH)
            gT = r3(A.bf16(4 * TH), 4)
            s1 = [A.bf16(512) for _ in range(2)]
            w_lo = A.lo
            w1 = [r3(A.bf16(KC * FF), KC) for _ in range(2)]
            w3 = [r3(A.bf16(KC * FF), KC) for _ in range(2)]
            w2 = [r3(A.bf16(4 * D), 4)] * 2
            pc = 0
            elist = [nexp] + list(range(nexp))
            for ei, e_ in enumerate(elist):
                wb = ei % 2
                for half in range(4):
                    ks = slice(half * 4, (half + 1) * 4)
                    dma("pool", w1[wb][:, ks, :], w_e1[e_, half * 512:(half + 1) * 512, :].rearrange("(a p) n -> p a n", p=128), w=[("w1", wb)])
                    dma("pool", w3[wb][:, ks, :], w_e3[e_, half * 512:(half + 1) * 512, :].rearrange("(a p) n -> p a n", p=128), w=[("w3", wb)])
                dma("pool", w2[wb], w_e2[e_].rearrange("(a p) n -> p a n", p=128), w=[("w2", 0)])
                for tb in range(TH // 512):
                    tbs = slice(tb * 512, (tb + 1) * 512)
                    for fc in range(4):
                        p1 = pc % 2; p3 = 2 + pc % 2; pc += 1
                        for kc in range(KC):
                            mm(ps[p1][:, :], w1[wb][:, kc, fc * 128:(fc + 1) * 128], vTh[:, kc, tbs], kc == 0, kc == KC - 1,
                               r=[("w1", wb), "vTh"], w=[("ps", p1)])
                        for kc in range(KC):
                            mm(ps[p3][:, :], w3[wb][:, kc, fc * 128:(fc + 1) * 128], vTh[:, kc, tbs], kc == 0, kc == KC - 1,
                               r=[("w3", wb), "vTh"], w=[("ps", p3)])
                        act(s1[p1], ps[p1][:, :], AF.Silu, r=[("ps", p1)], w=[("s1", p1)])
                        tt("dve", gT[:, fc, tbs], s1[p1], ps[p3][:, :], ALU.mult, r=[("s1", p1), ("ps", p3)], w=["gT"])
                for t in range(NTH):
                    tg = hf * NTH + t
                    for fb in range(4):
                        py = 4 + pc % 2; pc += 1
                        for fc in range(4):
                            mm(ps[py][:, :], gT[:, fc, t * 128:(t + 1) * 128], w2[wb][:, fc, fb * 512:(fb + 1) * 512], fc == 0, fc == 3,
                               r=["gT", ("w2", 0)], w=[("ps", py)])
                        o = accm[:, t, fb * 512:(fb + 1) * 512]
                        if ei == 0:
                            cp("dve", o, ps[py][:, :], r=[("ps", py)], w=["accm"])
                        else:
                            stt("dve", o, ps[py][:, :], Wr3[:, tg, e_:e_ + 1], o, ALU.mult, ALU.add,
                                r=[("ps", py), "accm", "Wr"], w=["accm"])
            if debug and hf == 0:
                for nm, src_, n_, ky in (("gT_dbg", gT.rearrange("p a b -> p (a b)"), 4 * TH, "gT"),
                                         ("w1a_dbg", w1[0].rearrange("p a b -> p (a b)"), KC * FF, ("w1", 0)),
                                         ("w1b_dbg", w1[1].rearrange("p a b -> p (a b)"), KC * FF, ("w1", 1)),
                                         ("w3a_dbg", w3[0].rearrange("p a b -> p (a b)"), KC * FF, ("w3", 0)),
                                         ("w3b_dbg", w3[1].rearrange("p a b -> p (a b)"), KC * FF, ("w3", 1)),
                                         ("w2_dbg", w2[0].rearrange("p a b -> p (a b)"), 4 * D, ("w2", 0))):
                    dd = nc.dram_tensor(nm, [128, n_], BF16, kind="ExternalOutput").ap()
                    dma("sp", dd, src_, r=[ky], w=[nm])
                vth_dbg = nc.dram_tensor("vth_dbg", [128, KC * TH], BF16, kind="ExternalOutput").ap()
                dma("sp", vth_dbg, vTh.rearrange("p a b -> p (a b)"), r=["vTh"], w=["vth_dbg"])
                acc_dbg = nc.dram_tensor("acc_dbg", [128, NTH * D], F32, kind="ExternalOutput").ap()
                dma("sp", acc_dbg, accm.rearrange("p a b -> p (a b)"), r=["accm"], w=["acc_dbg"])
            settle()
            A.reset(w_lo, ARENA_F)
            g2b = A.f32(D); l2g = A.f32(D); l2b = A.f32(D); h1t = A.f32(D); ot = [A.f32(D) for _ in range(2)]
            stats = A.f32(24); mv = A.f32(2); rs = A.f32(1)
            dma("sp", g2b, mod_s[7], r=[("mod_s", 7)], w=["bcf"])
            dma("sp", l2g, ln_bc[2], w=["bcf"])
            dma("sp", l2b, ln_bc[3], w=["bcf"])
            for t in range(NTH):
                tsl = slice(hf * TH + t * 128, hf * TH + (t + 1) * 128)
                o_ = ot[t % 2]; ok = ("ot", t % 2)
                dma("sp", h1t, h1_s[tsl, :], r=["h1_s"], w=["h1t"])
                tt("dve", o_, accm[:, t, :], g2b, ALU.mult, r=["accm", "bcf"], w=[ok])
                stt("dve", o_, h1t, float(ALPHA), o_, ALU.mult, ALU.add, r=["h1t", ok], w=[ok])
                layer_norm(o_, ok, stats, mv, rs)
                tt("dve", o_, o_, l2g, ALU.mult, r=[ok, "bcf"], w=[ok])
                tt("dve", o_, o_, l2b, ALU.add, r=[ok, "bcf"], w=[ok])
                dma("sp", out[tsl, :], o_, r=[ok], w=["out"])
            settle()

        P.emit(stack)
    return nc


def _prep(inputs, S, nexp=NE, cores=range(8)):
    TOK = S // 8
    f = lambda a: np.ascontiguousarray(np.asarray(a, dtype=np.float32))
    x = f(inputs["x"])[0]; c = f(inputs["c"])[0]; ctx = f(inputs["ctx"])[0]; cc = f(inputs["c_ctx"])
    cT = np.zeros((128, 32), np.float32)
    cT[:, 0::2] = c.reshape(16, 128).T
    cT[:, 1::2] = cc.reshape(16, 128).T
    bc = lambda v: np.ascontiguousarray(np.broadcast_to(f(v).reshape(1, -1), (128, f(v).size)))
    conv_w = f(inputs["conv_w"])[0]
    conv_wT = np.ascontiguousarray(conv_w.reshape(4, 8, 128).transpose(2, 1, 0).reshape(128, 32))
    conv_bT = np.ascontiguousarray(f(inputs["conv_b"])[0].reshape(8, 128).T)
    wa = f(inputs["rg_wa"])[0]; wx = f(inputs["rg_wx"])[0]
    rg_w = np.concatenate([wa.reshape(16, 128, 128), wx.reshape(16, 128, 128)], 0)
    rg_w = np.ascontiguousarray(rg_w.transpose(1, 0, 2).reshape(128, 32 * 128))
    vecT = lambda v: f(v)[0].reshape(16, 128).T
    rg_vec = np.ascontiguousarray(np.concatenate([vecT(inputs["rg_ba"]), vecT(inputs["rg_bx"]), vecT(inputs["rg_lam"])], 1))
    ln_bc = np.stack([bc(inputs["ln1_g"]), bc(inputs["ln1_b"]), bc(inputs["ln2_g"]), bc(inputs["ln2_b"])], 0)
    qk_g = np.ascontiguousarray(np.stack([f(inputs["q_norm"])[0], f(inputs["k_norm"])[0]], 1))
    half = 32
    inv_freq = (np.float32(10000.0) ** (-np.arange(half, dtype=np.float32) / np.float32(half))).astype(np.float32)
    t = np.arange(S)
    row = (t // 64).astype(np.float32); col = (t % 64).astype(np.float32)
    ang = np.zeros((128, S), np.float32)
    for d in range(128):
        pos = row if d < 64 else col
        ang[d] = pos * inv_freq[(d % 64) % half]
    cs_all = np.stack([np.cos(ang), np.sin(ang)], 0).astype(np.float32)
    Rm = np.zeros((128, 128), np.float32)
    for base in (0, 64):
        for i in range(32):
            Rm[base + i, base + i + 32] = -1.0
            Rm[base + 32 + i, base + i] = 1.0
    consts = np.stack([np.eye(128, dtype=np.float32), np.ones((128, 128), np.float32),
                       np.ascontiguousarray(Rm.T), np.zeros((128, 128), np.float32)], 0)
    w_e1 = np.concatenate([f(inputs["w_e1"])[0][:nexp], f(inputs["w_s1"])], 0)
    w_e3 = np.concatenate([f(inputs["w_e3"])[0][:nexp], f(inputs["w_s3"])], 0)
    w_e2 = np.concatenate([f(inputs["w_e2"])[0][:nexp], f(inputs["w_s2"])], 0)
    common = dict(
        x_all=x, ctx=ctx, cT=cT, w_mod=f(inputs["w_mod"])[0], b_mod_bc=bc(inputs["b_mod"]),
        w_in=f(inputs["w_in"])[0], qk_g=qk_g, conv_wT=conv_wT, conv_bT=conv_bT, rg_w=rg_w, rg_vec=rg_vec,
        w_out=f(inputs["w_out"])[0], ln_bc=ln_bc, w_router=f(inputs["w_router"])[0], eb_bc=bc(inputs["e_bias"]),
        w_e1=w_e1, w_e3=w_e3, w_e2=w_e2, cs_all=cs_all, consts=consts)
    maps = []
    for j in cores:
        m = dict(common)
        m["x_own"] = np.ascontiguousarray(x[j * TOK:(j + 1) * TOK])
        m["cs_own"] = np.ascontiguousarray(cs_all[:, :, j * TOK:(j + 1) * TOK])
        s = np.zeros((128, 8), np.float32); s[:, j] = 1.0
        m["sel"] = s
        maps.append(m)
    return maps


_NC_CACHE = {}


def kernel(**inputs):
    S = int(np.asarray(inputs["x"]).shape[1])
    if S not in _NC_CACHE:
        _NC_CACHE[S] = build(S)
    nc = _NC_CACHE[S]
    maps = _prep(inputs, S)
    res = run_bass_kernel_spmd(nc, maps, core_ids=list(range(8)))
    outs = [np.asarray(r["out"], dtype=np.float32) for r in res.results]
    return np.concatenate(outs, 0)[None, :, :]
```

```python
import numpy as np
import concourse.bass as bass
import concourse.mybir as mybir
from concourse.bass_utils import run_bass_kernel_spmd

F32 = mybir.dt.float32
BF16 = mybir.dt.bfloat16
ALU = mybir.AluOpType
AF = mybir.ActivationFunctionType
AX = mybir.AxisListType

D = 2048
KC = 16
NCTX = 256
NE = 64
FF = 512
ALPHA = 2.0 ** 0.25
EPS = 1e-6
ATT_SCALE = 128.0 ** -0.5
ARENA_F = 50176

TRACE_LOG = None
ENGS = ("pe", "act", "dve", "pool", "sp")
EPOCH = 12000
DPOOL = 8
QDEPTH = {"pool": 1}
QSLOTS = {"pool": 4}


class Prog:
    def __init__(self, nc):
        self.nc = nc
        self.ops = []
        self.lastw = {}
        self.rd_c = {}
        self.rd_d = {}
        self.barrier = set()
        self.eng_ops = {e: [] for e in ENGS}
        self.ncomp = {e: 0 for e in ENGS}
        self.ndma = {e: 0 for e in ENGS}
        self.dma_ids = {e: [] for e in ENGS}

    def add(self, eng, fn, r=(), w=(), dma=False):
        oid = len(self.ops)
        deps = set(self.barrier)
        for k in r:
            if k in self.lastw:
                deps.add(self.lastw[k])
        for k in w:
            if k in self.lastw:
                deps.add(self.lastw[k])
            deps.update(self.rd_c.get(k, {}).values())
            deps.update(self.rd_d.get(k, ()))
        for k in r:
            if dma:
                self.rd_d.setdefault(k, []).append(oid)
            else:
                self.rd_c.setdefault(k, {})[eng] = oid
        for k in w:
            self.lastw[k] = oid
            self.rd_c[k] = {}
            self.rd_d[k] = []
        op = dict(eng=eng, fn=fn, deps=deps, dma=dma, tag=(tuple(r), tuple(w)))
        if dma:
            i = self.ndma[eng]
            self.ndma[eng] += 1
            dp = QDEPTH.get(eng, DPOOL)
            ns = QSLOTS.get(eng, DPOOL)
            op["slot"] = i % ns
            op["target"] = 16 * (i // ns + 1)
            if i >= dp:
                deps.add(self.dma_ids[eng][i - dp])
            self.dma_ids[eng].append(oid)
        else:
            n = self.ncomp[eng]
            self.ncomp[eng] += 1
            op["epoch"] = n // EPOCH
            op["idx"] = n % EPOCH + 1
        self.ops.append(op)
        self.eng_ops[eng].append(oid)
        return oid

    def fence(self):
        b = set()
        for e in ENGS:
            if self.eng_ops[e]:
                comp = [o for o in self.eng_ops[e] if not self.ops[o]["dma"]]
                if comp:
                    b.add(comp[-1])
            b.update(self.dma_ids[e][-DPOOL:])
        self.barrier = b

    def emit(self, stack):
        nc = self.nc
        csem = {}
        dsem = {}
        for e in ENGS:
            for ep in range(self.ncomp[e] // EPOCH + 1):
                csem[(e, ep)] = stack.enter_context(nc.semaphore(f"c_{e}_{ep}"))
            if self.ndma[e]:
                for s in range(DPOOL):
                    dsem[(e, s)] = stack.enter_context(nc.semaphore(f"d_{e}_{s}"))
        block = stack.enter_context(nc.Block())
        ops = self.ops

        def run(engname, eng):
            waited = {}
            for oid in self.eng_ops[engname]:
                op = ops[oid]
                need = {}
                for d in op["deps"]:
                    p = ops[d]
                    if p["dma"]:
                        key = ("d", p["eng"], p["slot"])
                        val = p["target"]
                    else:
                        if p["eng"] == engname and engname == "pe":
                            continue
                        key = ("c", p["eng"], p["epoch"])
                        val = p["idx"]
                    if need.get(key, 0) < val:
                        need[key] = val
                for key, val in need.items():
                    if waited.get(key, 0) >= val:
                        continue
                    waited[key] = val
                    sem = dsem[(key[1], key[2])] if key[0] == "d" else csem[(key[1], key[2])]
                    eng.wait_ge(sem, val)
                    if TRACE_LOG is not None:
                        TRACE_LOG.append((engname, oid, "wait", key, val))
                if TRACE_LOG is not None:
                    TRACE_LOG.append((engname, oid, "op", op.get("tag"), (op.get("slot"), op.get("target")) if op["dma"] else (op["epoch"], op["idx"])))
                inst = op["fn"](eng)
                if op["dma"]:
                    inst.then_inc(dsem[(engname, op["slot"])], 16)
                else:
                    inst.then_inc(csem[(engname, op["epoch"])], 1)
            for oid in self.dma_ids[engname][-DPOOL:]:
                op = ops[oid]
                key = ("d", engname, op["slot"])
                if waited.get(key, 0) < op["target"]:
                    waited[key] = op["target"]
                    eng.wait_ge(dsem[(engname, op["slot"])], op["target"])

        @block.tensor
        def _(e):
            run("pe", e)

        @block.scalar
        def _(e):
            run("act", e)

        @block.vector
        def _(e):
            run("dve", e)

        @block.gpsimd
        def _(e):
            run("pool", e)

        @block.sync
        def _(e):
            run("sp", e)


class Arena:
    def __init__(self, ap):
        self.ap = ap
        self.lo = 0
        self.hi = ARENA_F

    def reset(self, lo=0, hi=ARENA_F):
        self.lo = lo
        self.hi = hi

    def f32(self, n0, top=False):
        n = (n0 + 7) // 8 * 8
        if top:
            self.hi -= n
            off = self.hi
        else:
            off = self.lo
            self.lo += n
        assert self.lo <= self.hi, ("arena overflow", self.lo, self.hi)
        return self.ap[:, off:off + n0]

    def bf16(self, n, top=False):
        nf = (n + 1) // 2
        a = self.f32(nf, top=top)
        return a.bitcast(BF16)[:, 0:n]


def r3(ap, a):
    return ap.rearrange("p (a b) -> p a b", a=a)


def build(S, stop=99, nexp=NE, debug=False, osub=9):
    TOK = S // 8
    NB = S // 512
    NBO = TOK // 512
    NTO = TOK // 128
    SA = NCTX + S
    NKC = SA // 128
    TH = min(TOK, 1024)
    NH = TOK // TH
    nc = bass.Bass("TRN2", target_bir_lowering=False)

    def din(name, shape, dt=F32):
        return nc.dram_tensor(name, list(shape), dt, kind="ExternalInput").ap()

    x_all = din("x_all", [S, D]); x_own = din("x_own", [TOK, D]); ctx = din("ctx", [NCTX, D])
    cT = din("cT", [128, 32])
    w_mod = din("w_mod", [D, 6 * D]); b_mod_bc = din("b_mod_bc", [128, 6 * D])
    w_in = din("w_in", [D, 3584])
    qk_g = din("qk_g", [128, 2])
    conv_wT = din("conv_wT", [128, 32]); conv_bT = din("conv_bT", [128, 8])
    rg_w = din("rg_w", [128, 32 * 128])
    rg_vec = din("rg_vec", [128, 48])
    w_out = din("w_out", [D, D])
    ln_bc = din("ln_bc", [4, 128, D])
    w_router = din("w_router", [D, NE]); eb_bc = din("eb_bc", [128, NE])
    w_e1 = din("w_e1", [nexp + 1, D, FF]); w_e3 = din("w_e3", [nexp + 1, D, FF]); w_e2 = din("w_e2", [nexp + 1, FF, D])
    cs_all = din("cs_all", [2, 128, S]); cs_own = din("cs_own", [2, 128, TOK])
    consts = din("consts", [4, 128, 128])
    sel_in = din("sel", [128, 8])
    out = nc.dram_tensor("out", [TOK, D], F32, kind="ExternalOutput").ap()

    skind = "ExternalOutput" if debug else "Internal"

    def dscr(name, shape, dt):
        return nc.dram_tensor(name, list(shape), dt, kind=skind).ap()

    kT_s = dscr("kT_s", [2, 128, SA], BF16)
    V_s = dscr("V_s", [SA, 256], BF16)
    XP = S + 8
    xr_s = dscr("xr_s", [1024, XP], F32)
    xrc_s = dscr("xrc_s", [1024, NCTX + 8], F32)
    gy_s = dscr("gy_s", [1024, TOK], F32)
    h1_s = dscr("h1_s", [TOK, D], F32)
    vT_s = dscr("vT_s", [128, KC * TOK], BF16)
    mod_s = dscr("mod_s", [8, 128, D], F32)
    mix_s = dscr("mix_s", [128, KC * TOK], BF16)

    import contextlib
    stack = contextlib.ExitStack()
    with stack:
        def sb(name, shape, dt=F32):
            return stack.enter_context(nc.sbuf_tensor("sb_" + name, list(shape), dt))

        FA = sb("arena", [128, ARENA_F])
        identf = sb("identf", [128, 128]); onesf = sb("onesf", [128, 128]); RTf = sb("RTf", [128, 128])
        zerof = sb("zerof", [128, 128])
        identb = sb("identb", [128, 128], BF16); onesb = sb("onesb", [128, 128], BF16)
        sT = sb("sT", [128, 32]); csT = sb("csT", [128, 32])
        qkg = sb("qkg", [128, 2]); qkg2 = sb("qkg2", [128, 2])
        cwT = sb("cwT", [128, 32]); cbT = sb("cbT", [128, 8])
        rgv = sb("rgv", [128, 48]); c8 = sb("c8", [128, 16]); c8t = sb("c8t", [128, 16])
        sel = sb("sel", [128, 8]); ebb = sb("ebb", [128, NE])
        Wr = sb("Wr", [128, NTO * (NE + 1)])
        ps = [stack.enter_context(nc.psum_tensor(f"ps{i}", [128, 512], F32)) for i in range(6)]
        ptb = [stack.enter_context(nc.psum_tensor(f"ptb{i}", [128, 1024], BF16)) for i in range(2)]

        P = Prog(nc)
        A = Arena(FA)
        uid = [0]

        def K(name):
            uid[0] += 1
            return (name, uid[0])

        def dma(q, out_ap, in_ap, r=(), w=(), **kw):
            P.add(q, lambda e: e.dma_start(out=out_ap, in_=in_ap, **kw), r=r, w=w, dma=True)

        def mm(o, lhsT, rhs, start, stop, r=(), w=()):
            P.add("pe", lambda e: e.matmul(o, lhsT, rhs, start=start, stop=stop), r=r, w=w)

        def tr(o, in_, ident, r=(), w=()):
            P.add("pe", lambda e: e.transpose(o, in_, ident), r=r, w=w)

        def act(o, in_, func, r=(), w=(), **kw):
            P.add("act", lambda e: e.activation(out=o, in_=in_, func=func, **kw), r=r, w=w)

        def tt(eng, o, a, b, op, r=(), w=()):
            P.add(eng, lambda e: e.tensor_tensor(out=o, in0=a, in1=b, op=op), r=r, w=w)

        def ts(eng, o, a, s1, s2, op0, op1=None, r=(), w=()):
            if op1 is None:
                P.add(eng, lambda e: e.tensor_scalar(out=o, in0=a, scalar1=s1, scalar2=None, op0=op0), r=r, w=w)
            else:
                P.add(eng, lambda e: e.tensor_scalar(out=o, in0=a, scalar1=s1, scalar2=s2, op0=op0, op1=op1), r=r, w=w)

        def stt(eng, o, a, s, b, op0, op1, r=(), w=()):
            P.add(eng, lambda e: e.scalar_tensor_tensor(out=o, in0=a, scalar=s, in1=b, op0=op0, op1=op1), r=r, w=w)

        def cp(eng, o, a, r=(), w=()):
            P.add(eng, lambda e: e.tensor_copy(out=o, in_=a), r=r, w=w)

        dly_t = sb("dly_t", [128, 512])

        def settle():
            P.fence()
            for _i in range(32):
                dma("sp", dly_t[:, :], x_own[0:128, 0:512], w=["dly_t"])
            P.fence()

        kc_ = "const"
        for i, t in enumerate((identf, onesf, RTf, zerof)):
            dma("sp", t[:, :], consts[i], w=[kc_])
        for src, dst in ((cT, csT), (qk_g, qkg), (conv_wT, cwT), (conv_bT, cbT), (rg_vec, rgv),
                         (sel_in, sel), (eb_bc, ebb)):
            dma("sp", dst[:, :], src, w=[kc_])
        cp("dve", identb[:, :], identf[:, :], r=[kc_], w=["identb"])
        cp("dve", onesb[:, :], onesf[:, :], r=[kc_], w=["onesb"])
        ts("dve", qkg2[:, :], qkg[:, :], float(128.0 ** 0.5), None, ALU.mult, r=[kc_], w=["qkg2"])
        act(sT[:, :], csT[:, :], AF.Silu, r=[kc_], w=["sT"])
        act(c8t[:, :], rgv[:, 32:48], AF.Exp, scale=-1.0, r=[kc_], w=["c8t"])
        act(c8[:, :], c8t[:, :], AF.Ln, bias=1.0, r=["c8t"], w=["c8"])
        ts("dve", c8[:, :], c8[:, :], -8.0, None, ALU.mult, r=["c8"], w=["c8"])
        for rr in range(8):
            rows = slice(rr * 128, (rr + 1) * 128)
            dma("sp", xr_s[rows, 0:2], zerof[:, 0:2], r=[kc_], w=["xr_s"])
            dma("sp", xr_s[rows, S + 2:S + 8], zerof[:, 0:6], r=[kc_], w=["xr_s"])
            dma("sp", xrc_s[rows, 0:2], zerof[:, 0:2], r=[kc_], w=["xrc_s"])
            dma("sp", xrc_s[rows, NCTX + 2:NCTX + 8], zerof[:, 0:6], r=[kc_], w=["xrc_s"])

        A.reset()
        sbc = A.f32(KC * 2 * 128)
        sbc4 = sbc.rearrange("p (a v m) -> p a v m", a=KC, v=2)
        for kc in range(KC):
            for v in range(2):
                ts("dve", sbc4[:, kc, v, :], onesf[:, :], sT[:, 2 * kc + v:2 * kc + v + 1], None, ALU.mult,
                   r=["sT", kc_], w=["sbc"])
        wm = [r3(A.f32(KC * 512), KC) for _ in range(2)]
        bmb = [A.f32(512) for _ in range(2)]
        mo = [A.f32(512) for _ in range(2)]
        MODV = {(0, 0): 0, (1, 0): 1, (0, 1): 2, (1, 1): 3, (2, 0): 4, (3, 0): 5, (4, 0): 6, (5, 0): 7}
        cnt = 0
        for nb in range(24):
            sec = nb // 4
            buf = nb % 2
            col = slice(nb * 512, (nb + 1) * 512)
            wsrc = w_mod[:, col].rearrange("(a p) n -> p a n", p=128)
            for c0 in range(0, KC, 4):
                dma("sp", wm[buf][:, c0:c0 + 4, :], wsrc[:, c0:c0 + 4, :], w=[("wm", buf)])
            dma("sp", bmb[buf], b_mod_bc[:, col], w=[("bmb", buf)])
            for v in ((0, 1) if sec < 2 else (0,)):
                pt = ps[cnt % 2]; pk = ("ps", cnt % 2)
                for kc in range(KC):
                    mm(pt[:, :], sbc4[:, kc, v, :], wm[buf][:, kc, :], kc == 0, kc == KC - 1,
                       r=[("wm", buf), "sbc"], w=[pk])
                mb = mo[cnt % 2]; mk = ("mo", cnt % 2)
                tt("dve", mb, pt[:, :], bmb[buf], ALU.add, r=[pk, ("bmb", buf)], w=[mk])
                if sec in (1, 4):
                    ts("dve", mb, mb, 1.0, None, ALU.add, r=[mk], w=[mk])
                idx = MODV[(sec, v)]
                dma("sp", mod_s[idx][:, (nb % 4) * 512:(nb % 4 + 1) * 512], mb, r=[mk], w=[("mod_s", idx)])
                cnt += 1
        settle()

        def norm_rope(praw, n, gcol, lat, cosb, sinb, kout, tmp, rkeys, wkeys):
            sq, t0, kn, t1 = tmp
            act(sq[:, :n], praw[:, :n], AF.Square, r=rkeys, w=["nr_sq"])
            mm(ps[1][:, :n], onesf[:, :], sq[:, :n], True, True, r=["nr_sq", kc_], w=[("ps", 1)])
            act(t0[:, :n], ps[1][:, :n], AF.Sqrt, bias=float(128 * EPS), r=[("ps", 1)], w=["nr_t0"])
            P.add("dve", lambda e: e.reciprocal(out=t0[:, :n], in_=t0[:, :n]), r=["nr_t0"], w=["nr_t0"])
            stt("dve", kn[:, :n], praw[:, :n], qkg2[:, gcol:gcol + 1], t0[:, :n], ALU.mult, ALU.mult,
                r=rkeys + ["nr_t0", "qkg2"], w=["nr_kn"])
            if lat:
                mm(ps[1][:, :n], RTf[:, :], kn[:, :n], True, True, r=["nr_kn", kc_], w=[("ps", 1)])
                tt("dve", t1[:, :n], kn[:, :n], cosb[:, :n], ALU.mult, r=["nr_kn", "cs"], w=["nr_t1"])
                tt("dve", sq[:, :n], ps[1][:, :n], sinb[:, :n], ALU.mult, r=[("ps", 1), "cs"], w=["nr_sq"])
                tt("dve", kout, t1[:, :n], sq[:, :n], ALU.add, r=["nr_t1", "nr_sq"], w=wkeys)
            else:
                cp("dve", kout, kn[:, :n], r=["nr_kn"], w=wkeys)

        def proj_pass(blocks, scb, shb, consumer):
            xt = [A.f32(D) for _ in range(2)]
            ub = [A.bf16(D) for _ in range(2)]
            uT = [r3(A.bf16(KC * 512), KC) for _ in range(2)]
            tcount = 0
            for bi, (src, ntl, isc, info) in enumerate(blocks):
                ubuf = bi % 2
                for t in range(ntl):
                    b = tcount % 2
                    tcount += 1
                    dma("sp", xt[b], src[t * 128:(t + 1) * 128, :], w=[("xt", b)])
                    tt("dve", xt[b], xt[b], scb[isc], ALU.mult, r=[("xt", b), "bcmod"], w=[("xt", b)])
                    tt("dve", ub[b], xt[b], shb[isc], ALU.add, r=[("xt", b), "bcmod"], w=[("ub", b)])
                    for hh in range(2):
                        for c in range(8):
                            kc = hh * 8 + c
                            tr(ptb[hh][:, c * 128:(c + 1) * 128], ub[b][:, kc * 128:(kc + 1) * 128], identb[:, :],
                               r=[("ub", b), "identb"], w=[("ptb", hh)])
                        act(uT[ubuf][:, hh * 8:(hh + 1) * 8, t * 128:(t + 1) * 128],
                            r3(ptb[hh][:, :], 8), AF.Copy, r=[("ptb", hh)], w=[("uT", ubuf)])
                consumer(uT[ubuf], ("uT", ubuf), ntl, isc, info)

        if stop <= 0:
            P.emit(stack)
            return nc
        A.reset()
        scb = [A.f32(D, top=True), A.f32(D, top=True)]
        shb = [A.f32(D, top=True), A.f32(D, top=True)]
        dma("sp", scb[0], mod_s[1], r=[("mod_s", 1)], w=["bcmod"])
        dma("sp", shb[0], mod_s[0], r=[("mod_s", 0)], w=["bcmod"])
        dma("sp", scb[1], mod_s[3], r=[("mod_s", 3)], w=["bcmod"])
        dma("sp", shb[1], mod_s[2], r=[("mod_s", 2)], w=["bcmod"])
        wA = r3(A.bf16(KC * 1536), KC)
        for kc in range(KC):
            dma("pool", wA[:, kc, :], w_in[kc * 128:(kc + 1) * 128, 1024:2560], w=["wA"])
        cosb = A.f32(512); sinb = A.f32(512)
        nrt = [A.f32(512) for _ in range(4)]
        kob = [A.bf16(512) for _ in range(2)]
        vb = [A.bf16(256) for _ in range(2)]
        xst = [A.f32(512) for _ in range(2)]
        ctrA = [0, 0, 0]

        def consA(uTb, uk, ntl, isc, info):
            n = ntl * 128
            t0 = info
            if not isc:
                dma("sp", cosb[:, :n], cs_all[0][:, t0 - NCTX:t0 - NCTX + n], w=["cs"])
                dma("sp", sinb[:, :n], cs_all[1][:, t0 - NCTX:t0 - NCTX + n], w=["cs"])
            for h in range(2):
                for kc in range(KC):
                    mm(ps[0][:, :n], wA[:, kc, h * 128:(h + 1) * 128], uTb[:, kc, :n], kc == 0, kc == KC - 1,
                       r=[uk, "wA"], w=[("ps", 0)])
                ko = kob[ctrA[0] % 2]; kk = ("kob", ctrA[0] % 2); ctrA[0] += 1
                norm_rope(ps[0], n, 1, not isc, cosb, sinb, ko[:, :n], nrt, [("ps", 0)], [kk])
                dma("sp", kT_s[h][:, t0:t0 + n], ko[:, :n], r=[kk], w=["kT_s"])
            for t in range(ntl):
                pi = 2 + ctrA[1] % 2
                for kc in range(KC):
                    mm(ps[pi][:, 0:256], uTb[:, kc, t * 128:(t + 1) * 128], wA[:, kc, 256:512], kc == 0, kc == KC - 1,
                       r=[uk, "wA"], w=[("ps", pi)])
                v_ = vb[ctrA[1] % 2]; vk = ("vb", ctrA[1] % 2); ctrA[1] += 1
                act(v_, ps[pi][:, 0:256], AF.Copy, r=[("ps", pi)], w=[vk])
                dma("sp", V_s[t0 + t * 128:t0 + (t + 1) * 128, :], v_, r=[vk], w=["V_s"])
            for ct in range(8):
                pi = 4 + ctrA[2] % 2
                for kc in range(KC):
                    mm(ps[pi][:, :n], wA[:, kc, 512 + ct * 128:512 + (ct + 1) * 128], uTb[:, kc, :n], kc == 0, kc == KC - 1,
                       r=[uk, "wA"], w=[("ps", pi)])
                xs = xst[ctrA[2] % 2]; xk = ("xst", ctrA[2] % 2); ctrA[2] += 1
                act(xs[:, :n], ps[pi][:, :n], AF.Copy, r=[("ps", pi)], w=[xk])
                rows = slice(ct * 128, (ct + 1) * 128)
                if isc:
                    dma("sp", xrc_s[rows, 2:2 + n], xs[:, :n], r=[xk], w=["xrc_s"])
                else:
                    tl = t0 - NCTX
                    dma("sp", xr_s[rows, 2 + tl:2 + tl + n], xs[:, :n], r=[xk], w=["xr_s"])

        blocksA = [(ctx, 2, 1, 0)] + [(x_all[b * 512:(b + 1) * 512, :], 4, 0, NCTX + b * 512) for b in range(NB)]
        proj_pass(blocksA, scb, shb, consA)
        settle()

        if stop <= 1:
            P.emit(stack)
            return nc
        A.reset()
        qT = r3(A.bf16(8 * TOK, top=True), 8)
        q_hi = A.hi
        scb = [A.f32(D)]; shb = [A.f32(D)]
        dma("sp", scb[0], mod_s[1], r=[("mod_s", 1)], w=["bcmod"])
        dma("sp", shb[0], mod_s[0], r=[("mod_s", 0)], w=["bcmod"])
        wQ = r3(A.bf16(KC * 2048), KC)
        for kc in range(KC):
            dma("pool", wQ[:, kc, 0:1024], w_in[kc * 128:(kc + 1) * 128, 0:1024], w=["wQ"])
            dma("pool", wQ[:, kc, 1024:2048], w_in[kc * 128:(kc + 1) * 128, 2560:3584], w=["wQ"])
        cosb = A.f32(512); sinb = A.f32(512)
        nrt = [A.f32(512) for _ in range(4)]
        gyb = [A.f32(512) for _ in range(2)]
        ctrQ = [0]

        def consQ(uTb, uk, ntl, isc, info):
            n = ntl * 128
            t0 = info
            dma("sp", cosb[:, :n], cs_own[0][:, t0:t0 + n], w=["cs"])
            dma("sp", sinb[:, :n], cs_own[1][:, t0:t0 + n], w=["cs"])
            for h in range(8):
                for kc in range(KC):
                    mm(ps[0][:, :n], wQ[:, kc, h * 128:(h + 1) * 128], uTb[:, kc, :n], kc == 0, kc == KC - 1,
                       r=[uk, "wQ"], w=[("ps", 0)])
                norm_rope(ps[0], n, 0, True, cosb, sinb, qT[:, h, t0:t0 + n], nrt, [("ps", 0)], ["qT"])
            for ct in range(8):
                pi = 4 + ctrQ[0] % 2
                for kc in range(KC):
                    mm(ps[pi][:, :n], wQ[:, kc, 1024 + ct * 128:1024 + (ct + 1) * 128], uTb[:, kc, :n], kc == 0, kc == KC - 1,
                       r=[uk, "wQ"], w=[("ps", pi)])
                g_ = gyb[ctrQ[0] % 2]; gk = ("gyb", ctrQ[0] % 2); ctrQ[0] += 1
                act(g_[:, :n], ps[pi][:, :n], AF.Gelu, r=[("ps", pi)], w=[gk])
                dma("sp", gy_s[ct * 128:(ct + 1) * 128, t0:t0 + n], g_[:, :n], r=[gk], w=["gy_s"])

        blocksQ = [(x_own[b * 512:(b + 1) * 512, :], 4, 0, b * 512) for b in range(NBO)]
        proj_pass(blocksQ, scb, shb, consQ)
        settle()

        if stop <= 2:
            P.emit(stack)
            return nc
        A.reset(0, q_hi)
        mixT = r3(A.bf16(KC * TOK), KC)
        mix_lo = A.lo
        kTg = A.bf16(SA)
        Vg = r3(A.bf16(NKC * 128), NKC)
        pT = [A.bf16(512) for _ in range(3)]
        rec = A.f32(512)
        it = 0
        for g in range(2):
            dma("sp", kTg, kT_s[g], r=["kT_s"], w=["kTg"])
            Vsrc = V_s[:, g * 128:(g + 1) * 128].rearrange("(n p) d -> p n d", p=128)
            for c0 in range(0, NKC, 4):
                c1 = min(c0 + 4, NKC)
                dma("sp", Vg[:, c0:c1, :], Vsrc[:, c0:c1, :], r=["V_s"], w=["Vg"])
            for h in range(4 * g, 4 * g + 4):
                for qb in range(NBO):
                    qs = slice(qb * 512, (qb + 1) * 512)
                    for kc in range(NKC):
                        si = it % 2; pi = it % 3; it += 1
                        mm(ps[si][:, :], kTg[:, kc * 128:(kc + 1) * 128], qT[:, h, qs], True, True,
                           r=["kTg", "qT"], w=[("ps", si)])
                        act(pT[pi], ps[si][:, :], AF.Exp, scale=float(ATT_SCALE), r=[("ps", si)], w=[("pT", pi)])
                        mm(ps[2][:, :], Vg[:, kc, :], pT[pi], kc == 0, kc == NKC - 1, r=["Vg", ("pT", pi)], w=[("ps", 2)])
                        mm(ps[3][:, :], onesb[:, :], pT[pi], kc == 0, kc == NKC - 1, r=["onesb", ("pT", pi)], w=[("ps", 3)])
                    P.add("dve", lambda e: e.reciprocal(out=rec, in_=ps[3][:, :]), r=[("ps", 3)], w=["rec"])
                    tt("dve", mixT[:, h, qs], ps[2][:, :], rec, ALU.mult, r=[("ps", 2), "rec"], w=["mixT"])
        settle()

        if stop <= 3:
            P.emit(stack)
            return nc
        A.reset(mix_lo, ARENA_F)
        rgw = r3(A.f32(4 * 128), 4)
        rg_w3 = rg_w.rearrange("p (a j) -> p a j", a=32)
        xc = A.f32(NCTX + S)
        xin = [A.f32(520) for _ in range(2)]
        acc = A.f32(TOK)
        gy = A.f32(TOK)
        rt = [A.f32(512) for _ in range(2)]
        itl = [A.f32(512) for _ in range(2)]
        at = [A.f32(512) for _ in range(2)]
        a2 = [A.f32(512) for _ in range(2)]
        bt = [A.f32(512) for _ in range(2)]
        hb = [A.f32(512) for _ in range(3)]
        ci = 0
        gi = 0
        hi_ = 0
        for ct in range(8):
            rows = slice(ct * 128, (ct + 1) * 128)
            dma("sp", gy, gy_s[rows, :], r=["gy_s"], w=["gy"])
            for q4 in range(4):
                dma("sp", rgw[:, q4, :], rg_w3[:, q4 * 8 + ct, :], w=["rgw"])
            P.add("dve", lambda e: e.memset(acc, 0.0), w=["acc"])
            segs = [(xrc_s, 0, NCTX, 0)] + [(xr_s, b * 512, 512, NCTX + b * 512) for b in range(NB)]
            for (srct, c0, n, xo) in segs:
                xi = xin[ci % 2]; xk = ("xin", ci % 2); ci += 1
                dma("sp", xi[:, :n + 3], srct[rows, c0:c0 + n + 3], r=["xr_s", "xrc_s"], w=[xk])
                o = xc[:, xo:xo + n]
                ts("dve", o, xi[:, 0:n], cwT[:, ct * 4:ct * 4 + 1], cbT[:, ct:ct + 1], ALU.mult, ALU.add,
                   r=[xk, kc_], w=["xc"])
                for j in range(1, 4):
                    stt("dve", o, xi[:, j:j + n], cwT[:, ct * 4 + j:ct * 4 + j + 1], o, ALU.mult, ALU.add,
                        r=[xk, "xc", kc_], w=["xc"])
            for d in range(2):
                wi = d * 8 + ct
                order = [(0, NCTX, None)] + [(NCTX + b * 512, 512, b) for b in range(NB)]
                if d == 1:
                    order = [(0, NCTX, None)] + [(NCTX + b * 512, 512, b) for b in reversed(range(NB))]
                prev = None
                for (xo, n, b) in order:
                    gb = gi % 2; gi += 1
                    xs = xc[:, xo:xo + n]
                    mm(ps[gb][:, :n], rgw[:, d, :], xs, True, True, r=["rgw", "xc"], w=[("ps", gb)])
                    mm(ps[2 + gb][:, :n], rgw[:, 2 + d, :], xs, True, True, r=["rgw", "xc"], w=[("ps", 2 + gb)])
                    r_ = rt[gb]; i_ = itl[gb]; a_ = at[gb]; a2_ = a2[gb]; b_ = bt[gb]
                    act(r_[:, :n], ps[gb][:, :n], AF.Sigmoid, bias=rgv[:, wi:wi + 1], r=[("ps", gb), kc_], w=[("rt", gb)])
                    act(i_[:, :n], ps[2 + gb][:, :n], AF.Sigmoid, bias=rgv[:, 16 + wi:16 + wi + 1], r=[("ps", 2 + gb), kc_], w=[("it", gb)])
                    act(a_[:, :n], r_[:, :n], AF.Exp, scale=c8[:, wi:wi + 1], r=[("rt", gb), "c8"], w=[("at", gb)])
                    tt("dve", a2_[:, :n], a_[:, :n], a_[:, :n], ALU.mult, r=[("at", gb)], w=[("a2", gb)])
                    act(a2_[:, :n], a2_[:, :n], AF.Sqrt, scale=-1.0, bias=1.0, r=[("a2", gb)], w=[("a2", gb)])
                    tt("dve", b_[:, :n], a2_[:, :n], i_[:, :n], ALU.mult, r=[("a2", gb), ("it", gb)], w=[("bt", gb)])
                    tt("dve", b_[:, :n], b_[:, :n], xs, ALU.mult, r=[("bt", gb), "xc"], w=[("bt", gb)])
                    h_ = hb[hi_ % 3]; hk = ("hb", hi_ % 3); hi_ += 1
                    init = 0.0 if prev is None else prev[0]
                    rk = [("at", gb), ("bt", gb)] + ([] if prev is None else [prev[1]])
                    if d == 0:
                        P.add("dve", lambda e, h_=h_, a_=a_, b_=b_, n=n, init=init: e.tensor_tensor_scan(
                            out=h_[:, :n], data0=a_[:, :n], data1=b_[:, :n], initial=init, op0=ALU.mult, op1=ALU.add),
                            r=rk, w=[hk])
                        prev = (h_[:, n - 1:n], hk)
                    else:
                        P.add("dve", lambda e, h_=h_, a_=a_, b_=b_, n=n, init=init: e.tensor_tensor_scan(
                            out=h_[:, n - 1::-1] if False else h_[:, 0:n][:, ::-1], data0=a_[:, 0:n][:, ::-1], data1=b_[:, 0:n][:, ::-1],
                            initial=init, op0=ALU.mult, op1=ALU.add), r=rk, w=[hk])
                        prev = (h_[:, 0:1], hk)
                    if b is not None:
                        c = b // NBO
                        po = (b % NBO) * 512
                        stt("dve", acc[:, po:po + 512], h_[:, :n], sel[:, c:c + 1], acc[:, po:po + 512], ALU.mult, ALU.add,
                            r=[hk, "acc", kc_], w=["acc"])
            tt("dve", mixT[:, 8 + ct, :], acc, gy, ALU.mult, r=["acc", "gy"], w=["mixT"])
        for c0 in range(0, KC, 4):
            dma("sp", r3(mix_s, KC)[:, c0:c0 + 4, :], mixT[:, c0:c0 + 4, :], r=["mixT"], w=["mix_s"])
        settle()

        if stop <= 4:
            P.emit(stack)
            return nc
        A.reset()
        mtb = [r3(A.bf16(KC * 128), KC) for _ in range(2)]
        wo = r3(A.bf16(KC * D), KC)
        for kc in range(KC):
            dma("pool", wo[:, kc, :], w_out[kc * 128:(kc + 1) * 128, :], w=["wo"])
        g1b = A.f32(D); l1g = A.f32(D); l1b = A.f32(D); s2b = A.f32(D); h2b = A.f32(D)
        dma("sp", g1b, mod_s[4], r=[("mod_s", 4)], w=["bco"])
        dma("sp", l1g, ln_bc[0], w=["bco"])
        dma("sp", l1b, ln_bc[1], w=["bco"])
        dma("sp", s2b, mod_s[6], r=[("mod_s", 6)], w=["bco"])
        dma("sp", h2b, mod_s[5], r=[("mod_s", 5)], w=["bco"])
        wr = r3(A.f32(KC * NE), KC)
        wrs = w_router.rearrange("(a p) n -> p a n", p=128)
        for c0 in range(0, KC, 4):
            dma("sp", wr[:, c0:c0 + 4, :], wrs[:, c0:c0 + 4, :], w=["wr"])
        xo_ = A.f32(D); zt = A.f32(D); h1t = A.f32(D)
        vTb = r3(A.bf16(KC * 128), KC); vT32 = r3(A.f32(KC * 128), KC)
        stats = A.f32(24); mv = A.f32(2); rs = A.f32(1)
        sc_ = A.f32(NE); bi_ = A.f32(NE); m8 = A.f32(64); gs = A.f32(8); g8 = A.f32(8); pen = A.f32(8)
        mk_ = A.f32(NE); t8 = A.f32(8); wsel = A.f32(NE); wsum = A.f32(1)
        Wr3 = r3(Wr[:, :], NTO)

        def layer_norm(z, key, stats, mv, rs):
            for c4 in range(4):
                P.add("dve", lambda e, c4=c4: e.bn_stats(out=stats[:, c4 * 6:(c4 + 1) * 6], in_=z[:, c4 * 512:(c4 + 1) * 512]),
                      r=[key], w=["stats"])
            P.add("dve", lambda e: e.bn_aggr(out=mv, in_=stats), r=["stats"], w=["mv"])
            act(rs, mv[:, 1:2], AF.Sqrt, bias=float(EPS), r=["mv"], w=["rs"])
            P.add("dve", lambda e: e.reciprocal(out=rs, in_=rs), r=["rs"], w=["rs"])
            ts("dve", z, z, mv[:, 0:1], rs[:, 0:1], ALU.subtract, ALU.mult, r=[key, "mv", "rs"], w=[key])

        for t in range(NTO):
            tsl = slice(t * 128, (t + 1) * 128)
            dma("sp", xo_, x_own[tsl, :], w=["xo"])
            mt = mtb[t % 2]; mtk = ("mt", t % 2)
            for c0 in range(0, KC, 8):
                dma("sp", mt[:, c0:c0 + 8, :], r3(mix_s, KC)[:, c0:c0 + 8, tsl], r=["mix_s"], w=[mtk])
            for fb in range(4):
                pi = fb
                for kc in range(KC):
                    mm(ps[pi][:, :], mt[:, kc, :], wo[:, kc, fb * 512:(fb + 1) * 512], kc == 0, kc == KC - 1,
                       r=[mtk, "wo"], w=[("ps", pi)])
                tt("dve", zt[:, fb * 512:(fb + 1) * 512], ps[pi][:, :], g1b[:, fb * 512:(fb + 1) * 512], ALU.mult,
                   r=[("ps", pi), "bco"], w=["zt"])
            stt("dve", zt, xo_, float(ALPHA), zt, ALU.mult, ALU.add, r=["xo", "zt"], w=["zt"])
            layer_norm(zt, "zt", stats, mv, rs)
            tt("dve", h1t, zt, l1g, ALU.mult, r=["zt", "bco"], w=["h1t"])
            tt("dve", h1t, h1t, l1b, ALU.add, r=["h1t", "bco"], w=["h1t"])
            dma("sp", h1_s[tsl, :], h1t, r=["h1t"], w=["h1_s"])
            if osub >= 2:
                tt("dve", zt, h1t, s2b, ALU.mult, r=["h1t", "bco"], w=["zt"])
                tt("dve", zt, zt, h2b, ALU.add, r=["zt", "bco"], w=["zt"])
                for q4 in range(4):
                    pi = q4
                    for c in range(4):
                        kc = q4 * 4 + c
                        mm(ps[pi][:, c * 128:(c + 1) * 128], zt[:, kc * 128:(kc + 1) * 128], identf[:, :], True, True, r=["zt", kc_], w=[("ps", pi)])
                    cp("dve", vT32[:, q4 * 4:(q4 + 1) * 4, :], r3(ps[pi][:, :], 4), r=[("ps", pi)], w=["vT32"])
                    act(vTb[:, q4 * 4:(q4 + 1) * 4, :], vT32[:, q4 * 4:(q4 + 1) * 4, :], AF.Copy, r=["vT32"], w=["vTb"])
                for c0 in range(0, KC, 8):
                    dma("sp", r3(vT_s, KC)[:, c0:c0 + 8, tsl], vTb[:, c0:c0 + 8, :], r=["vTb"], w=["vT_s"])
            if osub >= 3:
                for kc in range(KC):
                    mm(ps[4][:, 0:NE], vT32[:, kc, :], wr[:, kc, :], kc == 0, kc == KC - 1, r=["vT32", "wr"], w=[("ps", 4)])
                act(sc_, ps[4][:, 0:NE], AF.Sigmoid, r=[("ps", 4)], w=["sc"])
            if osub >= 4:
                tt("dve", bi_, sc_, ebb[:, :], ALU.add, r=["sc", kc_], w=["bi"])
                for g in range(8):
                    P.add("dve", lambda e, g=g: e.max(out=m8[:, g * 8:(g + 1) * 8], in_=bi_[:, g * 8:(g + 1) * 8]), r=["bi"], w=["m8"])
                m83 = r3(m8, 8)
                tt("dve", gs.rearrange("p (a b) -> p a b", b=1), m83[:, :, 0:1], m83[:, :, 1:2], ALU.add, r=["m8"], w=["gs"])
                P.add("dve", lambda e: e.max(out=g8, in_=gs), r=["gs"], w=["g8"])
                ts("dve", pen, gs, g8[:, 3:4], None, ALU.is_ge, r=["gs", "g8"], w=["pen"])
                ts("dve", pen, pen, -1.0, 1.0e9, ALU.add, ALU.mult, r=["pen"], w=["pen"])
                for g in range(8):
                    ts("dve", mk_[:, g * 8:(g + 1) * 8], bi_[:, g * 8:(g + 1) * 8], pen[:, g:g + 1], None, ALU.add,
                       r=["bi", "pen"], w=["mk"])
                P.add("dve", lambda e: e.max(out=t8, in_=mk_), r=["mk"], w=["t8"])
                ts("dve", wsel, mk_, t8[:, 7:8], None, ALU.is_ge, r=["mk", "t8"], w=["wsel"])
                tt("dve", wsel, wsel, sc_, ALU.mult, r=["wsel", "sc"], w=["wsel"])
                P.add("dve", lambda e: e.reduce_sum(out=wsum, in_=wsel, axis=AX.X), r=["wsel"], w=["wsum"])
                P.add("dve", lambda e: e.reciprocal(out=wsum, in_=wsum), r=["wsum"], w=["wsum"])
                ts("dve", Wr3[:, t, 0:NE], wsel, wsum[:, 0:1], 2.5, ALU.mult, ALU.mult, r=["wsel", "wsum"], w=["Wr"])
                ts("dve", Wr3[:, t, NE:NE + 1], onesf[:, 0:1], 1.0, None, ALU.mult, r=[kc_], w=["Wr"])
        if debug:
            wr_dbg = nc.dram_tensor("wr_dbg", [128, NTO * (NE + 1)], F32, kind="ExternalOutput").ap()
            dma("sp", wr_dbg, Wr[:, :], r=["Wr"], w=["wr_dbg"])
        settle()

        if stop <= 5:
            P.emit(stack)
            return nc
        NTH = TH // 128
        for hf in range(NH):
            A.reset()
            vTh = r3(A.bf16(KC * TH), KC)
            for c0 in range(0, KC, 4):
                dma("sp", vTh[:, c0:c0 + 4, :], r3(vT_s, KC)[:, c0:c0 + 4, hf * TH:(hf + 1) * TH], r=["vT_s"], w=["vTh"])
            if debug and hf == 0:
                vth0_dbg = nc.dram_tensor("vth0_dbg", [128, KC * TH], BF16, kind="ExternalOutput").ap()
                dma("sp", vth0_dbg, vTh.rearrange("p a b -> p (a b)"), r=["vTh"], w=["vth0_dbg"])
            accm = r3(A.f32(NTH * D), NTH)
            gT = r3(A.bf16(4 * TH), 4)
            s1 = [A.bf16(512) for _ in range(2)]
            w_lo = A.lo
            w1 = [r3(A.bf16(KC * FF), KC) for _ in range(2)]
            w3 = [r3(A.bf16(KC * FF), KC) for _ in range(2)]
            w2 = [r3(A.bf16(4 * D), 4)] * 2
            stg = [A.f32(1024) for _ in range(2)]
            stg_ctr = [0]
            pc = 0
            elist = [nexp] + list(range(nexp))
            for ei, e_ in enumerate(elist):
                wb = ei % 2
                for c in range(8):
                    for (wt, wsrc, wk) in ((w1[wb], w_e1, ("w1", wb)), (w3[wb], w_e3, ("w3", wb))):
                        b = stg_ctr[0] % 2; stg_ctr[0] += 1
                        dma("sp", r3(stg[b], 2), wsrc[e_, c * 256:(c + 1) * 256, :].rearrange("(a p) n -> p a n", p=128),
                            w=[("stg", b)])
                        cp("pool", wt[:, 2 * c:2 * c + 2, :], r3(stg[b], 2), r=[("stg", b)], w=[wk])
                for c in range(8):
                    fc_, hh_ = c // 2, c % 2
                    b = stg_ctr[0] % 2; stg_ctr[0] += 1
                    dma("sp", stg[b], w_e2[e_, fc_ * 128:(fc_ + 1) * 128, hh_ * 1024:(hh_ + 1) * 1024], w=[("stg", b)])
                    cp("pool", w2[wb][:, fc_, hh_ * 1024:(hh_ + 1) * 1024], stg[b], r=[("stg", b)], w=[("w2", 0)])
                for tb in range(TH // 512):
                    tbs = slice(tb * 512, (tb + 1) * 512)
                    for fc in range(4):
                        p1 = pc % 2; p3 = 2 + pc % 2; pc += 1
                        for kc in range(KC):
                            mm(ps[p1][:, :], w1[wb][:, kc, fc * 128:(fc + 1) * 128], vTh[:, kc, tbs], kc == 0, kc == KC - 1,
                               r=[("w1", wb), "vTh"], w=[("ps", p1)])
                        for kc in range(KC):
                            mm(ps[p3][:, :], w3[wb][:, kc, fc * 128:(fc + 1) * 128], vTh[:, kc, tbs], kc == 0, kc == KC - 1,
                               r=[("w3", wb), "vTh"], w=[("ps", p3)])
                        act(s1[p1], ps[p1][:, :], AF.Silu, r=[("ps", p1)], w=[("s1", p1)])
                        tt("dve", gT[:, fc, tbs], s1[p1], ps[p3][:, :], ALU.mult, r=[("s1", p1), ("ps", p3)], w=["gT"])
                for t in range(NTH):
                    tg = hf * NTH + t
                    for fb in range(4):
                        py = 4 + pc % 2; pc += 1
                        for fc in range(4):
                            mm(ps[py][:, :], gT[:, fc, t * 128:(t + 1) * 128], w2[wb][:, fc, fb * 512:(fb + 1) * 512], fc == 0, fc == 3,
                               r=["gT", ("w2", 0)], w=[("ps", py)])
                        o = accm[:, t, fb * 512:(fb + 1) * 512]
                        if ei == 0:
                            cp("dve", o, ps[py][:, :], r=[("ps", py)], w=["accm"])
                        else:
                            stt("dve", o, ps[py][:, :], Wr3[:, tg, e_:e_ + 1], o, ALU.mult, ALU.add,
                                r=[("ps", py), "accm", "Wr"], w=["accm"])
            if debug and hf == 0:
                for nm, src_, n_, ky in (("gT_dbg", gT.rearrange("p a b -> p (a b)"), 4 * TH, "gT"),
                                         ("w1a_dbg", w1[0].rearrange("p a b -> p (a b)"), KC * FF, ("w1", 0)),
                                         ("w1b_dbg", w1[1].rearrange("p a b -> p (a b)"), KC * FF, ("w1", 1)),
                                         ("w3a_dbg", w3[0].rearrange("p a b -> p (a b)"), KC * FF, ("w3", 0)),
                                         ("w3b_dbg", w3[1].rearrange("p a b -> p (a b)"), KC * FF, ("w3", 1)),
                                         ("w2_dbg", w2[0].rearrange("p a b -> p (a b)"), 4 * D, ("w2", 0))):
                    dd = nc.dram_tensor(nm, [128, n_], BF16, kind="ExternalOutput").ap()
                    dma("sp", dd, src_, r=[ky], w=[nm])
                vth_dbg = nc.dram_tensor("vth_dbg", [128, KC * TH], BF16, kind="ExternalOutput").ap()
                dma("sp", vth_dbg, vTh.rearrange("p a b -> p (a b)"), r=["vTh"], w=["vth_dbg"])
                acc_dbg = nc.dram_tensor("acc_dbg", [128, NTH * D], F32, kind="ExternalOutput").ap()
                dma("sp", acc_dbg, accm.rearrange("p a b -> p (a b)"), r=["accm"], w=["acc_dbg"])
            settle()
            A.reset(w_lo, ARENA_F)
            g2b = A.f32(D); l2g = A.f32(D); l2b = A.f32(D); h1t = A.f32(D); ot = [A.f32(D) for _ in range(2)]
            stats = A.f32(24); mv = A.f32(2); rs = A.f32(1)
            dma("sp", g2b, mod_s[7], r=[("mod_s", 7)], w=["bcf"])
            dma("sp", l2g, ln_bc[2], w=["bcf"])
            dma("sp", l2b, ln_bc[3], w=["bcf"])
            for t in range(NTH):
                tsl = slice(hf * TH + t * 128, hf * TH + (t + 1) * 128)
                o_ = ot[t % 2]; ok = ("ot", t % 2)
                dma("sp", h1t, h1_s[tsl, :], r=["h1_s"], w=["h1t"])
                tt("dve", o_, accm[:, t, :], g2b, ALU.mult, r=["accm", "bcf"], w=[ok])
                stt("dve", o_, h1t, float(ALPHA), o_, ALU.mult, ALU.add, r=["h1t", ok], w=[ok])
                layer_norm(o_, ok, stats, mv, rs)
                tt("dve", o_, o_, l2g, ALU.mult, r=[ok, "bcf"], w=[ok])
                tt("dve", o_, o_, l2b, ALU.add, r=[ok, "bcf"], w=[ok])
                dma("sp", out[tsl, :], o_, r=[ok], w=["out"])
            settle()

        P.emit(stack)
    return nc


def _prep(inputs, S, nexp=NE, cores=range(8)):
    TOK = S // 8
    f = lambda a: np.ascontiguousarray(np.asarray(a, dtype=np.float32))
    x = f(inputs["x"])[0]; c = f(inputs["c"])[0]; ctx = f(inputs["ctx"])[0]; cc = f(inputs["c_ctx"])
    cT = np.zeros((128, 32), np.float32)
    cT[:, 0::2] = c.reshape(16, 128).T
    cT[:, 1::2] = cc.reshape(16, 128).T
    bc = lambda v: np.ascontiguousarray(np.broadcast_to(f(v).reshape(1, -1), (128, f(v).size)))
    conv_w = f(inputs["conv_w"])[0]
    conv_wT = np.ascontiguousarray(conv_w.reshape(4, 8, 128).transpose(2, 1, 0).reshape(128, 32))
    conv_bT = np.ascontiguousarray(f(inputs["conv_b"])[0].reshape(8, 128).T)
    wa = f(inputs["rg_wa"])[0]; wx = f(inputs["rg_wx"])[0]
    rg_w = np.concatenate([wa.reshape(16, 128, 128), wx.reshape(16, 128, 128)], 0)
    rg_w = np.ascontiguousarray(rg_w.transpose(1, 0, 2).reshape(128, 32 * 128))
    vecT = lambda v: f(v)[0].reshape(16, 128).T
    rg_vec = np.ascontiguousarray(np.concatenate([vecT(inputs["rg_ba"]), vecT(inputs["rg_bx"]), vecT(inputs["rg_lam"])], 1))
    ln_bc = np.stack([bc(inputs["ln1_g"]), bc(inputs["ln1_b"]), bc(inputs["ln2_g"]), bc(inputs["ln2_b"])], 0)
    qk_g = np.ascontiguousarray(np.stack([f(inputs["q_norm"])[0], f(inputs["k_norm"])[0]], 1))
    half = 32
    inv_freq = (np.float32(10000.0) ** (-np.arange(half, dtype=np.float32) / np.float32(half))).astype(np.float32)
    t = np.arange(S)
    row = (t // 64).astype(np.float32); col = (t % 64).astype(np.float32)
    ang = np.zeros((128, S), np.float32)
    for d in range(128):
        pos = row if d < 64 else col
        ang[d] = pos * inv_freq[(d % 64) % half]
    cs_all = np.stack([np.cos(ang), np.sin(ang)], 0).astype(np.float32)
    Rm = np.zeros((128, 128), np.float32)
    for base in (0, 64):
        for i in range(32):
            Rm[base + i, base + i + 32] = -1.0
            Rm[base + 32 + i, base + i] = 1.0
    consts = np.stack([np.eye(128, dtype=np.float32), np.ones((128, 128), np.float32),
                       np.ascontiguousarray(Rm.T), np.zeros((128, 128), np.float32)], 0)
    w_e1 = np.concatenate([f(inputs["w_e1"])[0][:nexp], f(inputs["w_s1"])], 0)
    w_e3 = np.concatenate([f(inputs["w_e3"])[0][:nexp], f(inputs["w_s3"])], 0)
    w_e2 = np.concatenate([f(inputs["w_e2"])[0][:nexp], f(inputs["w_s2"])], 0)
    common = dict(
        x_all=x, ctx=ctx, cT=cT, w_mod=f(inputs["w_mod"])[0], b_mod_bc=bc(inputs["b_mod"]),
        w_in=f(inputs["w_in"])[0], qk_g=qk_g, conv_wT=conv_wT, conv_bT=conv_bT, rg_w=rg_w, rg_vec=rg_vec,
        w_out=f(inputs["w_out"])[0], ln_bc=ln_bc, w_router=f(inputs["w_router"])[0], eb_bc=bc(inputs["e_bias"]),
        w_e1=w_e1, w_e3=w_e3, w_e2=w_e2, cs_all=cs_all, consts=consts)
    maps = []
    for j in cores:
        m = dict(common)
        m["x_own"] = np.ascontiguousarray(x[j * TOK:(j + 1) * TOK])
        m["cs_own"] = np.ascontiguousarray(cs_all[:, :, j * TOK:(j + 1) * TOK])
        s = np.zeros((128, 8), np.float32); s[:, j] = 1.0
        m["sel"] = s
        maps.append(m)
    return maps


_NC_CACHE = {}


def kernel(**inputs):
    S = int(np.asarray(inputs["x"]).shape[1])
    if S not in _NC_CACHE:
        _NC_CACHE[S] = build(S)
    nc = _NC_CACHE[S]
    maps = _prep(inputs, S)
    res = run_bass_kernel_spmd(nc, maps, core_ids=list(range(8)))
    outs = [np.asarray(r["out"], dtype=np.float32) for r in res.results]
    return np.concatenate(outs, 0)[None, :, :]
```

```python
import numpy as np
import concourse.bass as bass
import concourse.mybir as mybir
from concourse.bass_utils import run_bass_kernel_spmd

F32 = mybir.dt.float32
BF16 = mybir.dt.bfloat16
ALU = mybir.AluOpType
AF = mybir.ActivationFunctionType
AX = mybir.AxisListType

D = 2048
KC = 16
NCTX = 256
NE = 64
FF = 512
ALPHA = 2.0 ** 0.25
EPS = 1e-6
ATT_SCALE = 128.0 ** -0.5
ARENA_F = 50176

TRACE_LOG = None
ENGS = ("pe", "act", "dve", "pool", "sp")
EPOCH = 12000
DPOOL = 8
QDEPTH = {"pool": 1}
QSLOTS = {"pool": 4}


class Prog:
    def __init__(self, nc):
        self.nc = nc
        self.ops = []
        self.lastw = {}
        self.rd_c = {}
        self.rd_d = {}
        self.barrier = set()
        self.eng_ops = {e: [] for e in ENGS}
        self.ncomp = {e: 0 for e in ENGS}
        self.ndma = {e: 0 for e in ENGS}
        self.dma_ids = {e: [] for e in ENGS}

    def add(self, eng, fn, r=(), w=(), dma=False):
        oid = len(self.ops)
        deps = set(self.barrier)
        for k in r:
            if k in self.lastw:
                deps.add(self.lastw[k])
        for k in w:
            if k in self.lastw:
                deps.add(self.lastw[k])
            deps.update(self.rd_c.get(k, {}).values())
            deps.update(self.rd_d.get(k, ()))
        for k in r:
            if dma:
                self.rd_d.setdefault(k, []).append(oid)
            else:
                self.rd_c.setdefault(k, {})[eng] = oid
        for k in w:
            self.lastw[k] = oid
            self.rd_c[k] = {}
            self.rd_d[k] = []
        op = dict(eng=eng, fn=fn, deps=deps, dma=dma, tag=(tuple(r), tuple(w)))
        if dma:
            i = self.ndma[eng]
            self.ndma[eng] += 1
            dp = QDEPTH.get(eng, DPOOL)
            ns = QSLOTS.get(eng, DPOOL)
            op["slot"] = i % ns
            op["target"] = 16 * (i // ns + 1)
            if i >= dp:
                deps.add(self.dma_ids[eng][i - dp])
            self.dma_ids[eng].append(oid)
        else:
            n = self.ncomp[eng]
            self.ncomp[eng] += 1
            op["epoch"] = n // EPOCH
            op["idx"] = n % EPOCH + 1
        self.ops.append(op)
        self.eng_ops[eng].append(oid)
        return oid

    def fence(self):
        b = set()
        for e in ENGS:
            if self.eng_ops[e]:
                comp = [o for o in self.eng_ops[e] if not self.ops[o]["dma"]]
                if comp:
                    b.add(comp[-1])
            b.update(self.dma_ids[e][-DPOOL:])
        self.barrier = b

    def emit(self, stack):
        nc = self.nc
        csem = {}
        dsem = {}
        for e in ENGS:
            for ep in range(self.ncomp[e] // EPOCH + 1):
                csem[(e, ep)] = stack.enter_context(nc.semaphore(f"c_{e}_{ep}"))
            if self.ndma[e]:
                for s in range(DPOOL):
                    dsem[(e, s)] = stack.enter_context(nc.semaphore(f"d_{e}_{s}"))
        block = stack.enter_context(nc.Block())
        ops = self.ops

        def run(engname, eng):
            waited = {}
            for oid in self.eng_ops[engname]:
                op = ops[oid]
                need = {}
                for d in op["deps"]:
                    p = ops[d]
                    if p["dma"]:
                        key = ("d", p["eng"], p["slot"])
                        val = p["target"]
                    else:
                        if p["eng"] == engname and engname == "pe":
                            continue
                        key = ("c", p["eng"], p["epoch"])
                        val = p["idx"]
                    if need.get(key, 0) < val:
                        need[key] = val
                for key, val in need.items():
                    if waited.get(key, 0) >= val:
                        continue
                    waited[key] = val
                    sem = dsem[(key[1], key[2])] if key[0] == "d" else csem[(key[1], key[2])]
                    eng.wait_ge(sem, val)
                    if TRACE_LOG is not None:
                        TRACE_LOG.append((engname, oid, "wait", key, val))
                if TRACE_LOG is not None:
                    TRACE_LOG.append((engname, oid, "op", op.get("tag"), (op.get("slot"), op.get("target")) if op["dma"] else (op["epoch"], op["idx"])))
                inst = op["fn"](eng)
                if op["dma"]:
                    inst.then_inc(dsem[(engname, op["slot"])], 16)
                else:
                    inst.then_inc(csem[(engname, op["epoch"])], 1)
            for oid in self.dma_ids[engname][-DPOOL:]:
                op = ops[oid]
                key = ("d", engname, op["slot"])
                if waited.get(key, 0) < op["target"]:
                    waited[key] = op["target"]
                    eng.wait_ge(dsem[(engname, op["slot"])], op["target"])

        @block.tensor
        def _(e):
            run("pe", e)

        @block.scalar
        def _(e):
            run("act", e)

        @block.vector
        def _(e):
            run("dve", e)

        @block.gpsimd
        def _(e):
            run("pool", e)

        @block.sync
        def _(e):
            run("sp", e)


class Arena:
    def __init__(self, ap):
        self.ap = ap
        self.lo = 0
        self.hi = ARENA_F

    def reset(self, lo=0, hi=ARENA_F):
        self.lo = lo
        self.hi = hi

    def f32(self, n0, top=False):
        n = (n0 + 7) // 8 * 8
        if top:
            self.hi -= n
            off = self.hi
        else:
            off = self.lo
            self.lo += n
        assert self.lo <= self.hi, ("arena overflow", self.lo, self.hi)
        return self.ap[:, off:off + n0]

    def bf16(self, n, top=False):
        nf = (n + 1) // 2
        a = self.f32(nf, top=top)
        return a.bitcast(BF16)[:, 0:n]


def r3(ap, a):
    return ap.rearrange("p (a b) -> p a b", a=a)


def build(S, stop=99, nexp=NE, debug=False, osub=9):
    TOK = S // 8
    NB = S // 512
    NBO = TOK // 512
    NTO = TOK // 128
    SA = NCTX + S
    NKC = SA // 128
    TH = min(TOK, 1024)
    NH = TOK // TH
    nc = bass.Bass("TRN2", target_bir_lowering=False)

    def din(name, shape, dt=F32):
        return nc.dram_tensor(name, list(shape), dt, kind="ExternalInput").ap()

    x_all = din("x_all", [S, D]); x_own = din("x_own", [TOK, D]); ctx = din("ctx", [NCTX, D])
    cT = din("cT", [128, 32])
    w_mod = din("w_mod", [D, 6 * D]); b_mod_bc = din("b_mod_bc", [128, 6 * D])
    w_in = din("w_in", [D, 3584])
    qk_g = din("qk_g", [128, 2])
    conv_wT = din("conv_wT", [128, 32]); conv_bT = din("conv_bT", [128, 8])
    rg_w = din("rg_w", [128, 32 * 128])
    rg_vec = din("rg_vec", [128, 48])
    w_out = din("w_out", [D, D])
    ln_bc = din("ln_bc", [4, 128, D])
    w_router = din("w_router", [D, NE]); eb_bc = din("eb_bc", [128, NE])
    w_e1 = din("w_e1", [nexp + 1, D, FF]); w_e3 = din("w_e3", [nexp + 1, D, FF]); w_e2 = din("w_e2", [nexp + 1, FF, D])
    cs_all = din("cs_all", [2, 128, S]); cs_own = din("cs_own", [2, 128, TOK])
    consts = din("consts", [4, 128, 128])
    sel_in = din("sel", [128, 8])
    out = nc.dram_tensor("out", [TOK, D], F32, kind="ExternalOutput").ap()

    skind = "ExternalOutput" if debug else "Internal"

    def dscr(name, shape, dt):
        return nc.dram_tensor(name, list(shape), dt, kind=skind).ap()

    kT_s = dscr("kT_s", [2, 128, SA], BF16)
    V_s = dscr("V_s", [SA, 256], BF16)
    XP = S + 8
    xr_s = dscr("xr_s", [1024, XP], F32)
    xrc_s = dscr("xrc_s", [1024, NCTX + 8], F32)
    gy_s = dscr("gy_s", [1024, TOK], F32)
    h1_s = dscr("h1_s", [TOK, D], F32)
    vT_s = dscr("vT_s", [128, KC * TOK], BF16)
    mod_s = dscr("mod_s", [8, 128, D], F32)
    mix_s = dscr("mix_s", [128, KC * TOK], BF16)

    import contextlib
    stack = contextlib.ExitStack()
    with stack:
        def sb(name, shape, dt=F32):
            return stack.enter_context(nc.sbuf_tensor("sb_" + name, list(shape), dt))

        FA = sb("arena", [128, ARENA_F])
        identf = sb("identf", [128, 128]); onesf = sb("onesf", [128, 128]); RTf = sb("RTf", [128, 128])
        zerof = sb("zerof", [128, 128])
        identb = sb("identb", [128, 128], BF16); onesb = sb("onesb", [128, 128], BF16)
        sT = sb("sT", [128, 32]); csT = sb("csT", [128, 32])
        qkg = sb("qkg", [128, 2]); qkg2 = sb("qkg2", [128, 2])
        cwT = sb("cwT", [128, 32]); cbT = sb("cbT", [128, 8])
        rgv = sb("rgv", [128, 48]); c8 = sb("c8", [128, 16]); c8t = sb("c8t", [128, 16])
        sel = sb("sel", [128, 8]); ebb = sb("ebb", [128, NE])
        Wr = sb("Wr", [128, NTO * (NE + 1)])
        ps = [stack.enter_context(nc.psum_tensor(f"ps{i}", [128, 512], F32)) for i in range(6)]
        ptb = [stack.enter_context(nc.psum_tensor(f"ptb{i}", [128, 1024], BF16)) for i in range(2)]

        P = Prog(nc)
        A = Arena(FA)
        uid = [0]

        def K(name):
            uid[0] += 1
            return (name, uid[0])

        def dma(q, out_ap, in_ap, r=(), w=(), **kw):
            P.add(q, lambda e: e.dma_start(out=out_ap, in_=in_ap, **kw), r=r, w=w, dma=True)

        def mm(o, lhsT, rhs, start, stop, r=(), w=()):
            P.add("pe", lambda e: e.matmul(o, lhsT, rhs, start=start, stop=stop), r=r, w=w)

        def tr(o, in_, ident, r=(), w=()):
            P.add("pe", lambda e: e.transpose(o, in_, ident), r=r, w=w)

        def act(o, in_, func, r=(), w=(), **kw):
            P.add("act", lambda e: e.activation(out=o, in_=in_, func=func, **kw), r=r, w=w)

        def tt(eng, o, a, b, op, r=(), w=()):
            P.add(eng, lambda e: e.tensor_tensor(out=o, in0=a, in1=b, op=op), r=r, w=w)

        def ts(eng, o, a, s1, s2, op0, op1=None, r=(), w=()):
            if op1 is None:
                P.add(eng, lambda e: e.tensor_scalar(out=o, in0=a, scalar1=s1, scalar2=None, op0=op0), r=r, w=w)
            else:
                P.add(eng, lambda e: e.tensor_scalar(out=o, in0=a, scalar1=s1, scalar2=s2, op0=op0, op1=op1), r=r, w=w)

        def stt(eng, o, a, s, b, op0, op1, r=(), w=()):
            P.add(eng, lambda e: e.scalar_tensor_tensor(out=o, in0=a, scalar=s, in1=b, op0=op0, op1=op1), r=r, w=w)

        def cp(eng, o, a, r=(), w=()):
            P.add(eng, lambda e: e.tensor_copy(out=o, in_=a), r=r, w=w)

        dly_t = sb("dly_t", [128, 512])

        def settle():
            P.fence()
            for _i in range(32):
                dma("sp", dly_t[:, :], x_own[0:128, 0:512], w=["dly_t"])
            P.fence()

        kc_ = "const"
        for i, t in enumerate((identf, onesf, RTf, zerof)):
            dma("sp", t[:, :], consts[i], w=[kc_])
        for src, dst in ((cT, csT), (qk_g, qkg), (conv_wT, cwT), (conv_bT, cbT), (rg_vec, rgv),
                         (sel_in, sel), (eb_bc, ebb)):
            dma("sp", dst[:, :], src, w=[kc_])
        cp("dve", identb[:, :], identf[:, :], r=[kc_], w=["identb"])
        cp("dve", onesb[:, :], onesf[:, :], r=[kc_], w=["onesb"])
        ts("dve", qkg2[:, :], qkg[:, :], float(128.0 ** 0.5), None, ALU.mult, r=[kc_], w=["qkg2"])
        act(sT[:, :], csT[:, :], AF.Silu, r=[kc_], w=["sT"])
        act(c8t[:, :], rgv[:, 32:48], AF.Exp, scale=-1.0, r=[kc_], w=["c8t"])
        act(c8[:, :], c8t[:, :], AF.Ln, bias=1.0, r=["c8t"], w=["c8"])
        ts("dve", c8[:, :], c8[:, :], -8.0, None, ALU.mult, r=["c8"], w=["c8"])
        for rr in range(8):
            rows = slice(rr * 128, (rr + 1) * 128)
            dma("sp", xr_s[rows, 0:2], zerof[:, 0:2], r=[kc_], w=["xr_s"])
            dma("sp", xr_s[rows, S + 2:S + 8], zerof[:, 0:6], r=[kc_], w=["xr_s"])
            dma("sp", xrc_s[rows, 0:2], zerof[:, 0:2], r=[kc_], w=["xrc_s"])
            dma("sp", xrc_s[rows, NCTX + 2:NCTX + 8], zerof[:, 0:6], r=[kc_], w=["xrc_s"])

        A.reset()
        sbc = A.f32(KC * 2 * 128)
        sbc4 = sbc.rearrange("p (a v m) -> p a v m", a=KC, v=2)
        for kc in range(KC):
            for v in range(2):
                ts("dve", sbc4[:, kc, v, :], onesf[:, :], sT[:, 2 * kc + v:2 * kc + v + 1], None, ALU.mult,
                   r=["sT", kc_], w=["sbc"])
        wm = [r3(A.f32(KC * 512), KC) for _ in range(2)]
        bmb = [A.f32(512) for _ in range(2)]
        mo = [A.f32(512) for _ in range(2)]
        MODV = {(0, 0): 0, (1, 0): 1, (0, 1): 2, (1, 1): 3, (2, 0): 4, (3, 0): 5, (4, 0): 6, (5, 0): 7}
        cnt = 0
        for nb in range(24):
            sec = nb // 4
            buf = nb % 2
            col = slice(nb * 512, (nb + 1) * 512)
            wsrc = w_mod[:, col].rearrange("(a p) n -> p a n", p=128)
            for c0 in range(0, KC, 4):
                dma("sp", wm[buf][:, c0:c0 + 4, :], wsrc[:, c0:c0 + 4, :], w=[("wm", buf)])
            dma("sp", bmb[buf], b_mod_bc[:, col], w=[("bmb", buf)])
            for v in ((0, 1) if sec < 2 else (0,)):
                pt = ps[cnt % 2]; pk = ("ps", cnt % 2)
                for kc in range(KC):
                    mm(pt[:, :], sbc4[:, kc, v, :], wm[buf][:, kc, :], kc == 0, kc == KC - 1,
                       r=[("wm", buf), "sbc"], w=[pk])
                mb = mo[cnt % 2]; mk = ("mo", cnt % 2)
                tt("dve", mb, pt[:, :], bmb[buf], ALU.add, r=[pk, ("bmb", buf)], w=[mk])
                if sec in (1, 4):
                    ts("dve", mb, mb, 1.0, None, ALU.add, r=[mk], w=[mk])
                idx = MODV[(sec, v)]
                dma("sp", mod_s[idx][:, (nb % 4) * 512:(nb % 4 + 1) * 512], mb, r=[mk], w=[("mod_s", idx)])
                cnt += 1
        settle()

        def norm_rope(praw, n, gcol, lat, cosb, sinb, kout, tmp, rkeys, wkeys):
            sq, t0, kn, t1 = tmp
            act(sq[:, :n], praw[:, :n], AF.Square, r=rkeys, w=["nr_sq"])
            mm(ps[1][:, :n], onesf[:, :], sq[:, :n], True, True, r=["nr_sq", kc_], w=[("ps", 1)])
            act(t0[:, :n], ps[1][:, :n], AF.Sqrt, bias=float(128 * EPS), r=[("ps", 1)], w=["nr_t0"])
            P.add("dve", lambda e: e.reciprocal(out=t0[:, :n], in_=t0[:, :n]), r=["nr_t0"], w=["nr_t0"])
            stt("dve", kn[:, :n], praw[:, :n], qkg2[:, gcol:gcol + 1], t0[:, :n], ALU.mult, ALU.mult,
                r=rkeys + ["nr_t0", "qkg2"], w=["nr_kn"])
            if lat:
                mm(ps[1][:, :n], RTf[:, :], kn[:, :n], True, True, r=["nr_kn", kc_], w=[("ps", 1)])
                tt("dve", t1[:, :n], kn[:, :n], cosb[:, :n], ALU.mult, r=["nr_kn", "cs"], w=["nr_t1"])
                tt("dve", sq[:, :n], ps[1][:, :n], sinb[:, :n], ALU.mult, r=[("ps", 1), "cs"], w=["nr_sq"])
                tt("dve", kout, t1[:, :n], sq[:, :n], ALU.add, r=["nr_t1", "nr_sq"], w=wkeys)
            else:
                cp("dve", kout, kn[:, :n], r=["nr_kn"], w=wkeys)

        def proj_pass(blocks, scb, shb, consumer):
            xt = [A.f32(D) for _ in range(2)]
            ub = [A.bf16(D) for _ in range(2)]
            uT = [r3(A.bf16(KC * 512), KC) for _ in range(2)]
            tcount = 0
            for bi, (src, ntl, isc, info) in enumerate(blocks):
                ubuf = bi % 2
                for t in range(ntl):
                    b = tcount % 2
                    tcount += 1
                    dma("sp", xt[b], src[t * 128:(t + 1) * 128, :], w=[("xt", b)])
                    tt("dve", xt[b], xt[b], scb[isc], ALU.mult, r=[("xt", b), "bcmod"], w=[("xt", b)])
                    tt("dve", ub[b], xt[b], shb[isc], ALU.add, r=[("xt", b), "bcmod"], w=[("ub", b)])
                    for hh in range(2):
                        for c in range(8):
                            kc = hh * 8 + c
                            tr(ptb[hh][:, c * 128:(c + 1) * 128], ub[b][:, kc * 128:(kc + 1) * 128], identb[:, :],
                               r=[("ub", b), "identb"], w=[("ptb", hh)])
                        act(uT[ubuf][:, hh * 8:(hh + 1) * 8, t * 128:(t + 1) * 128],
                            r3(ptb[hh][:, :], 8), AF.Copy, r=[("ptb", hh)], w=[("uT", ubuf)])
                consumer(uT[ubuf], ("uT", ubuf), ntl, isc, info)

        if stop <= 0:
            P.emit(stack)
            return nc
        A.reset()
        scb = [A.f32(D, top=True), A.f32(D, top=True)]
        shb = [A.f32(D, top=True), A.f32(D, top=True)]
        dma("sp", scb[0], mod_s[1], r=[("mod_s", 1)], w=["bcmod"])
        dma("sp", shb[0], mod_s[0], r=[("mod_s", 0)], w=["bcmod"])
        dma("sp", scb[1], mod_s[3], r=[("mod_s", 3)], w=["bcmod"])
        dma("sp", shb[1], mod_s[2], r=[("mod_s", 2)], w=["bcmod"])
        wA = r3(A.bf16(KC * 1536), KC)
        for kc in range(KC):
            dma("pool", wA[:, kc, :], w_in[kc * 128:(kc + 1) * 128, 1024:2560], w=["wA"])
        cosb = A.f32(512); sinb = A.f32(512)
        nrt = [A.f32(512) for _ in range(4)]
        kob = [A.bf16(512) for _ in range(2)]
        vb = [A.bf16(256) for _ in range(2)]
        xst = [A.f32(512) for _ in range(2)]
        ctrA = [0, 0, 0]

        def consA(uTb, uk, ntl, isc, info):
            n = ntl * 128
            t0 = info
            if not isc:
                dma("sp", cosb[:, :n], cs_all[0][:, t0 - NCTX:t0 - NCTX + n], w=["cs"])
                dma("sp", sinb[:, :n], cs_all[1][:, t0 - NCTX:t0 - NCTX + n], w=["cs"])
            for h in range(2):
                for kc in range(KC):
                    mm(ps[0][:, :n], wA[:, kc, h * 128:(h + 1) * 128], uTb[:, kc, :n], kc == 0, kc == KC - 1,
                       r=[uk, "wA"], w=[("ps", 0)])
                ko = kob[ctrA[0] % 2]; kk = ("kob", ctrA[0] % 2); ctrA[0] += 1
                norm_rope(ps[0], n, 1, not isc, cosb, sinb, ko[:, :n], nrt, [("ps", 0)], [kk])
                dma("sp", kT_s[h][:, t0:t0 + n], ko[:, :n], r=[kk], w=["kT_s"])
            for t in range(ntl):
                pi = 2 + ctrA[1] % 2
                for kc in range(KC):
                    mm(ps[pi][:, 0:256], uTb[:, kc, t * 128:(t + 1) * 128], wA[:, kc, 256:512], kc == 0, kc == KC - 1,
                       r=[uk, "wA"], w=[("ps", pi)])
                v_ = vb[ctrA[1] % 2]; vk = ("vb", ctrA[1] % 2); ctrA[1] += 1
                act(v_, ps[pi][:, 0:256], AF.Copy, r=[("ps", pi)], w=[vk])
                dma("sp", V_s[t0 + t * 128:t0 + (t + 1) * 128, :], v_, r=[vk], w=["V_s"])
            for ct in range(8):
                pi = 4 + ctrA[2] % 2
                for kc in range(KC):
                    mm(ps[pi][:, :n], wA[:, kc, 512 + ct * 128:512 + (ct + 1) * 128], uTb[:, kc, :n], kc == 0, kc == KC - 1,
                       r=[uk, "wA"], w=[("ps", pi)])
                xs = xst[ctrA[2] % 2]; xk = ("xst", ctrA[2] % 2); ctrA[2] += 1
                act(xs[:, :n], ps[pi][:, :n], AF.Copy, r=[("ps", pi)], w=[xk])
                rows = slice(ct * 128, (ct + 1) * 128)
                if isc:
                    dma("sp", xrc_s[rows, 2:2 + n], xs[:, :n], r=[xk], w=["xrc_s"])
                else:
                    tl = t0 - NCTX
                    dma("sp", xr_s[rows, 2 + tl:2 + tl + n], xs[:, :n], r=[xk], w=["xr_s"])

        blocksA = [(ctx, 2, 1, 0)] + [(x_all[b * 512:(b + 1) * 512, :], 4, 0, NCTX + b * 512) for b in range(NB)]
        proj_pass(blocksA, scb, shb, consA)
        settle()

        if stop <= 1:
            P.emit(stack)
            return nc
        A.reset()
        qT = r3(A.bf16(8 * TOK, top=True), 8)
        q_hi = A.hi
        scb = [A.f32(D)]; shb = [A.f32(D)]
        dma("sp", scb[0], mod_s[1], r=[("mod_s", 1)], w=["bcmod"])
        dma("sp", shb[0], mod_s[0], r=[("mod_s", 0)], w=["bcmod"])
        wQ = r3(A.bf16(KC * 2048), KC)
        for kc in range(KC):
            dma("pool", wQ[:, kc, 0:1024], w_in[kc * 128:(kc + 1) * 128, 0:1024], w=["wQ"])
            dma("pool", wQ[:, kc, 1024:2048], w_in[kc * 128:(kc + 1) * 128, 2560:3584], w=["wQ"])
        cosb = A.f32(512); sinb = A.f32(512)
        nrt = [A.f32(512) for _ in range(4)]
        gyb = [A.f32(512) for _ in range(2)]
        ctrQ = [0]

        def consQ(uTb, uk, ntl, isc, info):
            n = ntl * 128
            t0 = info
            dma("sp", cosb[:, :n], cs_own[0][:, t0:t0 + n], w=["cs"])
            dma("sp", sinb[:, :n], cs_own[1][:, t0:t0 + n], w=["cs"])
            for h in range(8):
                for kc in range(KC):
                    mm(ps[0][:, :n], wQ[:, kc, h * 128:(h + 1) * 128], uTb[:, kc, :n], kc == 0, kc == KC - 1,
                       r=[uk, "wQ"], w=[("ps", 0)])
                norm_rope(ps[0], n, 0, True, cosb, sinb, qT[:, h, t0:t0 + n], nrt, [("ps", 0)], ["qT"])
            for ct in range(8):
                pi = 4 + ctrQ[0] % 2
                for kc in range(KC):
                    mm(ps[pi][:, :n], wQ[:, kc, 1024 + ct * 128:1024 + (ct + 1) * 128], uTb[:, kc, :n], kc == 0, kc == KC - 1,
                       r=[uk, "wQ"], w=[("ps", pi)])
                g_ = gyb[ctrQ[0] % 2]; gk = ("gyb", ctrQ[0] % 2); ctrQ[0] += 1
                act(g_[:, :n], ps[pi][:, :n], AF.Gelu, r=[("ps", pi)], w=[gk])
                dma("sp", gy_s[ct * 128:(ct + 1) * 128, t0:t0 + n], g_[:, :n], r=[gk], w=["gy_s"])

        blocksQ = [(x_own[b * 512:(b + 1) * 512, :], 4, 0, b * 512) for b in range(NBO)]
        proj_pass(blocksQ, scb, shb, consQ)
        settle()

        if stop <= 2:
            P.emit(stack)
            return nc
        A.reset(0, q_hi)
        mixT = r3(A.bf16(KC * TOK), KC)
        mix_lo = A.lo
        kTg = A.bf16(SA)
        Vg = r3(A.bf16(NKC * 128), NKC)
        pT = [A.bf16(512) for _ in range(3)]
        rec = A.f32(512)
        it = 0
        hq = 0
        for g in range(2):
            dma("sp", kTg, kT_s[g], r=["kT_s"], w=["kTg"])
            Vsrc = V_s[:, g * 128:(g + 1) * 128].rearrange("(n p) d -> p n d", p=128)
            for c0 in range(0, NKC, 4):
                c1 = min(c0 + 4, NKC)
                dma("sp", Vg[:, c0:c1, :], Vsrc[:, c0:c1, :], r=["V_s"], w=["Vg"])
            for h in range(4 * g, 4 * g + 4):
                for qb in range(NBO):
                    qs = slice(qb * 512, (qb + 1) * 512)
                    po = 2 + 2 * (hq % 2); pl = 3 + 2 * (hq % 2); hq += 1

                    def s_mm(kc, it0):
                        si = (it0 + kc) % 2
                        mm(ps[si][:, :], kTg[:, kc * 128:(kc + 1) * 128], qT[:, h, qs], True, True,
                           r=["kTg", "qT"], w=[("ps", si)])

                    s_mm(0, it)
                    for kc in range(NKC):
                        si = (it + kc) % 2; pi = (it + kc) % 3
                        if kc + 1 < NKC:
                            s_mm(kc + 1, it)
                        act(pT[pi], ps[si][:, :], AF.Exp, scale=float(ATT_SCALE), r=[("ps", si)], w=[("pT", pi)])
                        mm(ps[po][:, :], Vg[:, kc, :], pT[pi], kc == 0, kc == NKC - 1, r=["Vg", ("pT", pi)], w=[("ps", po)])
                        mm(ps[pl][:, :], onesb[:, :], pT[pi], kc == 0, kc == NKC - 1, r=["onesb", ("pT", pi)], w=[("ps", pl)])
                    it += NKC
                    P.add("dve", lambda e, pl=pl: e.reciprocal(out=rec, in_=ps[pl][:, :]), r=[("ps", pl)], w=["rec"])
                    tt("dve", mixT[:, h, qs], ps[po][:, :], rec, ALU.mult, r=[("ps", po), "rec"], w=["mixT"])
        settle()

        if stop <= 3:
            P.emit(stack)
            return nc
        A.reset(mix_lo, ARENA_F)
        rgw = r3(A.f32(4 * 128), 4)
        rg_w3 = rg_w.rearrange("p (a j) -> p a j", a=32)
        xc = A.f32(NCTX + S)
        xin = [A.f32(520) for _ in range(2)]
        acc = A.f32(TOK)
        gy = A.f32(TOK)
        rt = [A.f32(512) for _ in range(2)]
        itl = [A.f32(512) for _ in range(2)]
        at = [A.f32(512) for _ in range(2)]
        a2 = [A.f32(512) for _ in range(2)]
        bt = [A.f32(512) for _ in range(2)]
        hb = [A.f32(512) for _ in range(3)]
        ci = 0
        gi = 0
        hi_ = 0
        for ct in range(8):
            rows = slice(ct * 128, (ct + 1) * 128)
            dma("sp", gy, gy_s[rows, :], r=["gy_s"], w=["gy"])
            for q4 in range(4):
                dma("sp", rgw[:, q4, :], rg_w3[:, q4 * 8 + ct, :], w=["rgw"])
            P.add("dve", lambda e: e.memset(acc, 0.0), w=["acc"])
            segs = [(xrc_s, 0, NCTX, 0)] + [(xr_s, b * 512, 512, NCTX + b * 512) for b in range(NB)]
            for (srct, c0, n, xo) in segs:
                xi = xin[ci % 2]; xk = ("xin", ci % 2); ci += 1
                dma("sp", xi[:, :n + 3], srct[rows, c0:c0 + n + 3], r=["xr_s", "xrc_s"], w=[xk])
                o = xc[:, xo:xo + n]
                ts("dve", o, xi[:, 0:n], cwT[:, ct * 4:ct * 4 + 1], cbT[:, ct:ct + 1], ALU.mult, ALU.add,
                   r=[xk, kc_], w=["xc"])
                for j in range(1, 4):
                    stt("dve", o, xi[:, j:j + n], cwT[:, ct * 4 + j:ct * 4 + j + 1], o, ALU.mult, ALU.add,
                        r=[xk, "xc", kc_], w=["xc"])
            for d in range(2):
                wi = d * 8 + ct
                order = [(0, NCTX, None)] + [(NCTX + b * 512, 512, b) for b in range(NB)]
                if d == 1:
                    order = [(0, NCTX, None)] + [(NCTX + b * 512, 512, b) for b in reversed(range(NB))]
                prev = None
                for (xo, n, b) in order:
                    gb = gi % 2; gi += 1
                    xs = xc[:, xo:xo + n]
                    mm(ps[gb][:, :n], rgw[:, d, :], xs, True, True, r=["rgw", "xc"], w=[("ps", gb)])
                    mm(ps[2 + gb][:, :n], rgw[:, 2 + d, :], xs, True, True, r=["rgw", "xc"], w=[("ps", 2 + gb)])
                    r_ = rt[gb]; i_ = itl[gb]; a_ = at[gb]; a2_ = a2[gb]; b_ = bt[gb]
                    act(r_[:, :n], ps[gb][:, :n], AF.Sigmoid, bias=rgv[:, wi:wi + 1], r=[("ps", gb), kc_], w=[("rt", gb)])
                    act(i_[:, :n], ps[2 + gb][:, :n], AF.Sigmoid, bias=rgv[:, 16 + wi:16 + wi + 1], r=[("ps", 2 + gb), kc_], w=[("it", gb)])
                    act(a_[:, :n], r_[:, :n], AF.Exp, scale=c8[:, wi:wi + 1], r=[("rt", gb), "c8"], w=[("at", gb)])
                    tt("dve", a2_[:, :n], a_[:, :n], a_[:, :n], ALU.mult, r=[("at", gb)], w=[("a2", gb)])
                    act(a2_[:, :n], a2_[:, :n], AF.Sqrt, scale=-1.0, bias=1.0, r=[("a2", gb)], w=[("a2", gb)])
                    tt("dve", b_[:, :n], a2_[:, :n], i_[:, :n], ALU.mult, r=[("a2", gb), ("it", gb)], w=[("bt", gb)])
                    tt("dve", b_[:, :n], b_[:, :n], xs, ALU.mult, r=[("bt", gb), "xc"], w=[("bt", gb)])
                    h_ = hb[hi_ % 3]; hk = ("hb", hi_ % 3); hi_ += 1
                    init = 0.0 if prev is None else prev[0]
                    rk = [("at", gb), ("bt", gb)] + ([] if prev is None else [prev[1]])
                    if d == 0:
                        P.add("dve", lambda e, h_=h_, a_=a_, b_=b_, n=n, init=init: e.tensor_tensor_scan(
                            out=h_[:, :n], data0=a_[:, :n], data1=b_[:, :n], initial=init, op0=ALU.mult, op1=ALU.add),
                            r=rk, w=[hk])
                        prev = (h_[:, n - 1:n], hk)
                    else:
                        P.add("dve", lambda e, h_=h_, a_=a_, b_=b_, n=n, init=init: e.tensor_tensor_scan(
                            out=h_[:, n - 1::-1] if False else h_[:, 0:n][:, ::-1], data0=a_[:, 0:n][:, ::-1], data1=b_[:, 0:n][:, ::-1],
                            initial=init, op0=ALU.mult, op1=ALU.add), r=rk, w=[hk])
                        prev = (h_[:, 0:1], hk)
                    if b is not None:
                        c = b // NBO
                        po = (b % NBO) * 512
                        stt("dve", acc[:, po:po + 512], h_[:, :n], sel[:, c:c + 1], acc[:, po:po + 512], ALU.mult, ALU.add,
                            r=[hk, "acc", kc_], w=["acc"])
            tt("dve", mixT[:, 8 + ct, :], acc, gy, ALU.mult, r=["acc", "gy"], w=["mixT"])
        for c0 in range(0, KC, 4):
            dma("sp", r3(mix_s, KC)[:, c0:c0 + 4, :], mixT[:, c0:c0 + 4, :], r=["mixT"], w=["mix_s"])
        settle()

        if stop <= 4:
            P.emit(stack)
            return nc
        A.reset()
        mtb = [r3(A.bf16(KC * 128), KC) for _ in range(2)]
        wo = r3(A.bf16(KC * D), KC)
        for kc in range(KC):
            dma("pool", wo[:, kc, :], w_out[kc * 128:(kc + 1) * 128, :], w=["wo"])
        g1b = A.f32(D); l1g = A.f32(D); l1b = A.f32(D); s2b = A.f32(D); h2b = A.f32(D)
        dma("sp", g1b, mod_s[4], r=[("mod_s", 4)], w=["bco"])
        dma("sp", l1g, ln_bc[0], w=["bco"])
        dma("sp", l1b, ln_bc[1], w=["bco"])
        dma("sp", s2b, mod_s[6], r=[("mod_s", 6)], w=["bco"])
        dma("sp", h2b, mod_s[5], r=[("mod_s", 5)], w=["bco"])
        wr = r3(A.f32(KC * NE), KC)
        wrs = w_router.rearrange("(a p) n -> p a n", p=128)
        for c0 in range(0, KC, 4):
            dma("sp", wr[:, c0:c0 + 4, :], wrs[:, c0:c0 + 4, :], w=["wr"])
        xo_ = A.f32(D); zt = A.f32(D); h1t = A.f32(D)
        vTb = r3(A.bf16(KC * 128), KC); vT32 = r3(A.f32(KC * 128), KC)
        stats = A.f32(24); mv = A.f32(2); rs = A.f32(1)
        sc_ = A.f32(NE); bi_ = A.f32(NE); m8 = A.f32(64); gs = A.f32(8); g8 = A.f32(8); pen = A.f32(8)
        mk_ = A.f32(NE); t8 = A.f32(8); wsel = A.f32(NE); wsum = A.f32(1)
        Wr3 = r3(Wr[:, :], NTO)

        def layer_norm(z, key, stats, mv, rs):
            for c4 in range(4):
                P.add("dve", lambda e, c4=c4: e.bn_stats(out=stats[:, c4 * 6:(c4 + 1) * 6], in_=z[:, c4 * 512:(c4 + 1) * 512]),
                      r=[key], w=["stats"])
            P.add("dve", lambda e: e.bn_aggr(out=mv, in_=stats), r=["stats"], w=["mv"])
            act(rs, mv[:, 1:2], AF.Sqrt, bias=float(EPS), r=["mv"], w=["rs"])
            P.add("dve", lambda e: e.reciprocal(out=rs, in_=rs), r=["rs"], w=["rs"])
            ts("dve", z, z, mv[:, 0:1], rs[:, 0:1], ALU.subtract, ALU.mult, r=[key, "mv", "rs"], w=[key])

        for t in range(NTO):
            tsl = slice(t * 128, (t + 1) * 128)
            dma("sp", xo_, x_own[tsl, :], w=["xo"])
            mt = mtb[t % 2]; mtk = ("mt", t % 2)
            for c0 in range(0, KC, 8):
                dma("sp", mt[:, c0:c0 + 8, :], r3(mix_s, KC)[:, c0:c0 + 8, tsl], r=["mix_s"], w=[mtk])
            for fb in range(4):
                pi = fb
                for kc in range(KC):
                    mm(ps[pi][:, :], mt[:, kc, :], wo[:, kc, fb * 512:(fb + 1) * 512], kc == 0, kc == KC - 1,
                       r=[mtk, "wo"], w=[("ps", pi)])
                tt("dve", zt[:, fb * 512:(fb + 1) * 512], ps[pi][:, :], g1b[:, fb * 512:(fb + 1) * 512], ALU.mult,
                   r=[("ps", pi), "bco"], w=["zt"])
            stt("dve", zt, xo_, float(ALPHA), zt, ALU.mult, ALU.add, r=["xo", "zt"], w=["zt"])
            layer_norm(zt, "zt", stats, mv, rs)
            tt("dve", h1t, zt, l1g, ALU.mult, r=["zt", "bco"], w=["h1t"])
            tt("dve", h1t, h1t, l1b, ALU.add, r=["h1t", "bco"], w=["h1t"])
            dma("sp", h1_s[tsl, :], h1t, r=["h1t"], w=["h1_s"])
            if osub >= 2:
                tt("dve", zt, h1t, s2b, ALU.mult, r=["h1t", "bco"], w=["zt"])
                tt("dve", zt, zt, h2b, ALU.add, r=["zt", "bco"], w=["zt"])
                for q4 in range(4):
                    pi = q4
                    for c in range(4):
                        kc = q4 * 4 + c
                        mm(ps[pi][:, c * 128:(c + 1) * 128], zt[:, kc * 128:(kc + 1) * 128], identf[:, :], True, True, r=["zt", kc_], w=[("ps", pi)])
                    cp("dve", vT32[:, q4 * 4:(q4 + 1) * 4, :], r3(ps[pi][:, :], 4), r=[("ps", pi)], w=["vT32"])
                    act(vTb[:, q4 * 4:(q4 + 1) * 4, :], vT32[:, q4 * 4:(q4 + 1) * 4, :], AF.Copy, r=["vT32"], w=["vTb"])
                for c0 in range(0, KC, 8):
                    dma("sp", r3(vT_s, KC)[:, c0:c0 + 8, tsl], vTb[:, c0:c0 + 8, :], r=["vTb"], w=["vT_s"])
            if osub >= 3:
                for kc in range(KC):
                    mm(ps[4][:, 0:NE], vT32[:, kc, :], wr[:, kc, :], kc == 0, kc == KC - 1, r=["vT32", "wr"], w=[("ps", 4)])
                act(sc_, ps[4][:, 0:NE], AF.Sigmoid, r=[("ps", 4)], w=["sc"])
            if osub >= 4:
                tt("dve", bi_, sc_, ebb[:, :], ALU.add, r=["sc", kc_], w=["bi"])
                for g in range(8):
                    P.add("dve", lambda e, g=g: e.max(out=m8[:, g * 8:(g + 1) * 8], in_=bi_[:, g * 8:(g + 1) * 8]), r=["bi"], w=["m8"])
                m83 = r3(m8, 8)
                tt("dve", gs.rearrange("p (a b) -> p a b", b=1), m83[:, :, 0:1], m83[:, :, 1:2], ALU.add, r=["m8"], w=["gs"])
                P.add("dve", lambda e: e.max(out=g8, in_=gs), r=["gs"], w=["g8"])
                ts("dve", pen, gs, g8[:, 3:4], None, ALU.is_ge, r=["gs", "g8"], w=["pen"])
                ts("dve", pen, pen, -1.0, 1.0e9, ALU.add, ALU.mult, r=["pen"], w=["pen"])
                for g in range(8):
                    ts("dve", mk_[:, g * 8:(g + 1) * 8], bi_[:, g * 8:(g + 1) * 8], pen[:, g:g + 1], None, ALU.add,
                       r=["bi", "pen"], w=["mk"])
                P.add("dve", lambda e: e.max(out=t8, in_=mk_), r=["mk"], w=["t8"])
                ts("dve", wsel, mk_, t8[:, 7:8], None, ALU.is_ge, r=["mk", "t8"], w=["wsel"])
                tt("dve", wsel, wsel, sc_, ALU.mult, r=["wsel", "sc"], w=["wsel"])
                P.add("dve", lambda e: e.reduce_sum(out=wsum, in_=wsel, axis=AX.X), r=["wsel"], w=["wsum"])
                P.add("dve", lambda e: e.reciprocal(out=wsum, in_=wsum), r=["wsum"], w=["wsum"])
                ts("dve", Wr3[:, t, 0:NE], wsel, wsum[:, 0:1], 2.5, ALU.mult, ALU.mult, r=["wsel", "wsum"], w=["Wr"])
                ts("dve", Wr3[:, t, NE:NE + 1], onesf[:, 0:1], 1.0, None, ALU.mult, r=[kc_], w=["Wr"])
        if debug:
            wr_dbg = nc.dram_tensor("wr_dbg", [128, NTO * (NE + 1)], F32, kind="ExternalOutput").ap()
            dma("sp", wr_dbg, Wr[:, :], r=["Wr"], w=["wr_dbg"])
        settle()

        if stop <= 5:
            P.emit(stack)
            return nc
        NTH = TH // 128
        for hf in range(NH):
            A.reset()
            vTh = r3(A.bf16(KC * TH), KC)
            for c0 in range(0, KC, 4):
                dma("sp", vTh[:, c0:c0 + 4, :], r3(vT_s, KC)[:, c0:c0 + 4, hf * TH:(hf + 1) * TH], r=["vT_s"], w=["vTh"])
            if debug and hf == 0:
                vth0_dbg = nc.dram_tensor("vth0_dbg", [128, KC * TH], BF16, kind="ExternalOutput").ap()
                dma("sp", vth0_dbg, vTh.rearrange("p a b -> p (a b)"), r=["vTh"], w=["vth0_dbg"])
            accm = r3(A.f32(NTH * D), NTH)
            gT = r3(A.bf16(4 * TH), 4)
            s1 = [A.bf16(512) for _ in range(2)]
            w_lo = A.lo
            w1 = [r3(A.bf16(KC * FF), KC) for _ in range(2)]
            w3 = [r3(A.bf16(KC * FF), KC) for _ in range(2)]
            w2 = [r3(A.bf16(4 * D), 4)] * 2
            stg = [A.f32(1024) for _ in range(2)]
            stg_ctr = [0]
            pc = 0
            elist = [nexp] + list(range(nexp))
            for ei, e_ in enumerate(elist):
                wb = ei % 2
                for c in range(8):
                    for (wt, wsrc, wk) in ((w1[wb], w_e1, ("w1", wb)), (w3[wb], w_e3, ("w3", wb))):
                        b = stg_ctr[0] % 2; stg_ctr[0] += 1
                        dma("sp", r3(stg[b], 2), wsrc[e_, c * 256:(c + 1) * 256, :].rearrange("(a p) n -> p a n", p=128),
                            w=[("stg", b)])
                        cp("pool", wt[:, 2 * c:2 * c + 2, :], r3(stg[b], 2), r=[("stg", b)], w=[wk])
                for c in range(8):
                    fc_, hh_ = c // 2, c % 2
                    b = stg_ctr[0] % 2; stg_ctr[0] += 1
                    dma("sp", stg[b], w_e2[e_, fc_ * 128:(fc_ + 1) * 128, hh_ * 1024:(hh_ + 1) * 1024], w=[("stg", b)])
                    cp("pool", w2[wb][:, fc_, hh_ * 1024:(hh_ + 1) * 1024], stg[b], r=[("stg", b)], w=[("w2", 0)])
                for tb in range(TH // 512):
                    tbs = slice(tb * 512, (tb + 1) * 512)
                    for fc in range(4):
                        p1 = pc % 2; p3 = 2 + pc % 2; pc += 1
                        for kc in range(KC):
                            mm(ps[p1][:, :], w1[wb][:, kc, fc * 128:(fc + 1) * 128], vTh[:, kc, tbs], kc == 0, kc == KC - 1,
                               r=[("w1", wb), "vTh"], w=[("ps", p1)])
                        for kc in range(KC):
                            mm(ps[p3][:, :], w3[wb][:, kc, fc * 128:(fc + 1) * 128], vTh[:, kc, tbs], kc == 0, kc == KC - 1,
                               r=[("w3", wb), "vTh"], w=[("ps", p3)])
                        act(s1[p1], ps[p1][:, :], AF.Silu, r=[("ps", p1)], w=[("s1", p1)])
                        tt("dve", gT[:, fc, tbs], s1[p1], ps[p3][:, :], ALU.mult, r=[("s1", p1), ("ps", p3)], w=["gT"])
                for t in range(NTH):
                    tg = hf * NTH + t
                    for fb in range(4):
                        py = 4 + pc % 2; pc += 1
                        for fc in range(4):
                            mm(ps[py][:, :], gT[:, fc, t * 128:(t + 1) * 128], w2[wb][:, fc, fb * 512:(fb + 1) * 512], fc == 0, fc == 3,
                               r=["gT", ("w2", 0)], w=[("ps", py)])
                        o = accm[:, t, fb * 512:(fb + 1) * 512]
                        if ei == 0:
                            cp("dve", o, ps[py][:, :], r=[("ps", py)], w=["accm"])
                        else:
                            stt("dve", o, ps[py][:, :], Wr3[:, tg, e_:e_ + 1], o, ALU.mult, ALU.add,
                                r=[("ps", py), "accm", "Wr"], w=["accm"])
            if debug and hf == 0:
                for nm, src_, n_, ky in (("gT_dbg", gT.rearrange("p a b -> p (a b)"), 4 * TH, "gT"),
                                         ("w1a_dbg", w1[0].rearrange("p a b -> p (a b)"), KC * FF, ("w1", 0)),
                                         ("w1b_dbg", w1[1].rearrange("p a b -> p (a b)"), KC * FF, ("w1", 1)),
                                         ("w3a_dbg", w3[0].rearrange("p a b -> p (a b)"), KC * FF, ("w3", 0)),
                                         ("w3b_dbg", w3[1].rearrange("p a b -> p (a b)"), KC * FF, ("w3", 1)),
                                         ("w2_dbg", w2[0].rearrange("p a b -> p (a b)"), 4 * D, ("w2", 0))):
                    dd = nc.dram_tensor(nm, [128, n_], BF16, kind="ExternalOutput").ap()
                    dma("sp", dd, src_, r=[ky], w=[nm])
                vth_dbg = nc.dram_tensor("vth_dbg", [128, KC * TH], BF16, kind="ExternalOutput").ap()
                dma("sp", vth_dbg, vTh.rearrange("p a b -> p (a b)"), r=["vTh"], w=["vth_dbg"])
                acc_dbg = nc.dram_tensor("acc_dbg", [128, NTH * D], F32, kind="ExternalOutput").ap()
                dma("sp", acc_dbg, accm.rearrange("p a b -> p (a b)"), r=["accm"], w=["acc_dbg"])
            settle()
            A.reset(w_lo, ARENA_F)
            g2b = A.f32(D); l2g = A.f32(D); l2b = A.f32(D); h1t = A.f32(D); ot = [A.f32(D) for _ in range(2)]
            stats = A.f32(24); mv = A.f32(2); rs = A.f32(1)
            dma("sp", g2b, mod_s[7], r=[("mod_s", 7)], w=["bcf"])
            dma("sp", l2g, ln_bc[2], w=["bcf"])
            dma("sp", l2b, ln_bc[3], w=["bcf"])
            for t in range(NTH):
                tsl = slice(hf * TH + t * 128, hf * TH + (t + 1) * 128)
                o_ = ot[t % 2]; ok = ("ot", t % 2)
                dma("sp", h1t, h1_s[tsl, :], r=["h1_s"], w=["h1t"])
                tt("dve", o_, accm[:, t, :], g2b, ALU.mult, r=["accm", "bcf"], w=[ok])
                stt("dve", o_, h1t, float(ALPHA), o_, ALU.mult, ALU.add, r=["h1t", ok], w=[ok])
                layer_norm(o_, ok, stats, mv, rs)
                tt("dve", o_, o_, l2g, ALU.mult, r=[ok, "bcf"], w=[ok])
                tt("dve", o_, o_, l2b, ALU.add, r=[ok, "bcf"], w=[ok])
                dma("sp", out[tsl, :], o_, r=[ok], w=["out"])
            settle()

        P.emit(stack)
    return nc


def _prep(inputs, S, nexp=NE, cores=range(8)):
    TOK = S // 8
    f = lambda a: np.ascontiguousarray(np.asarray(a, dtype=np.float32))
    x = f(inputs["x"])[0]; c = f(inputs["c"])[0]; ctx = f(inputs["ctx"])[0]; cc = f(inputs["c_ctx"])
    cT = np.zeros((128, 32), np.float32)
    cT[:, 0::2] = c.reshape(16, 128).T
    cT[:, 1::2] = cc.reshape(16, 128).T
    bc = lambda v: np.ascontiguousarray(np.broadcast_to(f(v).reshape(1, -1), (128, f(v).size)))
    conv_w = f(inputs["conv_w"])[0]
    conv_wT = np.ascontiguousarray(conv_w.reshape(4, 8, 128).transpose(2, 1, 0).reshape(128, 32))
    conv_bT = np.ascontiguousarray(f(inputs["conv_b"])[0].reshape(8, 128).T)
    wa = f(inputs["rg_wa"])[0]; wx = f(inputs["rg_wx"])[0]
    rg_w = np.concatenate([wa.reshape(16, 128, 128), wx.reshape(16, 128, 128)], 0)
    rg_w = np.ascontiguousarray(rg_w.transpose(1, 0, 2).reshape(128, 32 * 128))
    vecT = lambda v: f(v)[0].reshape(16, 128).T
    rg_vec = np.ascontiguousarray(np.concatenate([vecT(inputs["rg_ba"]), vecT(inputs["rg_bx"]), vecT(inputs["rg_lam"])], 1))
    ln_bc = np.stack([bc(inputs["ln1_g"]), bc(inputs["ln1_b"]), bc(inputs["ln2_g"]), bc(inputs["ln2_b"])], 0)
    qk_g = np.ascontiguousarray(np.stack([f(inputs["q_norm"])[0], f(inputs["k_norm"])[0]], 1))
    half = 32
    inv_freq = (np.float32(10000.0) ** (-np.arange(half, dtype=np.float32) / np.float32(half))).astype(np.float32)
    t = np.arange(S)
    row = (t // 64).astype(np.float32); col = (t % 64).astype(np.float32)
    ang = np.zeros((128, S), np.float32)
    for d in range(128):
        pos = row if d < 64 else col
        ang[d] = pos * inv_freq[(d % 64) % half]
    cs_all = np.stack([np.cos(ang), np.sin(ang)], 0).astype(np.float32)
    Rm = np.zeros((128, 128), np.float32)
    for base in (0, 64):
        for i in range(32):
            Rm[base + i, base + i + 32] = -1.0
            Rm[base + 32 + i, base + i] = 1.0
    consts = np.stack([np.eye(128, dtype=np.float32), np.ones((128, 128), np.float32),
                       np.ascontiguousarray(Rm.T), np.zeros((128, 128), np.float32)], 0)
    w_e1 = np.concatenate([f(inputs["w_e1"])[0][:nexp], f(inputs["w_s1"])], 0)
    w_e3 = np.concatenate([f(inputs["w_e3"])[0][:nexp], f(inputs["w_s3"])], 0)
    w_e2 = np.concatenate([f(inputs["w_e2"])[0][:nexp], f(inputs["w_s2"])], 0)
    common = dict(
        x_all=x, ctx=ctx, cT=cT, w_mod=f(inputs["w_mod"])[0], b_mod_bc=bc(inputs["b_mod"]),
        w_in=f(inputs["w_in"])[0], qk_g=qk_g, conv_wT=conv_wT, conv_bT=conv_bT, rg_w=rg_w, rg_vec=rg_vec,
        w_out=f(inputs["w_out"])[0], ln_bc=ln_bc, w_router=f(inputs["w_router"])[0], eb_bc=bc(inputs["e_bias"]),
        w_e1=w_e1, w_e3=w_e3, w_e2=w_e2, cs_all=cs_all, consts=consts)
    maps = []
    for j in cores:
        m = dict(common)
        m["x_own"] = np.ascontiguousarray(x[j * TOK:(j + 1) * TOK])
        m["cs_own"] = np.ascontiguousarray(cs_all[:, :, j * TOK:(j + 1) * TOK])
        s = np.zeros((128, 8), np.float32); s[:, j] = 1.0
        m["sel"] = s
        maps.append(m)
    return maps


_NC_CACHE = {}


def kernel(**inputs):
    S = int(np.asarray(inputs["x"]).shape[1])
    if S not in _NC_CACHE:
        _NC_CACHE[S] = build(S)
    nc = _NC_CACHE[S]
    maps = _prep(inputs, S)
    res = run_bass_kernel_spmd(nc, maps, core_ids=list(range(8)))
    outs = [np.asarray(r["out"], dtype=np.float32) for r in res.results]
    return np.concatenate(outs, 0)[None, :, :]
```

```python
import numpy as np
import concourse.bass as bass
import concourse.mybir as mybir
from concourse.bass_utils import run_bass_kernel_spmd

F32 = mybir.dt.float32
BF16 = mybir.dt.bfloat16
ALU = mybir.AluOpType
AF = mybir.ActivationFunctionType
AX = mybir.AxisListType

D = 2048
KC = 16
NCTX = 256
NE = 64
FF = 512
ALPHA = 2.0 ** 0.25
EPS = 1e-6
ATT_SCALE = 128.0 ** -0.5
ARENA_F = 50176

TRACE_LOG = None
ENGS = ("pe", "act", "dve", "pool", "sp")
EPOCH = 12000
DPOOL = 8
QDEPTH = {"pool": 1}
QSLOTS = {"pool": 4}


class Prog:
    def __init__(self, nc):
        self.nc = nc
        self.ops = []
        self.lastw = {}
        self.rd_c = {}
        self.rd_d = {}
        self.barrier = set()
        self.eng_ops = {e: [] for e in ENGS}
        self.ncomp = {e: 0 for e in ENGS}
        self.ndma = {e: 0 for e in ENGS}
        self.dma_ids = {e: [] for e in ENGS}

    def add(self, eng, fn, r=(), w=(), dma=False):
        oid = len(self.ops)
        deps = set(self.barrier)
        for k in r:
            if k in self.lastw:
                deps.add(self.lastw[k])
        for k in w:
            if k in self.lastw:
                deps.add(self.lastw[k])
            deps.update(self.rd_c.get(k, {}).values())
            deps.update(self.rd_d.get(k, ()))
        for k in r:
            if dma:
                self.rd_d.setdefault(k, []).append(oid)
            else:
                self.rd_c.setdefault(k, {})[eng] = oid
        for k in w:
            self.lastw[k] = oid
            self.rd_c[k] = {}
            self.rd_d[k] = []
        op = dict(eng=eng, fn=fn, deps=deps, dma=dma, tag=(tuple(r), tuple(w)))
        if dma:
            i = self.ndma[eng]
            self.ndma[eng] += 1
            dp = QDEPTH.get(eng, DPOOL)
            ns = QSLOTS.get(eng, DPOOL)
            op["slot"] = i % ns
            op["target"] = 16 * (i // ns + 1)
            if i >= dp:
                deps.add(self.dma_ids[eng][i - dp])
            self.dma_ids[eng].append(oid)
        else:
            n = self.ncomp[eng]
            self.ncomp[eng] += 1
            op["epoch"] = n // EPOCH
            op["idx"] = n % EPOCH + 1
        self.ops.append(op)
        self.eng_ops[eng].append(oid)
        return oid

    def fence(self):
        b = set()
        for e in ENGS:
            if self.eng_ops[e]:
                comp = [o for o in self.eng_ops[e] if not self.ops[o]["dma"]]
                if comp:
                    b.add(comp[-1])
            b.update(self.dma_ids[e][-DPOOL:])
        self.barrier = b

    def emit(self, stack):
        nc = self.nc
        csem = {}
        dsem = {}
        for e in ENGS:
            for ep in range(self.ncomp[e] // EPOCH + 1):
                csem[(e, ep)] = stack.enter_context(nc.semaphore(f"c_{e}_{ep}"))
            if self.ndma[e]:
                for s in range(DPOOL):
                    dsem[(e, s)] = stack.enter_context(nc.semaphore(f"d_{e}_{s}"))
        block = stack.enter_context(nc.Block())
        ops = self.ops

        def run(engname, eng):
            waited = {}
            for oid in self.eng_ops[engname]:
                op = ops[oid]
                need = {}
                for d in op["deps"]:
                    p = ops[d]
                    if p["dma"]:
                        key = ("d", p["eng"], p["slot"])
                        val = p["target"]
                    else:
                        if p["eng"] == engname and engname == "pe":
                            continue
                        key = ("c", p["eng"], p["epoch"])
                        val = p["idx"]
                    if need.get(key, 0) < val:
                        need[key] = val
                for key, val in need.items():
                    if waited.get(key, 0) >= val:
                        continue
                    waited[key] = val
                    sem = dsem[(key[1], key[2])] if key[0] == "d" else csem[(key[1], key[2])]
                    eng.wait_ge(sem, val)
                    if TRACE_LOG is not None:
                        TRACE_LOG.append((engname, oid, "wait", key, val))
                if TRACE_LOG is not None:
                    TRACE_LOG.append((engname, oid, "op", op.get("tag"), (op.get("slot"), op.get("target")) if op["dma"] else (op["epoch"], op["idx"])))
                inst = op["fn"](eng)
                if op["dma"]:
                    inst.then_inc(dsem[(engname, op["slot"])], 16)
                else:
                    inst.then_inc(csem[(engname, op["epoch"])], 1)
            for oid in self.dma_ids[engname][-DPOOL:]:
                op = ops[oid]
                key = ("d", engname, op["slot"])
                if waited.get(key, 0) < op["target"]:
                    waited[key] = op["target"]
                    eng.wait_ge(dsem[(engname, op["slot"])], op["target"])

        @block.tensor
        def _(e):
            run("pe", e)

        @block.scalar
        def _(e):
            run("act", e)

        @block.vector
        def _(e):
            run("dve", e)

        @block.gpsimd
        def _(e):
            run("pool", e)

        @block.sync
        def _(e):
            run("sp", e)


class Arena:
    def __init__(self, ap):
        self.ap = ap
        self.lo = 0
        self.hi = ARENA_F

    def reset(self, lo=0, hi=ARENA_F):
        self.lo = lo
        self.hi = hi

    def f32(self, n0, top=False):
        n = (n0 + 7) // 8 * 8
        if top:
            self.hi -= n
            off = self.hi
        else:
            off = self.lo
            self.lo += n
        assert self.lo <= self.hi, ("arena overflow", self.lo, self.hi)
        return self.ap[:, off:off + n0]

    def bf16(self, n, top=False):
        nf = (n + 1) // 2
        a = self.f32(nf, top=top)
        return a.bitcast(BF16)[:, 0:n]


def r3(ap, a):
    return ap.rearrange("p (a b) -> p a b", a=a)


def build(S, stop=99, nexp=NE, debug=False, osub=9):
    TOK = S // 8
    NB = S // 512
    NBO = TOK // 512
    NTO = TOK // 128
    SA = NCTX + S
    NKC = SA // 128
    TH = min(TOK, 1024)
    NH = TOK // TH
    nc = bass.Bass("TRN2", target_bir_lowering=False)

    def din(name, shape, dt=F32):
        return nc.dram_tensor(name, list(shape), dt, kind="ExternalInput").ap()

    x_all = din("x_all", [S, D]); x_own = din("x_own", [TOK, D]); ctx = din("ctx", [NCTX, D])
    cT = din("cT", [128, 32])
    w_mod = din("w_mod", [D, 6 * D]); b_mod_bc = din("b_mod_bc", [128, 6 * D])
    w_in = din("w_in", [D, 3584])
    qk_g = din("qk_g", [128, 2])
    conv_wT = din("conv_wT", [128, 32]); conv_bT = din("conv_bT", [128, 8])
    rg_w = din("rg_w", [128, 32 * 128])
    rg_vec = din("rg_vec", [128, 48])
    w_out = din("w_out", [D, D])
    ln_bc = din("ln_bc", [4, 128, D])
    w_router = din("w_router", [D, NE]); eb_bc = din("eb_bc", [128, NE])
    w_e1 = din("w_e1", [nexp + 1, D, FF]); w_e3 = din("w_e3", [nexp + 1, D, FF]); w_e2 = din("w_e2", [nexp + 1, FF, D])
    cs_all = din("cs_all", [2, 128, S]); cs_own = din("cs_own", [2, 128, TOK])
    consts = din("consts", [4, 128, 128])
    sel_in = din("sel", [128, 8])
    out = nc.dram_tensor("out", [TOK, D], F32, kind="ExternalOutput").ap()

    skind = "ExternalOutput" if debug else "Internal"

    def dscr(name, shape, dt):
        return nc.dram_tensor(name, list(shape), dt, kind=skind).ap()

    kT_s = dscr("kT_s", [2, 128, SA], BF16)
    V_s = dscr("V_s", [SA, 256], BF16)
    XP = S + 8
    xr_s = dscr("xr_s", [1024, XP], F32)
    xrc_s = dscr("xrc_s", [1024, NCTX + 8], F32)
    gy_s = dscr("gy_s", [1024, TOK], F32)
    h1_s = dscr("h1_s", [TOK, D], F32)
    vT_s = dscr("vT_s", [128, KC * TOK], BF16)
    mod_s = dscr("mod_s", [8, 128, D], F32)
    mix_s = dscr("mix_s", [128, KC * TOK], BF16)

    import contextlib
    stack = contextlib.ExitStack()
    with stack:
        def sb(name, shape, dt=F32):
            return stack.enter_context(nc.sbuf_tensor("sb_" + name, list(shape), dt))

        FA = sb("arena", [128, ARENA_F])
        identf = sb("identf", [128, 128]); onesf = sb("onesf", [128, 128]); RTf = sb("RTf", [128, 128])
        zerof = sb("zerof", [128, 128])
        identb = sb("identb", [128, 128], BF16); onesb = sb("onesb", [128, 128], BF16)
        sT = sb("sT", [128, 32]); csT = sb("csT", [128, 32])
        qkg = sb("qkg", [128, 2]); qkg2 = sb("qkg2", [128, 2])
        cwT = sb("cwT", [128, 32]); cbT = sb("cbT", [128, 8])
        rgv = sb("rgv", [128, 48]); c8 = sb("c8", [128, 16]); c8t = sb("c8t", [128, 16])
        sel = sb("sel", [128, 8]); ebb = sb("ebb", [128, NE])
        Wr = sb("Wr", [128, NTO * (NE + 1)])
        ps = [stack.enter_context(nc.psum_tensor(f"ps{i}", [128, 512], F32)) for i in range(6)]
        ptb = [stack.enter_context(nc.psum_tensor(f"ptb{i}", [128, 1024], BF16)) for i in range(2)]

        P = Prog(nc)
        A = Arena(FA)
        uid = [0]

        def K(name):
            uid[0] += 1
            return (name, uid[0])

        def dma(q, out_ap, in_ap, r=(), w=(), **kw):
            P.add(q, lambda e: e.dma_start(out=out_ap, in_=in_ap, **kw), r=r, w=w, dma=True)

        def mm(o, lhsT, rhs, start, stop, r=(), w=()):
            P.add("pe", lambda e: e.matmul(o, lhsT, rhs, start=start, stop=stop), r=r, w=w)

        def tr(o, in_, ident, r=(), w=()):
            P.add("pe", lambda e: e.transpose(o, in_, ident), r=r, w=w)

        def act(o, in_, func, r=(), w=(), **kw):
            P.add("act", lambda e: e.activation(out=o, in_=in_, func=func, **kw), r=r, w=w)

        def tt(eng, o, a, b, op, r=(), w=()):
            P.add(eng, lambda e: e.tensor_tensor(out=o, in0=a, in1=b, op=op), r=r, w=w)

        def ts(eng, o, a, s1, s2, op0, op1=None, r=(), w=()):
            if op1 is None:
                P.add(eng, lambda e: e.tensor_scalar(out=o, in0=a, scalar1=s1, scalar2=None, op0=op0), r=r, w=w)
            else:
                P.add(eng, lambda e: e.tensor_scalar(out=o, in0=a, scalar1=s1, scalar2=s2, op0=op0, op1=op1), r=r, w=w)

        def stt(eng, o, a, s, b, op0, op1, r=(), w=()):
            P.add(eng, lambda e: e.scalar_tensor_tensor(out=o, in0=a, scalar=s, in1=b, op0=op0, op1=op1), r=r, w=w)

        def cp(eng, o, a, r=(), w=()):
            P.add(eng, lambda e: e.tensor_copy(out=o, in_=a), r=r, w=w)

        dly_t = sb("dly_t", [128, 512])

        def settle():
            P.fence()
            for _i in range(8):
                dma("sp", dly_t[:, :], x_own[0:128, 0:512], w=["dly_t"])
            P.fence()

        kc_ = "const"
        for i, t in enumerate((identf, onesf, RTf, zerof)):
            dma("sp", t[:, :], consts[i], w=[kc_])
        for src, dst in ((cT, csT), (qk_g, qkg), (conv_wT, cwT), (conv_bT, cbT), (rg_vec, rgv),
                         (sel_in, sel), (eb_bc, ebb)):
            dma("sp", dst[:, :], src, w=[kc_])
        cp("dve", identb[:, :], identf[:, :], r=[kc_], w=["identb"])
        cp("dve", onesb[:, :], onesf[:, :], r=[kc_], w=["onesb"])
        ts("dve", qkg2[:, :], qkg[:, :], float(128.0 ** 0.5), None, ALU.mult, r=[kc_], w=["qkg2"])
        act(sT[:, :], csT[:, :], AF.Silu, r=[kc_], w=["sT"])
        act(c8t[:, :], rgv[:, 32:48], AF.Exp, scale=-1.0, r=[kc_], w=["c8t"])
        act(c8[:, :], c8t[:, :], AF.Ln, bias=1.0, r=["c8t"], w=["c8"])
        ts("dve", c8[:, :], c8[:, :], -8.0, None, ALU.mult, r=["c8"], w=["c8"])
        for rr in range(8):
            rows = slice(rr * 128, (rr + 1) * 128)
            dma("sp", xr_s[rows, 0:2], zerof[:, 0:2], r=[kc_], w=["xr_s"])
            dma("sp", xr_s[rows, S + 2:S + 8], zerof[:, 0:6], r=[kc_], w=["xr_s"])
            dma("sp", xrc_s[rows, 0:2], zerof[:, 0:2], r=[kc_], w=["xrc_s"])
            dma("sp", xrc_s[rows, NCTX + 2:NCTX + 8], zerof[:, 0:6], r=[kc_], w=["xrc_s"])

        A.reset()
        sbc = A.f32(KC * 2 * 128)
        sbc4 = sbc.rearrange("p (a v m) -> p a v m", a=KC, v=2)
        for kc in range(KC):
            for v in range(2):
                ts("dve", sbc4[:, kc, v, :], onesf[:, :], sT[:, 2 * kc + v:2 * kc + v + 1], None, ALU.mult,
                   r=["sT", kc_], w=["sbc"])
        wm = [r3(A.f32(KC * 512), KC) for _ in range(2)]
        bmb = [A.f32(512) for _ in range(2)]
        mo = [A.f32(512) for _ in range(2)]
        MODV = {(0, 0): 0, (1, 0): 1, (0, 1): 2, (1, 1): 3, (2, 0): 4, (3, 0): 5, (4, 0): 6, (5, 0): 7}
        cnt = 0
        for nb in range(24):
            sec = nb // 4
            buf = nb % 2
            col = slice(nb * 512, (nb + 1) * 512)
            wsrc = w_mod[:, col].rearrange("(a p) n -> p a n", p=128)
            for c0 in range(0, KC, 4):
                dma("sp", wm[buf][:, c0:c0 + 4, :], wsrc[:, c0:c0 + 4, :], w=[("wm", buf)])
            dma("sp", bmb[buf], b_mod_bc[:, col], w=[("bmb", buf)])
            for v in ((0, 1) if sec < 2 else (0,)):
                pt = ps[cnt % 2]; pk = ("ps", cnt % 2)
                for kc in range(KC):
                    mm(pt[:, :], sbc4[:, kc, v, :], wm[buf][:, kc, :], kc == 0, kc == KC - 1,
                       r=[("wm", buf), "sbc"], w=[pk])
                mb = mo[cnt % 2]; mk = ("mo", cnt % 2)
                tt("dve", mb, pt[:, :], bmb[buf], ALU.add, r=[pk, ("bmb", buf)], w=[mk])
                if sec in (1, 4):
                    ts("dve", mb, mb, 1.0, None, ALU.add, r=[mk], w=[mk])
                idx = MODV[(sec, v)]
                dma("sp", mod_s[idx][:, (nb % 4) * 512:(nb % 4 + 1) * 512], mb, r=[mk], w=[("mod_s", idx)])
                cnt += 1
        settle()

        def norm_rope(praw, n, gcol, lat, cosb, sinb, kout, tmp, rkeys, wkeys):
            sq, t0, kn, t1 = tmp
            act(sq[:, :n], praw[:, :n], AF.Square, r=rkeys, w=["nr_sq"])
            mm(ps[1][:, :n], onesf[:, :], sq[:, :n], True, True, r=["nr_sq", kc_], w=[("ps", 1)])
            act(t0[:, :n], ps[1][:, :n], AF.Sqrt, bias=float(128 * EPS), r=[("ps", 1)], w=["nr_t0"])
            P.add("dve", lambda e: e.reciprocal(out=t0[:, :n], in_=t0[:, :n]), r=["nr_t0"], w=["nr_t0"])
            stt("dve", kn[:, :n], praw[:, :n], qkg2[:, gcol:gcol + 1], t0[:, :n], ALU.mult, ALU.mult,
                r=rkeys + ["nr_t0", "qkg2"], w=["nr_kn"])
            if lat:
                mm(ps[1][:, :n], RTf[:, :], kn[:, :n], True, True, r=["nr_kn", kc_], w=[("ps", 1)])
                tt("dve", t1[:, :n], kn[:, :n], cosb[:, :n], ALU.mult, r=["nr_kn", "cs"], w=["nr_t1"])
                tt("dve", sq[:, :n], ps[1][:, :n], sinb[:, :n], ALU.mult, r=[("ps", 1), "cs"], w=["nr_sq"])
                tt("dve", kout, t1[:, :n], sq[:, :n], ALU.add, r=["nr_t1", "nr_sq"], w=wkeys)
            else:
                cp("dve", kout, kn[:, :n], r=["nr_kn"], w=wkeys)

        def proj_pass(blocks, scb, shb, consumer):
            xt = [A.f32(D) for _ in range(2)]
            ub = [A.bf16(D) for _ in range(2)]
            uT = [r3(A.bf16(KC * 512), KC) for _ in range(2)]
            tcount = 0
            for bi, (src, ntl, isc, info) in enumerate(blocks):
                ubuf = bi % 2
                for t in range(ntl):
                    b = tcount % 2
                    tcount += 1
                    dma("sp", xt[b], src[t * 128:(t + 1) * 128, :], w=[("xt", b)])
                    tt("dve", xt[b], xt[b], scb[isc], ALU.mult, r=[("xt", b), "bcmod"], w=[("xt", b)])
                    tt("dve", ub[b], xt[b], shb[isc], ALU.add, r=[("xt", b), "bcmod"], w=[("ub", b)])
                    for hh in range(2):
                        for c in range(8):
                            kc = hh * 8 + c
                            tr(ptb[hh][:, c * 128:(c + 1) * 128], ub[b][:, kc * 128:(kc + 1) * 128], identb[:, :],
                               r=[("ub", b), "identb"], w=[("ptb", hh)])
                        act(uT[ubuf][:, hh * 8:(hh + 1) * 8, t * 128:(t + 1) * 128],
                            r3(ptb[hh][:, :], 8), AF.Copy, r=[("ptb", hh)], w=[("uT", ubuf)])
                consumer(uT[ubuf], ("uT", ubuf), ntl, isc, info)

        if stop <= 0:
            P.emit(stack)
            return nc
        A.reset()
        scb = [A.f32(D, top=True), A.f32(D, top=True)]
        shb = [A.f32(D, top=True), A.f32(D, top=True)]
        dma("sp", scb[0], mod_s[1], r=[("mod_s", 1)], w=["bcmod"])
        dma("sp", shb[0], mod_s[0], r=[("mod_s", 0)], w=["bcmod"])
        dma("sp", scb[1], mod_s[3], r=[("mod_s", 3)], w=["bcmod"])
        dma("sp", shb[1], mod_s[2], r=[("mod_s", 2)], w=["bcmod"])
        wA = r3(A.bf16(KC * 1536), KC)
        for kc in range(KC):
            dma("pool", wA[:, kc, :], w_in[kc * 128:(kc + 1) * 128, 1024:2560], w=["wA"])
        cosb = A.f32(512); sinb = A.f32(512)
        nrt = [A.f32(512) for _ in range(4)]
        kob = [A.bf16(512) for _ in range(2)]
        vb = [A.bf16(256) for _ in range(2)]
        xst = [A.f32(512) for _ in range(2)]
        ctrA = [0, 0, 0]

        def consA(uTb, uk, ntl, isc, info):
            n = ntl * 128
            t0 = info
            if not isc:
                dma("sp", cosb[:, :n], cs_all[0][:, t0 - NCTX:t0 - NCTX + n], w=["cs"])
                dma("sp", sinb[:, :n], cs_all[1][:, t0 - NCTX:t0 - NCTX + n], w=["cs"])
            for h in range(2):
                for kc in range(KC):
                    mm(ps[0][:, :n], wA[:, kc, h * 128:(h + 1) * 128], uTb[:, kc, :n], kc == 0, kc == KC - 1,
                       r=[uk, "wA"], w=[("ps", 0)])
                ko = kob[ctrA[0] % 2]; kk = ("kob", ctrA[0] % 2); ctrA[0] += 1
                norm_rope(ps[0], n, 1, not isc, cosb, sinb, ko[:, :n], nrt, [("ps", 0)], [kk])
                dma("sp", kT_s[h][:, t0:t0 + n], ko[:, :n], r=[kk], w=["kT_s"])
            for t in range(ntl):
                pi = 2 + ctrA[1] % 2
                for kc in range(KC):
                    mm(ps[pi][:, 0:256], uTb[:, kc, t * 128:(t + 1) * 128], wA[:, kc, 256:512], kc == 0, kc == KC - 1,
                       r=[uk, "wA"], w=[("ps", pi)])
                v_ = vb[ctrA[1] % 2]; vk = ("vb", ctrA[1] % 2); ctrA[1] += 1
                act(v_, ps[pi][:, 0:256], AF.Copy, r=[("ps", pi)], w=[vk])
                dma("sp", V_s[t0 + t * 128:t0 + (t + 1) * 128, :], v_, r=[vk], w=["V_s"])
            for ct in range(8):
                pi = 4 + ctrA[2] % 2
                for kc in range(KC):
                    mm(ps[pi][:, :n], wA[:, kc, 512 + ct * 128:512 + (ct + 1) * 128], uTb[:, kc, :n], kc == 0, kc == KC - 1,
                       r=[uk, "wA"], w=[("ps", pi)])
                xs = xst[ctrA[2] % 2]; xk = ("xst", ctrA[2] % 2); ctrA[2] += 1
                act(xs[:, :n], ps[pi][:, :n], AF.Copy, r=[("ps", pi)], w=[xk])
                rows = slice(ct * 128, (ct + 1) * 128)
                if isc:
                    dma("sp", xrc_s[rows, 2:2 + n], xs[:, :n], r=[xk], w=["xrc_s"])
                else:
                    tl = t0 - NCTX
                    dma("sp", xr_s[rows, 2 + tl:2 + tl + n], xs[:, :n], r=[xk], w=["xr_s"])

        blocksA = [(ctx, 2, 1, 0)] + [(x_all[b * 512:(b + 1) * 512, :], 4, 0, NCTX + b * 512) for b in range(NB)]
        proj_pass(blocksA, scb, shb, consA)
        settle()

        if stop <= 1:
            P.emit(stack)
            return nc
        A.reset()
        qT = r3(A.bf16(8 * TOK, top=True), 8)
        q_hi = A.hi
        scb = [A.f32(D)]; shb = [A.f32(D)]
        dma("sp", scb[0], mod_s[1], r=[("mod_s", 1)], w=["bcmod"])
        dma("sp", shb[0], mod_s[0], r=[("mod_s", 0)], w=["bcmod"])
        wQ = r3(A.bf16(KC * 2048), KC)
        for kc in range(KC):
            dma("pool", wQ[:, kc, 0:1024], w_in[kc * 128:(kc + 1) * 128, 0:1024], w=["wQ"])
            dma("pool", wQ[:, kc, 1024:2048], w_in[kc * 128:(kc + 1) * 128, 2560:3584], w=["wQ"])
        cosb = A.f32(512); sinb = A.f32(512)
        nrt = [A.f32(512) for _ in range(4)]
        gyb = [A.f32(512) for _ in range(2)]
        ctrQ = [0]

        def consQ(uTb, uk, ntl, isc, info):
            n = ntl * 128
            t0 = info
            dma("sp", cosb[:, :n], cs_own[0][:, t0:t0 + n], w=["cs"])
            dma("sp", sinb[:, :n], cs_own[1][:, t0:t0 + n], w=["cs"])
            for h in range(8):
                for kc in range(KC):
                    mm(ps[0][:, :n], wQ[:, kc, h * 128:(h + 1) * 128], uTb[:, kc, :n], kc == 0, kc == KC - 1,
                       r=[uk, "wQ"], w=[("ps", 0)])
                norm_rope(ps[0], n, 0, True, cosb, sinb, qT[:, h, t0:t0 + n], nrt, [("ps", 0)], ["qT"])
            for ct in range(8):
                pi = 4 + ctrQ[0] % 2
                for kc in range(KC):
                    mm(ps[pi][:, :n], wQ[:, kc, 1024 + ct * 128:1024 + (ct + 1) * 128], uTb[:, kc, :n], kc == 0, kc == KC - 1,
                       r=[uk, "wQ"], w=[("ps", pi)])
                g_ = gyb[ctrQ[0] % 2]; gk = ("gyb", ctrQ[0] % 2); ctrQ[0] += 1
                act(g_[:, :n], ps[pi][:, :n], AF.Gelu, r=[("ps", pi)], w=[gk])
                dma("sp", gy_s[ct * 128:(ct + 1) * 128, t0:t0 + n], g_[:, :n], r=[gk], w=["gy_s"])

        blocksQ = [(x_own[b * 512:(b + 1) * 512, :], 4, 0, b * 512) for b in range(NBO)]
        proj_pass(blocksQ, scb, shb, consQ)
        settle()

        if stop <= 2:
            P.emit(stack)
            return nc
        A.reset(0, q_hi)
        mixT = r3(A.bf16(KC * TOK), KC)
        mix_lo = A.lo
        kTg = A.bf16(SA)
        Vg = r3(A.bf16(NKC * 128), NKC)
        pT = [A.bf16(512) for _ in range(3)]
        rec = A.f32(512)
        it = 0
        hq = 0
        for g in range(2):
            dma("sp", kTg, kT_s[g], r=["kT_s"], w=["kTg"])
            Vsrc = V_s[:, g * 128:(g + 1) * 128].rearrange("(n p) d -> p n d", p=128)
            for c0 in range(0, NKC, 4):
                c1 = min(c0 + 4, NKC)
                dma("sp", Vg[:, c0:c1, :], Vsrc[:, c0:c1, :], r=["V_s"], w=["Vg"])
            for h in range(4 * g, 4 * g + 4):
                for qb in range(NBO):
                    qs = slice(qb * 512, (qb + 1) * 512)
                    po = 2 + 2 * (hq % 2); pl = 3 + 2 * (hq % 2); hq += 1

                    def s_mm(kc, it0):
                        si = (it0 + kc) % 2
                        mm(ps[si][:, :], kTg[:, kc * 128:(kc + 1) * 128], qT[:, h, qs], True, True,
                           r=["kTg", "qT"], w=[("ps", si)])

                    s_mm(0, it)
                    for kc in range(NKC):
                        si = (it + kc) % 2; pi = (it + kc) % 3
                        if kc + 1 < NKC:
                            s_mm(kc + 1, it)
                        act(pT[pi], ps[si][:, :], AF.Exp, scale=float(ATT_SCALE), r=[("ps", si)], w=[("pT", pi)])
                        mm(ps[po][:, :], Vg[:, kc, :], pT[pi], kc == 0, kc == NKC - 1, r=["Vg", ("pT", pi)], w=[("ps", po)])
                        mm(ps[pl][:, :], onesb[:, :], pT[pi], kc == 0, kc == NKC - 1, r=["onesb", ("pT", pi)], w=[("ps", pl)])
                    it += NKC
                    P.add("dve", lambda e, pl=pl: e.reciprocal(out=rec, in_=ps[pl][:, :]), r=[("ps", pl)], w=["rec"])
                    tt("dve", mixT[:, h, qs], ps[po][:, :], rec, ALU.mult, r=[("ps", po), "rec"], w=["mixT"])
        settle()

        if stop <= 3:
            P.emit(stack)
            return nc
        A.reset(mix_lo, ARENA_F)
        rgw = r3(A.f32(4 * 128), 4)
        rg_w3 = rg_w.rearrange("p (a j) -> p a j", a=32)
        xc = A.f32(NCTX + S)
        xin = [A.f32(520) for _ in range(2)]
        acc = A.f32(TOK)
        gy = A.f32(TOK)
        rt = [A.f32(512) for _ in range(2)]
        itl = [A.f32(512) for _ in range(2)]
        at = [A.f32(512) for _ in range(2)]
        a2 = [A.f32(512) for _ in range(2)]
        bt = [A.f32(512) for _ in range(2)]
        hb = [A.f32(512) for _ in range(3)]
        ci = 0
        gi = 0
        hi_ = 0
        for ct in range(8):
            rows = slice(ct * 128, (ct + 1) * 128)
            dma("sp", gy, gy_s[rows, :], r=["gy_s"], w=["gy"])
            for q4 in range(4):
                dma("sp", rgw[:, q4, :], rg_w3[:, q4 * 8 + ct, :], w=["rgw"])
            P.add("dve", lambda e: e.memset(acc, 0.0), w=["acc"])
            segs = [(xrc_s, 0, NCTX, 0)] + [(xr_s, b * 512, 512, NCTX + b * 512) for b in range(NB)]
            for (srct, c0, n, xo) in segs:
                xi = xin[ci % 2]; xk = ("xin", ci % 2); ci += 1
                dma("sp", xi[:, :n + 3], srct[rows, c0:c0 + n + 3], r=["xr_s", "xrc_s"], w=[xk])
                o = xc[:, xo:xo + n]
                ts("dve", o, xi[:, 0:n], cwT[:, ct * 4:ct * 4 + 1], cbT[:, ct:ct + 1], ALU.mult, ALU.add,
                   r=[xk, kc_], w=["xc"])
                for j in range(1, 4):
                    stt("dve", o, xi[:, j:j + n], cwT[:, ct * 4 + j:ct * 4 + j + 1], o, ALU.mult, ALU.add,
                        r=[xk, "xc", kc_], w=["xc"])
            for d in range(2):
                wi = d * 8 + ct
                order = [(0, NCTX, None)] + [(NCTX + b * 512, 512, b) for b in range(NB)]
                if d == 1:
                    order = [(0, NCTX, None)] + [(NCTX + b * 512, 512, b) for b in reversed(range(NB))]
                prev = None
                for p0 in range(0, len(order), 2):
                    pair = order[p0:p0 + 2]
                    for gb, (xo, n, b) in enumerate(pair):
                        xs = xc[:, xo:xo + n]
                        mm(ps[gb][:, :n], rgw[:, d, :], xs, True, True, r=["rgw", "xc"], w=[("ps", gb)])
                        mm(ps[2 + gb][:, :n], rgw[:, 2 + d, :], xs, True, True, r=["rgw", "xc"], w=[("ps", 2 + gb)])
                    for gb, (xo, n, b) in enumerate(pair):
                        act(rt[gb][:, :n], ps[gb][:, :n], AF.Sigmoid, bias=rgv[:, wi:wi + 1], r=[("ps", gb), kc_], w=[("rt", gb)])
                        act(itl[gb][:, :n], ps[2 + gb][:, :n], AF.Sigmoid, bias=rgv[:, 16 + wi:16 + wi + 1], r=[("ps", 2 + gb), kc_], w=[("it", gb)])
                    for gb, (xo, n, b) in enumerate(pair):
                        act(at[gb][:, :n], rt[gb][:, :n], AF.Exp, scale=c8[:, wi:wi + 1], r=[("rt", gb), "c8"], w=[("at", gb)])
                    for gb, (xo, n, b) in enumerate(pair):
                        tt("dve", a2[gb][:, :n], at[gb][:, :n], at[gb][:, :n], ALU.mult, r=[("at", gb)], w=[("a2", gb)])
                    for gb, (xo, n, b) in enumerate(pair):
                        act(a2[gb][:, :n], a2[gb][:, :n], AF.Sqrt, scale=-1.0, bias=1.0, r=[("a2", gb)], w=[("a2", gb)])
                    for gb, (xo, n, b) in enumerate(pair):
                        xs = xc[:, xo:xo + n]
                        a_ = at[gb]; b_ = bt[gb]
                        tt("dve", b_[:, :n], a2[gb][:, :n], itl[gb][:, :n], ALU.mult, r=[("a2", gb), ("it", gb)], w=[("bt", gb)])
                        tt("dve", b_[:, :n], b_[:, :n], xs, ALU.mult, r=[("bt", gb), "xc"], w=[("bt", gb)])
                        h_ = hb[hi_ % 3]; hk = ("hb", hi_ % 3); hi_ += 1
                        init = 0.0 if prev is None else prev[0]
                        rk = [("at", gb), ("bt", gb)] + ([] if prev is None else [prev[1]])
                        if d == 0:
                            P.add("dve", lambda e, h_=h_, a_=a_, b_=b_, n=n, init=init: e.tensor_tensor_scan(
                                out=h_[:, :n], data0=a_[:, :n], data1=b_[:, :n], initial=init, op0=ALU.mult, op1=ALU.add),
                                r=rk, w=[hk])
                            prev = (h_[:, n - 1:n], hk)
                        else:
                            P.add("dve", lambda e, h_=h_, a_=a_, b_=b_, n=n, init=init: e.tensor_tensor_scan(
                                out=h_[:, 0:n][:, ::-1], data0=a_[:, 0:n][:, ::-1], data1=b_[:, 0:n][:, ::-1],
                                initial=init, op0=ALU.mult, op1=ALU.add), r=rk, w=[hk])
                            prev = (h_[:, 0:1], hk)
                        if b is not None:
                            c = b // NBO
                            po = (b % NBO) * 512
                            stt("dve", acc[:, po:po + 512], h_[:, :n], sel[:, c:c + 1], acc[:, po:po + 512], ALU.mult, ALU.add,
                                r=[hk, "acc", kc_], w=["acc"])
            tt("dve", mixT[:, 8 + ct, :], acc, gy, ALU.mult, r=["acc", "gy"], w=["mixT"])
        for c0 in range(0, KC, 4):
            dma("sp", r3(mix_s, KC)[:, c0:c0 + 4, :], mixT[:, c0:c0 + 4, :], r=["mixT"], w=["mix_s"])
        settle()

        if stop <= 4:
            P.emit(stack)
            return nc
        A.reset()
        mtb = [r3(A.bf16(KC * 128), KC) for _ in range(2)]
        wo = r3(A.bf16(KC * D), KC)
        for kc in range(KC):
            dma("pool", wo[:, kc, :], w_out[kc * 128:(kc + 1) * 128, :], w=["wo"])
        g1b = A.f32(D); l1g = A.f32(D); l1b = A.f32(D); s2b = A.f32(D); h2b = A.f32(D)
        dma("sp", g1b, mod_s[4], r=[("mod_s", 4)], w=["bco"])
        dma("sp", l1g, ln_bc[0], w=["bco"])
        dma("sp", l1b, ln_bc[1], w=["bco"])
        dma("sp", s2b, mod_s[6], r=[("mod_s", 6)], w=["bco"])
        dma("sp", h2b, mod_s[5], r=[("mod_s", 5)], w=["bco"])
        wr = r3(A.f32(KC * NE), KC)
        wrs = w_router.rearrange("(a p) n -> p a n", p=128)
        for c0 in range(0, KC, 4):
            dma("sp", wr[:, c0:c0 + 4, :], wrs[:, c0:c0 + 4, :], w=["wr"])
        xo_ = A.f32(D); zt = A.f32(D); h1t = A.f32(D)
        vTb = r3(A.bf16(KC * 128), KC); vT32 = r3(A.f32(KC * 128), KC)
        stats = A.f32(24); mv = A.f32(2); rs = A.f32(1)
        sc_ = A.f32(NE); bi_ = A.f32(NE); m8 = A.f32(64); gs = A.f32(8); g8 = A.f32(8); pen = A.f32(8)
        mk_ = A.f32(NE); t8 = A.f32(8); wsel = A.f32(NE); wsum = A.f32(1)
        Wr3 = r3(Wr[:, :], NTO)

        def layer_norm(z, key, stats, mv, rs):
            for c4 in range(4):
                P.add("dve", lambda e, c4=c4: e.bn_stats(out=stats[:, c4 * 6:(c4 + 1) * 6], in_=z[:, c4 * 512:(c4 + 1) * 512]),
                      r=[key], w=["stats"])
            P.add("dve", lambda e: e.bn_aggr(out=mv, in_=stats), r=["stats"], w=["mv"])
            act(rs, mv[:, 1:2], AF.Sqrt, bias=float(EPS), r=["mv"], w=["rs"])
            P.add("dve", lambda e: e.reciprocal(out=rs, in_=rs), r=["rs"], w=["rs"])
            ts("dve", z, z, mv[:, 0:1], rs[:, 0:1], ALU.subtract, ALU.mult, r=[key, "mv", "rs"], w=[key])

        for t in range(NTO):
            tsl = slice(t * 128, (t + 1) * 128)
            dma("sp", xo_, x_own[tsl, :], w=["xo"])
            mt = mtb[t % 2]; mtk = ("mt", t % 2)
            for c0 in range(0, KC, 8):
                dma("sp", mt[:, c0:c0 + 8, :], r3(mix_s, KC)[:, c0:c0 + 8, tsl], r=["mix_s"], w=[mtk])
            for fb in range(4):
                pi = fb
                for kc in range(KC):
                    mm(ps[pi][:, :], mt[:, kc, :], wo[:, kc, fb * 512:(fb + 1) * 512], kc == 0, kc == KC - 1,
                       r=[mtk, "wo"], w=[("ps", pi)])
                tt("dve", zt[:, fb * 512:(fb + 1) * 512], ps[pi][:, :], g1b[:, fb * 512:(fb + 1) * 512], ALU.mult,
                   r=[("ps", pi), "bco"], w=["zt"])
            stt("dve", zt, xo_, float(ALPHA), zt, ALU.mult, ALU.add, r=["xo", "zt"], w=["zt"])
            layer_norm(zt, "zt", stats, mv, rs)
            tt("dve", h1t, zt, l1g, ALU.mult, r=["zt", "bco"], w=["h1t"])
            tt("dve", h1t, h1t, l1b, ALU.add, r=["h1t", "bco"], w=["h1t"])
            dma("sp", h1_s[tsl, :], h1t, r=["h1t"], w=["h1_s"])
            if osub >= 2:
                tt("dve", zt, h1t, s2b, ALU.mult, r=["h1t", "bco"], w=["zt"])
                tt("dve", zt, zt, h2b, ALU.add, r=["zt", "bco"], w=["zt"])
                for q4 in range(4):
                    pi = q4
                    for c in range(4):
                        kc = q4 * 4 + c
                        mm(ps[pi][:, c * 128:(c + 1) * 128], zt[:, kc * 128:(kc + 1) * 128], identf[:, :], True, True, r=["zt", kc_], w=[("ps", pi)])
                    cp("dve", vT32[:, q4 * 4:(q4 + 1) * 4, :], r3(ps[pi][:, :], 4), r=[("ps", pi)], w=["vT32"])
                    act(vTb[:, q4 * 4:(q4 + 1) * 4, :], vT32[:, q4 * 4:(q4 + 1) * 4, :], AF.Copy, r=["vT32"], w=["vTb"])
                for c0 in range(0, KC, 8):
                    dma("sp", r3(vT_s, KC)[:, c0:c0 + 8, tsl], vTb[:, c0:c0 + 8, :], r=["vTb"], w=["vT_s"])
            if osub >= 3:
                for kc in range(KC):
                    mm(ps[4][:, 0:NE], vT32[:, kc, :], wr[:, kc, :], kc == 0, kc == KC - 1, r=["vT32", "wr"], w=[("ps", 4)])
                act(sc_, ps[4][:, 0:NE], AF.Sigmoid, r=[("ps", 4)], w=["sc"])
            if osub >= 4:
                tt("dve", bi_, sc_, ebb[:, :], ALU.add, r=["sc", kc_], w=["bi"])
                for g in range(8):
                    P.add("dve", lambda e, g=g: e.max(out=m8[:, g * 8:(g + 1) * 8], in_=bi_[:, g * 8:(g + 1) * 8]), r=["bi"], w=["m8"])
                m83 = r3(m8, 8)
                tt("dve", gs.rearrange("p (a b) -> p a b", b=1), m83[:, :, 0:1], m83[:, :, 1:2], ALU.add, r=["m8"], w=["gs"])
                P.add("dve", lambda e: e.max(out=g8, in_=gs), r=["gs"], w=["g8"])
                ts("dve", pen, gs, g8[:, 3:4], None, ALU.is_ge, r=["gs", "g8"], w=["pen"])
                ts("dve", pen, pen, -1.0, 1.0e9, ALU.add, ALU.mult, r=["pen"], w=["pen"])
                for g in range(8):
                    ts("dve", mk_[:, g * 8:(g + 1) * 8], bi_[:, g * 8:(g + 1) * 8], pen[:, g:g + 1], None, ALU.add,
                       r=["bi", "pen"], w=["mk"])
                P.add("dve", lambda e: e.max(out=t8, in_=mk_), r=["mk"], w=["t8"])
                ts("dve", wsel, mk_, t8[:, 7:8], None, ALU.is_ge, r=["mk", "t8"], w=["wsel"])
                tt("dve", wsel, wsel, sc_, ALU.mult, r=["wsel", "sc"], w=["wsel"])
                P.add("dve", lambda e: e.reduce_sum(out=wsum, in_=wsel, axis=AX.X), r=["wsel"], w=["wsum"])
                P.add("dve", lambda e: e.reciprocal(out=wsum, in_=wsum), r=["wsum"], w=["wsum"])
                ts("dve", Wr3[:, t, 0:NE], wsel, wsum[:, 0:1], 2.5, ALU.mult, ALU.mult, r=["wsel", "wsum"], w=["Wr"])
                ts("dve", Wr3[:, t, NE:NE + 1], onesf[:, 0:1], 1.0, None, ALU.mult, r=[kc_], w=["Wr"])
        if debug:
            wr_dbg = nc.dram_tensor("wr_dbg", [128, NTO * (NE + 1)], F32, kind="ExternalOutput").ap()
            dma("sp", wr_dbg, Wr[:, :], r=["Wr"], w=["wr_dbg"])
        settle()

        if stop <= 5:
            P.emit(stack)
            return nc
        NTH = TH // 128
        for hf in range(NH):
            A.reset()
            vTh = r3(A.bf16(KC * TH), KC)
            for c0 in range(0, KC, 4):
                dma("sp", vTh[:, c0:c0 + 4, :], r3(vT_s, KC)[:, c0:c0 + 4, hf * TH:(hf + 1) * TH], r=["vT_s"], w=["vTh"])
            if debug and hf == 0:
                vth0_dbg = nc.dram_tensor("vth0_dbg", [128, KC * TH], BF16, kind="ExternalOutput").ap()
                dma("sp", vth0_dbg, vTh.rearrange("p a b -> p (a b)"), r=["vTh"], w=["vth0_dbg"])
            accm = r3(A.f32(NTH * D), NTH)
            gT = r3(A.bf16(4 * TH), 4)
            s1 = [A.bf16(512) for _ in range(2)]
            w_lo = A.lo
            w1 = [r3(A.bf16(KC * FF), KC) for _ in range(2)]
            w3 = [r3(A.bf16(KC * FF), KC) for _ in range(2)]
            w2 = [r3(A.bf16(4 * D), 4)] * 2
            stg = [A.f32(1024) for _ in range(2)]
            stg_ctr = [0]
            pc = 0
            elist = [nexp] + list(range(nexp))
            for ei, e_ in enumerate(elist):
                wb = ei % 2
                for c in range(8):
                    for (wt, wsrc, wk) in ((w1[wb], w_e1, ("w1", wb)), (w3[wb], w_e3, ("w3", wb))):
                        b = stg_ctr[0] % 2; stg_ctr[0] += 1
                        dma("sp", r3(stg[b], 2), wsrc[e_, c * 256:(c + 1) * 256, :].rearrange("(a p) n -> p a n", p=128),
                            w=[("stg", b)])
                        cp("pool", wt[:, 2 * c:2 * c + 2, :], r3(stg[b], 2), r=[("stg", b)], w=[wk])
                for c in range(8):
                    fc_, hh_ = c // 2, c % 2
                    b = stg_ctr[0] % 2; stg_ctr[0] += 1
                    dma("sp", stg[b], w_e2[e_, fc_ * 128:(fc_ + 1) * 128, hh_ * 1024:(hh_ + 1) * 1024], w=[("stg", b)])
                    cp("pool", w2[wb][:, fc_, hh_ * 1024:(hh_ + 1) * 1024], stg[b], r=[("stg", b)], w=[("w2", 0)])
                for tb in range(TH // 512):
                    tbs = slice(tb * 512, (tb + 1) * 512)
                    for fc in range(4):
                        p1 = pc % 2; p3 = 2 + pc % 2; pc += 1
                        for kc in range(KC):
                            mm(ps[p1][:, :], w1[wb][:, kc, fc * 128:(fc + 1) * 128], vTh[:, kc, tbs], kc == 0, kc == KC - 1,
                               r=[("w1", wb), "vTh"], w=[("ps", p1)])
                        for kc in range(KC):
                            mm(ps[p3][:, :], w3[wb][:, kc, fc * 128:(fc + 1) * 128], vTh[:, kc, tbs], kc == 0, kc == KC - 1,
                               r=[("w3", wb), "vTh"], w=[("ps", p3)])
                        act(s1[p1], ps[p1][:, :], AF.Silu, r=[("ps", p1)], w=[("s1", p1)])
                        tt("dve", gT[:, fc, tbs], s1[p1], ps[p3][:, :], ALU.mult, r=[("s1", p1), ("ps", p3)], w=["gT"])
                for t in range(NTH):
                    tg = hf * NTH + t
                    for fb in range(4):
                        py = 4 + pc % 2; pc += 1
                        for fc in range(4):
                            mm(ps[py][:, :], gT[:, fc, t * 128:(t + 1) * 128], w2[wb][:, fc, fb * 512:(fb + 1) * 512], fc == 0, fc == 3,
                               r=["gT", ("w2", 0)], w=[("ps", py)])
                        o = accm[:, t, fb * 512:(fb + 1) * 512]
                        if ei == 0:
                            cp("dve", o, ps[py][:, :], r=[("ps", py)], w=["accm"])
                        else:
                            stt("dve", o, ps[py][:, :], Wr3[:, tg, e_:e_ + 1], o, ALU.mult, ALU.add,
                                r=[("ps", py), "accm", "Wr"], w=["accm"])
            if debug and hf == 0:
                for nm, src_, n_, ky in (("gT_dbg", gT.rearrange("p a b -> p (a b)"), 4 * TH, "gT"),
                                         ("w1a_dbg", w1[0].rearrange("p a b -> p (a b)"), KC * FF, ("w1", 0)),
                                         ("w1b_dbg", w1[1].rearrange("p a b -> p (a b)"), KC * FF, ("w1", 1)),
                                         ("w3a_dbg", w3[0].rearrange("p a b -> p (a b)"), KC * FF, ("w3", 0)),
                                         ("w3b_dbg", w3[1].rearrange("p a b -> p (a b)"), KC * FF, ("w3", 1)),
                                         ("w2_dbg", w2[0].rearrange("p a b -> p (a b)"), 4 * D, ("w2", 0))):
                    dd = nc.dram_tensor(nm, [128, n_], BF16, kind="ExternalOutput").ap()
                    dma("sp", dd, src_, r=[ky], w=[nm])
                vth_dbg = nc.dram_tensor("vth_dbg", [128, KC * TH], BF16, kind="ExternalOutput").ap()
                dma("sp", vth_dbg, vTh.rearrange("p a b -> p (a b)"), r=["vTh"], w=["vth_dbg"])
                acc_dbg = nc.dram_tensor("acc_dbg", [128, NTH * D], F32, kind="ExternalOutput").ap()
                dma("sp", acc_dbg, accm.rearrange("p a b -> p (a b)"), r=["accm"], w=["acc_dbg"])
            settle()
            A.reset(w_lo, ARENA_F)
            g2b = A.f32(D); l2g = A.f32(D); l2b = A.f32(D); h1t = A.f32(D); ot = [A.f32(D) for _ in range(2)]
            stats = A.f32(24); mv = A.f32(2); rs = A.f32(1)
            dma("sp", g2b, mod_s[7], r=[("mod_s", 7)], w=["bcf"])
            dma("sp", l2g, ln_bc[2], w=["bcf"])
            dma("sp", l2b, ln_bc[3], w=["bcf"])
            for t in range(NTH):
                tsl = slice(hf * TH + t * 128, hf * TH + (t + 1) * 128)
                o_ = ot[t % 2]; ok = ("ot", t % 2)
                dma("sp", h1t, h1_s[tsl, :], r=["h1_s"], w=["h1t"])
                tt("dve", o_, accm[:, t, :], g2b, ALU.mult, r=["accm", "bcf"], w=[ok])
                stt("dve", o_, h1t, float(ALPHA), o_, ALU.mult, ALU.add, r=["h1t", ok], w=[ok])
                layer_norm(o_, ok, stats, mv, rs)
                tt("dve", o_, o_, l2g, ALU.mult, r=[ok, "bcf"], w=[ok])
                tt("dve", o_, o_, l2b, ALU.add, r=[ok, "bcf"], w=[ok])
                dma("sp", out[tsl, :], o_, r=[ok], w=["out"])
            settle()

        P.emit(stack)
    return nc


def _prep(inputs, S, nexp=NE, cores=range(8)):
    TOK = S // 8
    f = lambda a: np.ascontiguousarray(np.asarray(a, dtype=np.float32))
    x = f(inputs["x"])[0]; c = f(inputs["c"])[0]; ctx = f(inputs["ctx"])[0]; cc = f(inputs["c_ctx"])
    cT = np.zeros((128, 32), np.float32)
    cT[:, 0::2] = c.reshape(16, 128).T
    cT[:, 1::2] = cc.reshape(16, 128).T
    bc = lambda v: np.ascontiguousarray(np.broadcast_to(f(v).reshape(1, -1), (128, f(v).size)))
    conv_w = f(inputs["conv_w"])[0]
    conv_wT = np.ascontiguousarray(conv_w.reshape(4, 8, 128).transpose(2, 1, 0).reshape(128, 32))
    conv_bT = np.ascontiguousarray(f(inputs["conv_b"])[0].reshape(8, 128).T)
    wa = f(inputs["rg_wa"])[0]; wx = f(inputs["rg_wx"])[0]
    rg_w = np.concatenate([wa.reshape(16, 128, 128), wx.reshape(16, 128, 128)], 0)
    rg_w = np.ascontiguousarray(rg_w.transpose(1, 0, 2).reshape(128, 32 * 128))
    vecT = lambda v: f(v)[0].reshape(16, 128).T
    rg_vec = np.ascontiguousarray(np.concatenate([vecT(inputs["rg_ba"]), vecT(inputs["rg_bx"]), vecT(inputs["rg_lam"])], 1))
    ln_bc = np.stack([bc(inputs["ln1_g"]), bc(inputs["ln1_b"]), bc(inputs["ln2_g"]), bc(inputs["ln2_b"])], 0)
    qk_g = np.ascontiguousarray(np.stack([f(inputs["q_norm"])[0], f(inputs["k_norm"])[0]], 1))
    half = 32
    inv_freq = (np.float32(10000.0) ** (-np.arange(half, dtype=np.float32) / np.float32(half))).astype(np.float32)
    t = np.arange(S)
    row = (t // 64).astype(np.float32); col = (t % 64).astype(np.float32)
    ang = np.zeros((128, S), np.float32)
    for d in range(128):
        pos = row if d < 64 else col
        ang[d] = pos * inv_freq[(d % 64) % half]
    cs_all = np.stack([np.cos(ang), np.sin(ang)], 0).astype(np.float32)
    Rm = np.zeros((128, 128), np.float32)
    for base in (0, 64):
        for i in range(32):
            Rm[base + i, base + i + 32] = -1.0
            Rm[base + 32 + i, base + i] = 1.0
    consts = np.stack([np.eye(128, dtype=np.float32), np.ones((128, 128), np.float32),
                       np.ascontiguousarray(Rm.T), np.zeros((128, 128), np.float32)], 0)
    w_e1 = np.concatenate([f(inputs["w_e1"])[0][:nexp], f(inputs["w_s1"])], 0)
    w_e3 = np.concatenate([f(inputs["w_e3"])[0][:nexp], f(inputs["w_s3"])], 0)
    w_e2 = np.concatenate([f(inputs["w_e2"])[0][:nexp], f(inputs["w_s2"])], 0)
    common = dict(
        x_all=x, ctx=ctx, cT=cT, w_mod=f(inputs["w_mod"])[0], b_mod_bc=bc(inputs["b_mod"]),
        w_in=f(inputs["w_in"])[0], qk_g=qk_g, conv_wT=conv_wT, conv_bT=conv_bT, rg_w=rg_w, rg_vec=rg_vec,
        w_out=f(inputs["w_out"])[0], ln_bc=ln_bc, w_router=f(inputs["w_router"])[0], eb_bc=bc(inputs["e_bias"]),
        w_e1=w_e1, w_e3=w_e3, w_e2=w_e2, cs_all=cs_all, consts=consts)
    maps = []
    for j in cores:
        m = dict(common)
        m["x_own"] = np.ascontiguousarray(x[j * TOK:(j + 1) * TOK])
        m["cs_own"] = np.ascontiguousarray(cs_all[:, :, j * TOK:(j + 1) * TOK])
        s = np.zeros((128, 8), np.float32); s[:, j] = 1.0
        m["sel"] = s
        maps.append(m)
    return maps


_NC_CACHE = {}


def kernel(**inputs):
    S = int(np.asarray(inputs["x"]).shape[1])
    if S not in _NC_CACHE:
        _NC_CACHE[S] = build(S)
    nc = _NC_CACHE[S]
    maps = _prep(inputs, S)
    res = run_bass_kernel_spmd(nc, maps, core_ids=list(range(8)))
    outs = [np.asarray(r["out"], dtype=np.float32) for r in res.results]
    return np.concatenate(outs, 0)[None, :, :]
```
